# Optimizing a Trainium2 kernel written in Bass

```python
import jax, jax.numpy as jnp
from jax import lax
import numpy as np

D_MODEL = 2048
BATCH = 16
SEQ = 256
DEPTH = 2
DEC_BATCH = 4
DEC_SEQ = 2048
PAST_LEN = 256

GRID_W = 64
CHUNK = 128
Q_BLOCK = 128
HEAD_DIM = 128
N_Q_HEADS = 8
N_KV_HEADS = 2
Q_PER_KV = N_Q_HEADS // N_KV_HEADS
ATTN_WIDTH = N_Q_HEADS * HEAD_DIM
KV_WIDTH = N_KV_HEADS * HEAD_DIM
N_SGU_HEADS = 8
SGU_HEAD_DIM = 128
SGU_WIDTH = N_SGU_HEADS * SGU_HEAD_DIM
MIX_WIDTH = ATTN_WIDTH + SGU_WIDTH
IN_WIDTH = ATTN_WIDTH + 2 * KV_WIDTH + 2 * SGU_WIDTH
ROPE_THETA = 10000.0
ROPE_AXIS_DIM = HEAD_DIM // 2
D_FF = 5632
N_EXPERTS = 8
TOP_K = 2
D_FF_EXPERT = 2816
N_DENSE = (DEPTH + 1) // 2
N_MOE = DEPTH // 2
N_MOD = 6
EPS = 1e-6

kernel_name = "hybrid_sgu_gqa_diffusion_step"


def rmsnorm(x, g):
    xf = x.astype(jnp.float32)
    y = xf * lax.rsqrt(jnp.mean(xf * xf, axis=-1, keepdims=True) + EPS)
    return (y * g.astype(jnp.float32)).astype(x.dtype)


def axial_angles(n_tokens):
    n_rows = n_tokens // GRID_W
    rows = jnp.broadcast_to(jnp.arange(n_rows)[:, None], (n_rows, GRID_W)).reshape(-1)
    cols = jnp.broadcast_to(jnp.arange(GRID_W)[None, :], (n_rows, GRID_W)).reshape(-1)
    inv = ROPE_THETA ** (-jnp.arange(0, ROPE_AXIS_DIM, 2, dtype=jnp.float32) / ROPE_AXIS_DIM)
    return rows.astype(jnp.float32)[:, None] * inv, cols.astype(jnp.float32)[:, None] * inv


def rotate_half_axis(x, ang):
    half = ROPE_AXIS_DIM // 2
    x1, x2 = x[..., :half], x[..., half:]
    cos = jnp.cos(ang)[:, None, :]
    sin = jnp.sin(ang)[:, None, :]
    return jnp.concatenate([x1 * cos - x2 * sin, x2 * cos + x1 * sin], axis=-1)


def apply_rope_2d(x):
    ang_r, ang_c = axial_angles(x.shape[1])
    xf = x.astype(jnp.float32)
    out = jnp.concatenate([rotate_half_axis(xf[..., :ROPE_AXIS_DIM], ang_r),
                           rotate_half_axis(xf[..., ROPE_AXIS_DIM:], ang_c)], axis=-1)
    return out.astype(x.dtype)


def block_attention(q, k, v):
    B, Lq = q.shape[0], q.shape[1]
    nb = Lq // Q_BLOCK
    qb = q.reshape(B, nb, Q_BLOCK, N_KV_HEADS, Q_PER_KV, HEAD_DIM).transpose(1, 0, 2, 3, 4, 5)
    scale = HEAD_DIM ** -0.5

    def one_block(q_blk):
        s = jnp.einsum('bqkgd,bskd->bkgqs', q_blk, k, preferred_element_type=jnp.float32) * scale
        p = jax.nn.softmax(s, axis=-1)
        return jnp.einsum('bkgqs,bskd->bqkgd', p.astype(v.dtype), v)

    out = lax.map(one_block, qb)
    return out.transpose(1, 0, 2, 3, 4, 5).reshape(B, Lq, ATTN_WIDTH)


def chunk_spatial_gating(u, g, w_s, b_s, g_norm):
    B, L = u.shape[0], u.shape[1]
    n = L // CHUNK
    gh = rmsnorm(g.reshape(B, n, CHUNK, N_SGU_HEADS, SGU_HEAD_DIM), g_norm)
    mixed = jnp.einsum('hpq,bnqhd->bnphd', w_s, gh) + b_s.T[None, None, :, :, None]
    return u * mixed.reshape(B, L, SGU_WIDTH)


def modulation(cond, w_ada, b_ada):
    m = jax.nn.silu(cond[..., None, :]) @ w_ada + b_ada
    return jnp.split(m, N_MOD, axis=-1)


def mix_projections(h, w_in, q_norm, k_norm):
    B, L = h.shape[0], h.shape[1]
    proj = h @ w_in
    q, k, v, u, g = jnp.split(proj, [ATTN_WIDTH, ATTN_WIDTH + KV_WIDTH, ATTN_WIDTH + 2 * KV_WIDTH,
                                     ATTN_WIDTH + 2 * KV_WIDTH + SGU_WIDTH], axis=-1)
    q = rmsnorm(q.reshape(B, L, N_Q_HEADS, HEAD_DIM), q_norm)
    k = rmsnorm(k.reshape(B, L, N_KV_HEADS, HEAD_DIM), k_norm)
    v = v.reshape(B, L, N_KV_HEADS, HEAD_DIM)
    return q, k, v, u, g


def merge_groups(attn_out, sgu_out, out_norm, w_out):
    o = jnp.concatenate([rmsnorm(attn_out, out_norm[:ATTN_WIDTH]),
                         rmsnorm(sgu_out, out_norm[ATTN_WIDTH:])], axis=-1)
    return o @ w_out


def swiglu(h, w_gate, w_up, w_down):
    return (jax.nn.silu(h @ w_gate) * (h @ w_up)) @ w_down


def moe_swiglu(h, w_router, b_router, w_gate, w_up, w_down):
    B, L, D = h.shape
    t = h.reshape(-1, D)
    logits = (t @ w_router).astype(jnp.float32) + b_router.astype(jnp.float32)
    top_val, top_idx = lax.top_k(logits, TOP_K)
    top_w = jax.nn.softmax(top_val, axis=-1)
    combine = jnp.sum(jax.nn.one_hot(top_idx, N_EXPERTS, dtype=jnp.float32) * top_w[..., None], axis=1)
    out = jnp.zeros_like(t)
    for e in range(N_EXPERTS):
        out = out + combine[:, e:e + 1].astype(t.dtype) * swiglu(t, w_gate[e], w_up[e], w_down[e])
    return out.reshape(B, L, D)


def channel_mixer(i, h, ffn_w_gate, ffn_w_up, ffn_w_down, w_router, b_router,
                  moe_w_gate, moe_w_up, moe_w_down):
    j = i // 2
    if i % 2 == 0:
        return swiglu(h, ffn_w_gate[j], ffn_w_up[j], ffn_w_down[j])
    return moe_swiglu(h, w_router[j], b_router[j], moe_w_gate[j], moe_w_up[j], moe_w_down[j])


def setup_inputs(seed: int = 0) -> dict:
    key = jax.random.key(seed)
    ks = jax.random.split(key, 32)
    f32 = jnp.float32
    nrm = lambda k, shape, s: jax.random.normal(k, shape, f32) * s
    D = D_MODEL
    return {
        "x_prompt": nrm(ks[0], (BATCH, SEQ, D), 1.0),
        "x_sample": nrm(ks[1], (DEC_BATCH, DEC_SEQ, D), 1.0),
        "cache_k": nrm(ks[2], (DEC_BATCH, DEPTH, PAST_LEN, N_KV_HEADS, HEAD_DIM), 1.0),
        "cache_v": nrm(ks[3], (DEC_BATCH, DEPTH, PAST_LEN, N_KV_HEADS, HEAD_DIM), 1.0),
        "c": nrm(ks[4], (DEC_BATCH, D), 1.0),
        "c_ctx": nrm(ks[5], (D,), 1.0),
        "w_ada": nrm(ks[6], (DEPTH, D, N_MOD * D), D ** -0.5),
        "b_ada": nrm(ks[7], (DEPTH, N_MOD * D), 0.01),
        "norm1_g": 1.0 + nrm(ks[8], (DEPTH, D), 0.02),
        "norm2_g": 1.0 + nrm(ks[9], (DEPTH, D), 0.02),
        "w_in": nrm(ks[10], (DEPTH, D, IN_WIDTH), D ** -0.5),
        "q_norm_g": 1.0 + nrm(ks[11], (DEPTH, HEAD_DIM), 0.02),
        "k_norm_g": 1.0 + nrm(ks[12], (DEPTH, HEAD_DIM), 0.02),
        "sgu_norm_g": 1.0 + nrm(ks[13], (DEPTH, N_SGU_HEADS, SGU_HEAD_DIM), 0.02),
        "w_spatial": nrm(ks[14], (DEPTH, N_SGU_HEADS, CHUNK, CHUNK), CHUNK ** -0.5),
        "b_spatial": 1.0 + nrm(ks[15], (DEPTH, N_SGU_HEADS, CHUNK), 0.01),
        "out_norm_g": 1.0 + nrm(ks[16], (DEPTH, MIX_WIDTH), 0.02),
        "w_out": nrm(ks[17], (DEPTH, MIX_WIDTH, D), MIX_WIDTH ** -0.5),
        "ffn_w_gate": nrm(ks[18], (N_DENSE, D, D_FF), D ** -0.5),
        "ffn_w_up": nrm(ks[19], (N_DENSE, D, D_FF), D ** -0.5),
        "ffn_w_down": nrm(ks[20], (N_DENSE, D_FF, D), D_FF ** -0.5),
        "w_router": nrm(ks[21], (N_MOE, D, N_EXPERTS), D ** -0.5),
        "b_router": nrm(ks[22], (N_MOE, N_EXPERTS), 0.01),
        "moe_w_gate": nrm(ks[23], (N_MOE, N_EXPERTS, D, D_FF_EXPERT), D ** -0.5),
        "moe_w_up": nrm(ks[24], (N_MOE, N_EXPERTS, D, D_FF_EXPERT), D ** -0.5),
        "moe_w_down": nrm(ks[25], (N_MOE, N_EXPERTS, D_FF_EXPERT, D), D_FF_EXPERT ** -0.5),
    }


def reference(x_prompt, x_sample, cache_k, cache_v, c, c_ctx, w_ada, b_ada, norm1_g, norm2_g,
              w_in, q_norm_g, k_norm_g, sgu_norm_g, w_spatial, b_spatial, out_norm_g, w_out,
              ffn_w_gate, ffn_w_up, ffn_w_down, w_router, b_router, moe_w_gate, moe_w_up, moe_w_down):
    x = x_prompt
    ks_out, vs_out = [], []
    for i in range(DEPTH):
        sh1, sc1, g1, sh2, sc2, g2 = modulation(c_ctx, w_ada[i], b_ada[i])
        h = rmsnorm(x, norm1_g[i]) * (1.0 + sc1) + sh1
        q, k, v, u, g = mix_projections(h, w_in[i], q_norm_g[i], k_norm_g[i])
        attn = block_attention(q, k, v)
        sgu = chunk_spatial_gating(u, g, w_spatial[i], b_spatial[i], sgu_norm_g[i])
        x = x + g1 * merge_groups(attn, sgu, out_norm_g[i], w_out[i])
        h2 = rmsnorm(x, norm2_g[i]) * (1.0 + sc2) + sh2
        x = x + g2 * channel_mixer(i, h2, ffn_w_gate, ffn_w_up, ffn_w_down, w_router, b_router,
                                   moe_w_gate, moe_w_up, moe_w_down)
        ks_out.append(k)
        vs_out.append(v)
    y_prompt = x
    new_cache_k = jnp.stack(ks_out, axis=1)
    new_cache_v = jnp.stack(vs_out, axis=1)

    x = x_sample
    for i in range(DEPTH):
        sh1, sc1, g1, sh2, sc2, g2 = modulation(c, w_ada[i], b_ada[i])
        h = rmsnorm(x, norm1_g[i]) * (1.0 + sc1) + sh1
        q, k, v, u, g = mix_projections(h, w_in[i], q_norm_g[i], k_norm_g[i])
        q = apply_rope_2d(q)
        k = apply_rope_2d(k)
        k_all = jnp.concatenate([cache_k[:, i].astype(k.dtype), k], axis=1)
        v_all = jnp.concatenate([cache_v[:, i].astype(v.dtype), v], axis=1)
        attn = block_attention(q, k_all, v_all)
        sgu = chunk_spatial_gating(u, g, w_spatial[i], b_spatial[i], sgu_norm_g[i])
        x = x + g1 * merge_groups(attn, sgu, out_norm_g[i], w_out[i])
        h2 = rmsnorm(x, norm2_g[i]) * (1.0 + sc2) + sh2
        x = x + g2 * channel_mixer(i, h2, ffn_w_gate, ffn_w_up, ffn_w_down, w_router, b_router,
                                   moe_w_gate, moe_w_up, moe_w_down)
    y_sample = x
    return (y_prompt, y_sample, new_cache_k, new_cache_v)
```

```python
import numpy as np
from contextlib import ExitStack
import concourse.bass as bass
import concourse.mybir as mybir
from concourse.bass_utils import run_bass_kernel_spmd

F32 = mybir.dt.float32
BF16 = mybir.dt.bfloat16
ALU = mybir.AluOpType
AF = mybir.ActivationFunctionType

ENGS = ("pe", "act", "dve", "pool", "sp")

D = 2048
KC = 16
TT = 512
NL = 2
INW = 3584
DFF = 5632
NE = 8
DFE = 2816
EPS = 1e-6
NCORES = 8


class Buf:
    __slots__ = ("name", "w", "r", "excl")

    def __init__(self, name="", excl=False):
        self.name = name
        self.excl = excl
        self.w = None
        self.r = []


class Prog:
    NDMA = 6

    def __init__(self, nc, stack):
        self.nc = nc
        self.stack = stack
        self.ops = {e: [] for e in ENGS}
        self.sem = {}
        self.cnt = {}
        self.seen = {e: {} for e in ENGS}
        for e in ENGS:
            self._mk("c_" + e)
        self.dma_rr = {}
        for q in ("sp", "pool"):
            for i in range(self.NDMA):
                self._mk("d_%s_%d" % (q, i))
            self.dma_rr[q] = 0
        self.n_wait = 0
        self.n_ops = 0

    def _mk(self, key):
        self.sem[key] = self.stack.enter_context(self.nc.semaphore(key))
        self.cnt[key] = 0

    def _deps(self, reads, writes):
        d = {}

        def add(ev):
            if ev is None:
                return
            k, v = ev
            if d.get(k, 0) < v:
                d[k] = v
        for b in reads:
            add(b.w)
        for b in writes:
            add(b.w)
            for ev in b.r:
                add(ev)
        return d

    def _emit_waits(self, eng, deps):
        seen = self.seen[eng]
        for k, v in deps.items():
            if eng == "pe" and k == "c_pe":
                continue
            if seen.get(k, 0) >= v:
                continue
            seen[k] = v
            sem = self.sem[k]
            self.ops[eng].append(lambda E, sem=sem, v=v: E.wait_ge(sem, v))
            self.n_wait += 1

    def _record(self, ev, reads, writes):
        for b in reads:
            b.r.append(ev)
            if len(b.r) > 48:
                m = {}
                for k, v in b.r:
                    if m.get(k, 0) < v:
                        m[k] = v
                b.r = list(m.items())
        for b in writes:
            b.w = ev
            b.r = []

    def op(self, eng, fn, reads=(), writes=()):
        if any(b.excl for b in reads):
            writes = list(writes) + [b for b in reads if b.excl]
            reads = [b for b in reads if not b.excl]
        deps = self._deps(reads, writes)
        self._emit_waits(eng, deps)
        key = "c_" + eng
        self.cnt[key] += 1
        v = self.cnt[key]
        sem = self.sem[key]
        self.ops[eng].append(lambda E, fn=fn, sem=sem: fn(E).then_inc(sem, 1))
        self._record((key, v), reads, writes)
        self.n_ops += 1

    def dma(self, q, out, in_, reads=(), writes=()):
        deps = self._deps(reads, writes)
        i = self.dma_rr[q]
        self.dma_rr[q] = (i + 1) % self.NDMA
        key = "d_%s_%d" % (q, i)
        if self.cnt[key] > 0 and deps.get(key, 0) < self.cnt[key]:
            deps[key] = self.cnt[key]
        self._emit_waits(q, deps)
        self.cnt[key] += 16
        v = self.cnt[key]
        sem = self.sem[key]
        self.ops[q].append(lambda E, out=out, in_=in_, sem=sem: E.dma_start(out=out, in_=in_).then_inc(sem, 16))
        self._record((key, v), reads, writes)
        self.n_ops += 1

    def finish(self):
        deps = {k: v for k, v in self.cnt.items() if k.startswith("d_") and v > 0}
        self._emit_waits("sp", deps)
        ops = self.ops
        with self.nc.Block() as block:
            @block.tensor
            def _(E):
                for f in ops["pe"]:
                    f(E)

            @block.scalar
            def _(E):
                for f in ops["act"]:
                    f(E)

            @block.vector
            def _(E):
                for f in ops["dve"]:
                    f(E)

            @block.gpsimd
            def _(E):
                for f in ops["pool"]:
                    f(E)

            @block.sync
            def _(E):
                for f in ops["sp"]:
                    f(E)


class T:
    def __init__(self, t, n=1, excl=False, name=""):
        self.t = t
        self.b = [Buf(name + str(i), excl) for i in range(n)]
        self.B = self.b[0]


def build_program(debug=0):
    nc = bass.Bass("TRN2", target_bir_lowering=False)
    dt_in = lambda name, shape: nc.dram_tensor(name, list(shape), F32, kind="ExternalInput").ap()
    dt_out = lambda name, shape: nc.dram_tensor(name, list(shape), F32, kind="ExternalOutput").ap()
    xs_d = dt_in("xs", (4, TT, D))
    xp_d = dt_in("xp", (TT, D))
    ropec_d = dt_in("ropec", (4, 128, TT))
    ropes_d = dt_in("ropes", (4, 128, TT))
    ck_d = dt_in("ck", (NL, 256, 256))
    cv_d = dt_in("cv", (NL, 256, 256))
    cvec_d = dt_in("cvec", (128, KC, 2))
    n1g_d = dt_in("n1g", (128, NL, KC))
    n2g_d = dt_in("n2g", (128, NL, KC))
    bada_d = dt_in("bada", (128, NL, 96))
    qg_d = dt_in("qg", (128, NL))
    kg_d = dt_in("kg", (128, NL))
    sgn_d = dt_in("sgn", (128, NL, 8))
    bsrep_d = dt_in("bsrep", (128, NL, 8, 128))
    wsT_d = dt_in("wsT", (128, NL, 8, 128))
    ong_d = dt_in("ong", (128, NL, KC))
    wr_d = dt_in("wr", (128, KC, NE))
    brrep_d = dt_in("brrep", (128, NE))
    ident_d = dt_in("ident", (128, 128))
    rt_d = dt_in("rt", (128, 128))
    wada_d = dt_in("w_ada", (NL, D, 6 * D))
    win_d = dt_in("w_in", (NL, D, INW))
    wout_d = dt_in("w_out", (NL, D, D))
    fg_d = dt_in("ffn_w_gate", (1, D, DFF))
    fu_d = dt_in("ffn_w_up", (1, D, DFF))
    fd_d = dt_in("ffn_w_down", (1, DFF, D))
    mg_d = dt_in("moe_w_gate", (1, NE, D, DFE))
    mu_d = dt_in("moe_w_up", (1, NE, D, DFE))
    md_d = dt_in("moe_w_down", (1, NE, DFE, D))
    yp_d = dt_out("yp", (TT, D))
    ys_d = dt_out("ys", (2, TT, D))
    nk_d = dt_out("nk", (2, NL, 256, 256))
    nv_d = dt_out("nv", (2, NL, 256, 256))
    bIN = Buf("dram_in")

    with ExitStack() as st:
        P = Prog(nc, st)

        def sb(name, shape, dt, n=1, stack=st):
            return T(stack.enter_context(nc.sbuf_tensor("s_" + name, list(shape), dt)), n, name=name)

        bank = {}
        for nm in ("A0", "A1", "B0", "B1", "C", "Dk", "G", "H"):
            bank[nm] = T(st.enter_context(nc.psum_tensor("ps" + nm, [128, 512], F32)), 1, excl=True, name="ps" + nm)

        ident = sb("ident", (128, 128), F32)
        identb = sb("identb", (128, 128), BF16)
        rt = sb("rt", (128, 128), F32)
        onesb = sb("onesb", (128, 128), BF16)
        epsc = sb("epsc", (128, 1), F32)
        cvec = sb("cvec", (128, KC, 2), F32)
        scb = sb("scb", (128, KC, 2), BF16)
        n1g = sb("n1g", (128, NL, KC), F32)
        n2g = sb("n2g", (128, NL, KC), F32)
        bada = sb("bada", (128, NL, 96), F32)
        qg = sb("qg", (128, NL), F32)
        kg = sb("kg", (128, NL), F32)
        sgn = sb("sgn", (128, NL, 8), F32)
        bsrep = sb("bsrep", (128, 1, 8, 128), F32)
        wsT = sb("wsT", (128, NL, 8, 128), BF16)
        ong = sb("ong", (128, NL, KC), F32)
        wr = sb("wr", (128, KC, NE), F32)
        brrep = sb("brrep", (128, NE), F32)
        mod = sb("mod", (128, NL, 2, 96), F32)
        A1 = sb("A1", (128, NL, 2, KC), F32)
        A2 = sb("A2", (128, NL, 2, KC), F32)

        for (t_, d_) in ((ident, ident_d), (rt, rt_d), (cvec, cvec_d), (n1g, n1g_d), (n2g, n2g_d), (bada, bada_d),
                         (qg, qg_d), (kg, kg_d), (sgn, sgn_d), (ong, ong_d),
                         (wr, wr_d), (brrep, brrep_d)):
            P.dma("sp", t_.t[:], d_, reads=[bIN], writes=[t_.B])
        P.op("dve", lambda E: E.memset(onesb.t[:], 1.0), writes=[onesb.B])
        P.op("dve", lambda E: E.memset(epsc.t[:], EPS), writes=[epsc.B])
        P.op("dve", lambda E: E.tensor_copy(identb.t[:], ident.t[:]), reads=[ident.B], writes=[identb.B])
        P.dma("pool", wsT.t[:], wsT_d, reads=[bIN], writes=[wsT.B])
        P.op("act", lambda E: E.activation(out=scb.t[:], in_=cvec.t[:], func=AF.Silu), reads=[cvec.B], writes=[scb.B])

        NSLOT = 4
        wslots = [sb("wslot%d" % i, (128, 4096), BF16) for i in range(NSLOT)]
        wstate = {"plan": [], "issued": 0, "taken": 0, "released": 0}

        def w_plan(items):
            wstate["plan"].extend(items)

        def w_issue_upto(n):
            while wstate["issued"] < min(n, len(wstate["plan"])):
                i = wstate["issued"]
                src, shape = wstate["plan"][i]
                s = wslots[i % NSLOT]
                if shape[0] == "in":
                    ncols = shape[1]
                    dst = s.t[:, 0:KC * ncols].rearrange("p (k n) -> p k n", k=KC)
                    P.dma("pool", dst, src.rearrange("(k p) n -> p k n", p=128), reads=[bIN], writes=[s.B])
                else:
                    nch = shape[1]
                    dst = s.t[:, 0:nch * D].rearrange("p (k n) -> p k n", k=nch)
                    P.dma("pool", dst, src.rearrange("(k p) n -> p k n", p=128), reads=[bIN], writes=[s.B])
                wstate["issued"] += 1

        def w_get():
            i = wstate["taken"]
            assert i < len(wstate["plan"]), "weight plan exhausted"
            w_issue_upto(max(wstate["released"] + NSLOT, i + 1))
            assert wstate["issued"] <= wstate["released"] + NSLOT and i - wstate["released"] < NSLOT
            wstate["taken"] += 1
            s = wslots[i % NSLOT]
            src, shape = wstate["plan"][i]
            if shape[0] == "in":
                return s.t[:, 0:KC * shape[1]].rearrange("p (k n) -> p k n", k=KC), s.B
            return s.t[:, 0:shape[1] * D].rearrange("p (k n) -> p k n", k=shape[1]), s.B

        def w_release():
            wstate["released"] = wstate["taken"]
            w_issue_upto(wstate["released"] + NSLOT)

        def mm(out_bank, out_ap, lhsT, rhs, start, stop, reads):
            P.op("pe", lambda E: E.matmul(out_ap, lhsT=lhsT, rhs=rhs, start=start, stop=stop), reads=reads, writes=[out_bank.B])

        rr = {"act_dve": 0}

        def plan_mod():
            return [(wada_d[l, :, pc * 256:(pc + 1) * 256], ("in", 256)) for l in range(NL) for pc in range(48)]

        def emit_mod():
            for l in range(NL):
                for pc in range(48):
                    w, wb = w_get()
                    bk = bank["A%d" % (pc % 2)]
                    for c2 in range(2):
                        for kc in range(KC):
                            mm(bk, bk.t[:, c2 * 2:c2 * 2 + 2], w[:, kc, c2 * 128:(c2 + 1) * 128], scb.t[:, kc, :],
                               kc == 0, kc == KC - 1, [wb, scb.B])
                    w_release()
                    for c2 in range(2):
                        j = pc * 2 + c2
                        P.op("dve", lambda E, l=l, j=j, c2=c2, bk=bk: E.tensor_scalar(
                            out=mod.t[:, l, :, j], in0=bk.t[:, c2 * 2:c2 * 2 + 2], scalar1=bada.t[:, l, j:j + 1], scalar2=None,
                            op0=ALU.add), reads=[bk.B, bada.B], writes=[mod.B])
            for l in range(NL):
                for cnd in range(2):
                    for (A, g_, off) in ((A1, n1g, 16), (A2, n2g, 64)):
                        P.op("dve", lambda E, A=A, g_=g_, off=off, l=l, cnd=cnd: E.scalar_tensor_tensor(
                            out=A.t[:, l, cnd, :], in0=mod.t[:, l, cnd, off:off + 16], scalar=1.0, in1=g_.t[:, l, :],
                            op0=ALU.add, op1=ALU.mult), reads=[mod.B, g_.B], writes=[A.B])

        def mvec(l, cnd, which):
            return mod.t[:, l, cnd, which * 16:(which + 1) * 16]

        xstage = sb("xstage", (128, 1024), F32, n=1)
        kvstage = sb("kvstage", (128, 4, 256), F32)
        xstg = [(xstage.t[:], xstage.B), (kvstage.t[:].rearrange("p b d -> p (b d)"), kvstage.B)]

        def load_x(src, x):
            for blk in range(4):
                for g4 in range(4):
                    st_ap, st_b = xstg[(blk * 2 + g4 // 2) % 2]
                    if g4 % 2 == 0:
                        P.dma("sp", st_ap, src[blk * 128:(blk + 1) * 128, (g4 // 2) * 1024:(g4 // 2 + 1) * 1024], reads=[bIN], writes=[st_b])
                    bk = bank["H"] if g4 % 2 == 0 else bank["G"]
                    for j in range(4):
                        kc = g4 * 4 + j
                        kl = kc % 8
                        P.op("pe", lambda E, bk=bk, j=j, kl=kl, st_ap=st_ap: E.transpose(bk.t[:, j * 128:(j + 1) * 128],
                                                                                        st_ap[:, kl * 128:(kl + 1) * 128], ident.t[:]),
                             reads=[st_b, ident.B], writes=[bk.B])
                    dst = x.t[:, g4 * 4:(g4 + 1) * 4, blk * 128:(blk + 1) * 128]
                    srcp = bk.t[:].rearrange("p (j t) -> p j t", j=4)
                    if g4 % 2 == 0:
                        P.op("dve", lambda E, dst=dst, srcp=srcp: E.tensor_copy(dst, srcp), reads=[bk.B], writes=[x.B])
                    else:
                        P.op("act", lambda E, dst=dst, srcp=srcp: E.activation(out=dst, in_=srcp, func=AF.Copy), reads=[bk.B], writes=[x.B])

        def store_x(x, dst):
            for blk in range(4):
                for g4 in range(4):
                    st_ap, st_b = xstg[(blk * 2 + g4 // 2) % 2]
                    bk = bank["H"] if g4 % 2 == 0 else bank["G"]
                    for j in range(4):
                        kc = g4 * 4 + j
                        P.op("pe", lambda E, bk=bk, j=j, kc=kc, blk=blk: E.transpose(
                            bk.t[:, j * 128:(j + 1) * 128], x.t[:, kc, blk * 128:(blk + 1) * 128], ident.t[:]),
                            reads=[x.B, ident.B], writes=[bk.B])
                    dsts = st_ap[:, (g4 % 2) * 512:(g4 % 2 + 1) * 512]
                    if g4 % 2 == 0:
                        P.op("dve", lambda E, dsts=dsts, bk=bk: E.tensor_copy(dsts, bk.t[:]), reads=[bk.B], writes=[st_b])
                    else:
                        P.op("act", lambda E, dsts=dsts, bk=bk: E.activation(out=dsts, in_=bk.t[:], func=AF.Copy), reads=[bk.B], writes=[st_b])
                        P.dma("sp", dst[blk * 128:(blk + 1) * 128, (g4 // 2) * 1024:(g4 // 2 + 1) * 1024], st_ap, reads=[st_b], writes=[Buf()])

        sqr = sb("sqr", (128, 3, TT), BF16, n=3)
        rstd = sb("rstd", (128, TT), F32)
        tmpf = sb("tmpf", (128, 2, TT), F32, n=2)

        def sum_sq_to_rstd(n_feat, dst):
            G = bank["G"]
            P.op("act", lambda E: E.activation(out=dst.t[:], in_=G.t[:], func=AF.Sqrt, bias=epsc.t[:, 0:1], scale=1.0 / n_feat),
                 reads=[G.B, epsc.B], writes=[dst.B])
            P.op("dve", lambda E: E.reciprocal(out=dst.t[:], in_=dst.t[:]), reads=[dst.B], writes=[dst.B])

        def norm_mod(x, h, Avec, Bvec):
            G = bank["G"]
            for kc in range(KC):
                i = kc % 2
                P.op("act", lambda E, kc=kc, i=i: E.activation(out=sqr.t[:, i, :], in_=x.t[:, kc, :], func=AF.Square),
                     reads=[x.B], writes=[sqr.b[i]])
                mm(G, G.t[:], onesb.t[:], sqr.t[:, i, :], kc == 0, kc == KC - 1, [onesb.B, sqr.b[i]])
            sum_sq_to_rstd(float(D), rstd)
            for kc in range(KC):
                i = kc % 2
                P.op("dve", lambda E, kc=kc, i=i: E.scalar_tensor_tensor(
                    out=tmpf.t[:, i, :], in0=x.t[:, kc, :], scalar=Avec[:, kc:kc + 1], in1=rstd.t[:], op0=ALU.mult, op1=ALU.mult),
                    reads=[x.B, rstd.B, A1.B, A2.B], writes=[tmpf.b[i]])
                P.op("act", lambda E, kc=kc, i=i: E.activation(out=h.t[:, kc, :], in_=tmpf.t[:, i, :], func=AF.Identity,
                                                              bias=Bvec[:, kc:kc + 1], scale=1.0),
                     reads=[tmpf.b[i], mod.B], writes=[h.B])

        hq = sb("hq", (128, TT), F32)
        hq1 = sb("hq1", (128, TT), F32)
        hq2 = [hq, hq1]
        hrs = sb("hrs", (128, TT), F32)
        r1 = sb("r1", (128, TT), F32)
        r2 = sb("r2", (128, TT), F32)
        qb = sb("qb", (128, TT), BF16)
        qb1 = sb("qb1", (128, TT), BF16)
        qb2 = [qb, qb1]
        ropec = sb("ropec", (128, TT), F32)
        ropes = sb("ropes", (128, TT), F32)

        def head_norm(ps, gain_ap, out_f32=None, out_bf=None):
            G = bank["G"]
            P.op("act", lambda E: E.activation(out=sqr.t[:, 0, :], in_=ps.t[:], func=AF.Square), reads=[ps.B], writes=[sqr.b[0]])
            mm(G, G.t[:], onesb.t[:], sqr.t[:, 0, :], True, True, [onesb.B, sqr.b[0]])
            sum_sq_to_rstd(128.0, hrs)
            if out_f32 is not None:
                P.op("dve", lambda E: E.scalar_tensor_tensor(out=out_f32.t[:], in0=ps.t[:], scalar=gain_ap, in1=hrs.t[:],
                                                             op0=ALU.mult, op1=ALU.mult), reads=[ps.B, hrs.B, qg.B, kg.B], writes=[out_f32.B])
            else:
                P.op("dve", lambda E: E.scalar_tensor_tensor(out=out_bf, in0=ps.t[:], scalar=gain_ap, in1=hrs.t[:],
                                                             op0=ALU.mult, op1=ALU.mult), reads=[ps.B, hrs.B, qg.B, kg.B], writes=[])

        def rope_to(src_f32, out_ap, out_buf):
            H = bank["H"]
            mm(H, H.t[:], rt.t[:], src_f32.t[:], True, True, [rt.B, src_f32.B])
            P.op("dve", lambda E: E.tensor_tensor(out=src_f32.t[:], in0=src_f32.t[:], in1=ropec.t[:], op=ALU.mult),
                 reads=[src_f32.B, ropec.B], writes=[src_f32.B])
            P.op("dve", lambda E: E.tensor_tensor(out=tmpf.t[:, 1, :], in0=H.t[:], in1=ropes.t[:], op=ALU.mult),
                 reads=[H.B, ropes.B], writes=[tmpf.b[1]])
            P.op("dve", lambda E: E.tensor_tensor(out=out_ap, in0=src_f32.t[:], in1=tmpf.t[:, 1, :], op=ALU.add),
                 reads=[src_f32.B, tmpf.b[1]], writes=[out_buf])


        def plan_kv(l):
            return [(win_d[l, :, 1024:1280], ("in", 256)), (win_d[l, :, 1280:1536], ("in", 256))]

        def emit_kv(l, h, KTb, Vb, kcol0, vblk0, rope, cache_out=None):
            wk, wkb = w_get()
            for kvh in range(2):
                bk = bank["A%d" % kvh]
                for kc in range(KC):
                    mm(bk, bk.t[:], wk[:, kc, kvh * 128:(kvh + 1) * 128], h.t[:, kc, :], kc == 0, kc == KC - 1, [wkb, h.B])
                dst = KTb.t[:, kvh, kcol0:kcol0 + TT]
                if rope:
                    head_norm(bk, kg.t[:, l:l + 1], out_f32=hq)
                    rope_to(hq, dst, KTb.B)
                else:
                    head_norm(bk, kg.t[:, l:l + 1], out_f32=hq)
                    P.op("act", lambda E, dst=dst: E.activation(out=dst, in_=hq.t[:], func=AF.Copy), reads=[hq.B], writes=[KTb.B])
                    if cache_out is not None:
                        H = bank["H"]
                        for blk in range(4):
                            P.op("pe", lambda E, blk=blk: E.transpose(H.t[:, blk * 128:(blk + 1) * 128], hq.t[:, blk * 128:(blk + 1) * 128], ident.t[:]),
                                 reads=[hq.B, ident.B], writes=[H.B])
                        P.op("dve", lambda E, kvh=kvh: E.tensor_copy(kvstage.t[:, :, kvh * 128:(kvh + 1) * 128],
                                                                     H.t[:].rearrange("p (b d) -> p b d", b=4)),
                             reads=[H.B], writes=[kvstage.B])
            if cache_out is not None:
                for blk in range(4):
                    P.dma("sp", nk_d[blk // 2, l, (blk % 2) * 128:(blk % 2 + 1) * 128, :], kvstage.t[:, blk, :],
                          reads=[kvstage.B], writes=[Buf()])
            w_release()
            wv, wvb = w_get()
            for blk in range(4):
                bk = bank["B%d" % (blk // 2)]
                o_ap = bk.t[:, (blk % 2) * 256:(blk % 2 + 1) * 256]
                for kc in range(KC):
                    mm(bk, o_ap, h.t[:, kc, blk * 128:(blk + 1) * 128], wv[:, kc, :], kc == 0, kc == KC - 1, [wvb, h.B])
                if blk % 2 == 1:
                    P.op("act", lambda E, bk=bk, blk=blk: E.activation(out=Vb.t[:, vblk0 + blk - 1:vblk0 + blk + 1, :],
                                                                      in_=bk.t[:].rearrange("p (b d) -> p b d", b=2), func=AF.Copy),
                         reads=[bk.B], writes=[Vb.B])
                    if cache_out is not None:
                        P.op("dve", lambda E, bk=bk, blk=blk: E.tensor_copy(kvstage.t[:, blk - 1:blk + 1, :],
                                                                            bk.t[:].rearrange("p (b d) -> p b d", b=2)),
                             reads=[bk.B], writes=[kvstage.B])
            w_release()
            if cache_out is not None:
                for blk in range(4):
                    P.dma("sp", nv_d[blk // 2, l, (blk % 2) * 128:(blk % 2 + 1) * 128, :], kvstage.t[:, blk, :],
                          reads=[kvstage.B], writes=[Buf()])

        PT = sb("PT", (128, 4, TT), BF16, n=4)
        uf = sb("uf", (128, TT), F32)
        ghat = sb("ghat", (128, 4, 256), BF16)
        gss = sb("gss", (128, 8), F32)
        gsq = sb("gsq", (128, 128), F32)
        ssa = sb("ssa", (128, TT), F32)
        sss = sb("sss", (128, TT), F32)
        SCALE = 1.0 / float(np.sqrt(128.0))

        def plan_mixer(l):
            items = []
            for hp in range(4):
                items.append((win_d[l, :, 2560 + hp * 256:2560 + (hp + 1) * 256], ("in", 256)))
                items.append((win_d[l, :, 1536 + hp * 256:1536 + (hp + 1) * 256], ("in", 256)))
                items.append((win_d[l, :, hp * 256:(hp + 1) * 256], ("in", 256)))
            for pc in range(8):
                items.append((wout_d[l, :, pc * 256:(pc + 1) * 256], ("in", 256)))
            return items

        deferred = []

        def flush():
            for f in deferred:
                f()
            del deferred[:]

        def accum_sumsq(src_f32_ap, src_buf, acc, first, si):
            G = bank["G"]
            if any(getattr(f, "si", None) == si for f in deferred):
                flush()
            P.op("act", lambda E: E.activation(out=sqr.t[:, si, :], in_=src_f32_ap, func=AF.Square), reads=[src_buf], writes=[sqr.b[si]])

            def part2():
                mm(G, G.t[:], onesb.t[:], sqr.t[:, si, :], True, True, [onesb.B, sqr.b[si]])
                if first:
                    P.op("dve", lambda E: E.tensor_copy(acc.t[:], G.t[:]), reads=[G.B], writes=[acc.B])
                else:
                    P.op("dve", lambda E: E.tensor_tensor(out=acc.t[:], in0=acc.t[:], in1=G.t[:], op=ALU.add), reads=[G.B, acc.B], writes=[acc.B])
            part2.si = si
            deferred.append(part2)

        def emit_mixer(l, cnd, x, h, o, KTb, Vb, groups, rope):
            P.dma("sp", bsrep.t[:, 0], bsrep_d[:, l], reads=[bIN], writes=[bsrep.B])
            for hp in range(4):
                c4 = hp
                wg_, wgb = w_get()
                for blk in range(4):
                    bk = bank["A%d" % (blk % 2)]
                    o_ap = bk.t[:, 0:256]
                    for kc in range(KC):
                        mm(bk, o_ap, h.t[:, kc, blk * 128:(blk + 1) * 128], wg_[:, kc, :], kc == 0, kc == KC - 1, [wgb, h.B])
                    P.op("dve", lambda E, c4=c4: E.memset(gss.t[:, c4 * 2:c4 * 2 + 2], 0.0), reads=[gss.B], writes=[gss.B])
                    for hh in range(2):
                        hd = c4 * 2 + hh
                        P.op("act", lambda E, bk=bk, hh=hh, hd=hd: E.activation(out=gsq.t[:], in_=bk.t[:, hh * 128:(hh + 1) * 128], func=AF.Square,
                                                                               accum_out=gss.t[:, hd:hd + 1]),
                             reads=[bk.B], writes=[gsq.B, gss.B])
                    P.op("act", lambda E, c4=c4: E.activation(out=gss.t[:, c4 * 2:c4 * 2 + 2], in_=gss.t[:, c4 * 2:c4 * 2 + 2], func=AF.Sqrt,
                                                              bias=epsc.t[:, 0:1], scale=1.0 / 128.0), reads=[gss.B, epsc.B], writes=[gss.B])
                    P.op("dve", lambda E, c4=c4: E.reciprocal(out=gss.t[:, c4 * 2:c4 * 2 + 2], in_=gss.t[:, c4 * 2:c4 * 2 + 2]),
                         reads=[gss.B], writes=[gss.B])
                    for hh in range(2):
                        hd = c4 * 2 + hh
                        P.op("dve", lambda E, bk=bk, hh=hh, hd=hd, blk=blk: E.tensor_scalar(
                            out=ghat.t[:, blk, hh * 128:(hh + 1) * 128], in0=bk.t[:, hh * 128:(hh + 1) * 128],
                            scalar1=gss.t[:, hd:hd + 1], scalar2=None, op0=ALU.mult), reads=[bk.B, gss.B], writes=[ghat.B])
                w_release()
                flush()
                wu_, wub = w_get()
                wq_, wqb = w_get()
                C, Dk = bank["C"], bank["Dk"]

                def sgu_head(hh):
                    hd = hp * 2 + hh
                    bkA = bank["A0"]
                    for kc in range(KC):
                        mm(bkA, bkA.t[:], wu_[:, kc, hh * 128:(hh + 1) * 128], h.t[:, kc, :], kc == 0, kc == KC - 1, [wub, h.B])
                    P.op("act", lambda E: E.activation(out=uf.t[:], in_=bkA.t[:], func=AF.Copy), reads=[bkA.B], writes=[uf.B])
                    bkB = bank["B%d" % hh]
                    for blk in range(4):
                        mm(bkB, bkB.t[:, blk * 128:(blk + 1) * 128], ghat.t[:, blk, hh * 128:(hh + 1) * 128], wsT.t[:, l, hd, :],
                           True, True, [ghat.B, wsT.B])
                    for blk in range(4):
                        P.op("dve", lambda E, blk=blk: E.scalar_tensor_tensor(
                            out=r1.t[:, blk * 128:(blk + 1) * 128], in0=bkB.t[:, blk * 128:(blk + 1) * 128], scalar=sgn.t[:, l, hd:hd + 1],
                            in1=bsrep.t[:, 0, hd, :], op0=ALU.mult, op1=ALU.add), reads=[bkB.B, sgn.B, bsrep.B], writes=[r1.B])
                    P.op("dve", lambda E: E.tensor_tensor(out=r2.t[:], in0=r1.t[:], in1=uf.t[:], op=ALU.mult), reads=[r1.B, uf.B], writes=[r2.B])
                    P.op("act", lambda E: E.activation(out=o.t[:, 8 + hd, :], in_=r2.t[:], func=AF.Copy, scale=ong.t[:, l, 8 + hd:9 + hd]),
                         reads=[r2.B, ong.B], writes=[o.B])
                    accum_sumsq(r2.t[:], r2.B, sss, hd == 0, 1)

                def q_proj_norm(hh):
                    bkQ = bank["A1"]
                    for kc in range(KC):
                        mm(bkQ, bkQ.t[:], wq_[:, kc, hh * 128:(hh + 1) * 128], h.t[:, kc, :], kc == 0, kc == KC - 1, [wqb, h.B])
                    head_norm(bkQ, qg.t[:, l:l + 1], out_f32=hq2[hh])

                def q_finish(hh):
                    if rope:
                        rope_to(hq2[hh], qb2[hh].t[:], qb2[hh].B)
                    else:
                        P.op("act", lambda E: E.activation(out=qb2[hh].t[:], in_=hq2[hh].t[:], func=AF.Copy), reads=[hq2[hh].B], writes=[qb2[hh].B])

                def attention(hh, hook=None):
                    hd = hp * 2 + hh
                    kvh = hd // 4
                    qbh = qb2[hh]
                    for gi, (q0, q1, kblocks) in enumerate(groups):
                        nkb = len(kblocks)

                        def score(ji, q0=q0, q1=q1, kblocks=kblocks):
                            j = kblocks[ji]
                            bS = bank["B%d" % (ji % 2)]
                            mm(bS, bS.t[:, q0:q1], KTb.t[:, kvh, j * 128:(j + 1) * 128], qbh.t[:, q0:q1], True, True, [KTb.B, qbh.B])
                            P.op("act", lambda E: E.activation(out=PT.t[:, ji % 4, q0:q1], in_=bS.t[:, q0:q1], func=AF.Exp, scale=SCALE),
                                 reads=[bS.B], writes=[PT.b[ji % 4]])
                        score(0)
                        for ji in range(nkb):
                            if ji + 1 < nkb:
                                score(ji + 1)
                            j = kblocks[ji]
                            mm(C, C.t[:, q0:q1], Vb.t[:, j, kvh * 128:(kvh + 1) * 128], PT.t[:, ji % 4, q0:q1], ji == 0, ji == nkb - 1,
                               [Vb.B, PT.b[ji % 4]])
                            mm(Dk, Dk.t[:, q0:q1], onesb.t[:], PT.t[:, ji % 4, q0:q1], ji == 0, ji == nkb - 1, [onesb.B, PT.b[ji % 4]])
                            if hook is not None and gi == 0 and ji == min(3, nkb - 1):
                                hook()
                    P.op("dve", lambda E: E.reciprocal(out=r1.t[:], in_=Dk.t[:]), reads=[Dk.B], writes=[r1.B])
                    P.op("dve", lambda E: E.tensor_tensor(out=r2.t[:], in0=C.t[:], in1=r1.t[:], op=ALU.mult), reads=[C.B, r1.B], writes=[r2.B])
                    P.op("act", lambda E: E.activation(out=o.t[:, hd, :], in_=r2.t[:], func=AF.Copy, scale=ong.t[:, l, hd:hd + 1]),
                         reads=[r2.B, ong.B], writes=[o.B])
                    accum_sumsq(r2.t[:], r2.B, ssa, hd == 0, 2)

                sgu_head(0)
                q_proj_norm(0)
                sgu_head(1)
                flush()
                q_finish(0)
                q_proj_norm(1)
                flush()
                attention(0, hook=lambda: q_finish(1))
                attention(1)
                w_release()
            flush()
            rsa, rss = ssa, sss
            for (acc, dstr) in ((ssa, ssa), (sss, sss)):
                P.op("act", lambda E, acc=acc, dstr=dstr: E.activation(out=dstr.t[:], in_=acc.t[:], func=AF.Sqrt, bias=epsc.t[:, 0:1], scale=1.0 / 1024.0),
                     reads=[acc.B, epsc.B], writes=[dstr.B])
                P.op("dve", lambda E, dstr=dstr: E.reciprocal(out=dstr.t[:], in_=dstr.t[:]), reads=[dstr.B], writes=[dstr.B])
            g1 = mvec(l, cnd, 2)
            for pc in range(8):
                wo_, wob = w_get()
                for c2 in range(2):
                    oc = pc * 2 + c2
                    bkA, bkB = bank["A%d" % c2], bank["B%d" % c2]
                    for kc in range(8):
                        mm(bkA, bkA.t[:], wo_[:, kc, c2 * 128:(c2 + 1) * 128], o.t[:, kc, :], kc == 0, kc == 7, [wob, o.B])
                    for kc in range(8, 16):
                        mm(bkB, bkB.t[:], wo_[:, kc, c2 * 128:(c2 + 1) * 128], o.t[:, kc, :], kc == 8, kc == 15, [wob, o.B])
                    if c2 == 1:
                        w_release()
                    P.op("dve", lambda E, bkA=bkA: E.tensor_tensor(out=r1.t[:], in0=bkA.t[:], in1=rsa.t[:], op=ALU.mult), reads=[bkA.B, rsa.B], writes=[r1.B])
                    P.op("dve", lambda E, bkB=bkB: E.tensor_tensor(out=r2.t[:], in0=bkB.t[:], in1=rss.t[:], op=ALU.mult), reads=[bkB.B, rss.B], writes=[r2.B])
                    P.op("dve", lambda E: E.tensor_tensor(out=r1.t[:], in0=r1.t[:], in1=r2.t[:], op=ALU.add), reads=[r1.B, r2.B], writes=[r1.B])
                    P.op("dve", lambda E, oc=oc: E.scalar_tensor_tensor(out=x.t[:, oc, :], in0=r1.t[:], scalar=g1[:, oc:oc + 1], in1=x.t[:, oc, :],
                                                                       op0=ALU.mult, op1=ALU.add), reads=[r1.B, mod.B, x.B], writes=[x.B])

        sg = sb("sg", (128, 2, TT), F32, n=2)
        actb = sb("actb", (128, 2, 4, TT), BF16, n=2)
        cwrep = sb("cwrep", (128, TT), F32)
        cw4 = sb("cw4", (128, 4, NE), F32)
        lg = sb("lg", (128, NE), F32)
        lg8 = sb("lg8", (128, 8), F32)
        cw = sb("cw", (128, NE), F32)
        cwb = sb("cwb", (128, 128), F32)
        tcol = sb("tcol", (128, 4), F32)
        wrp = sb("wrp", (128, KC, NE), F32)

        def ffn_panel_list(l):
            out = []
            if l == 0:
                for p in range(DFF // 256):
                    out.append((fg_d[0][:, p * 256:(p + 1) * 256], fu_d[0][:, p * 256:(p + 1) * 256], fd_d[0][p * 256:(p + 1) * 256, :], None))
            else:
                for e in range(NE):
                    for p in range(DFE // 256):
                        out.append((mg_d[0, e][:, p * 256:(p + 1) * 256], mu_d[0, e][:, p * 256:(p + 1) * 256],
                                    md_d[0, e][p * 256:(p + 1) * 256, :], e))
            assert len(out) % 2 == 0
            return out

        def plan_ffn(l):
            pl = ffn_panel_list(l)
            items = []
            for pp in range(len(pl) // 2):
                for half in range(2):
                    g_, u_, d_, e = pl[pp * 2 + half]
                    items.append((g_, ("in", 256)))
                    items.append((u_, ("in", 256)))
                for half in range(2):
                    items.append((pl[pp * 2 + half][2], ("rows", 2)))
            return items

        def emit_ffn_panels(l, x, h2, g2):
            pl = ffn_panel_list(l)
            cur_e = None
            for pp in range(len(pl) // 2):
                pi = pp % 2
                for half in range(2):
                    e = pl[pp * 2 + half][3]
                    if e is not None and e != cur_e:
                        emit_cwrep(e)
                        cur_e = e
                    wg_, wgb = w_get()
                    for c2 in range(2):
                        bkG = bank["A%d" % c2]
                        for kc in range(KC):
                            mm(bkG, bkG.t[:], wg_[:, kc, c2 * 128:(c2 + 1) * 128], h2.t[:, kc, :], kc == 0, kc == KC - 1, [wgb, h2.B])
                    w_release()
                    wu_, wub = w_get()
                    for c2 in range(2):
                        bkU = bank["B%d" % c2]
                        for kc in range(KC):
                            mm(bkU, bkU.t[:], wu_[:, kc, c2 * 128:(c2 + 1) * 128], h2.t[:, kc, :], kc == 0, kc == KC - 1, [wub, h2.B])
                    w_release()
                    for c2 in range(2):
                        bkG, bkU = bank["A%d" % c2], bank["B%d" % c2]
                        P.op("act", lambda E, bkG=bkG, c2=c2: E.activation(out=sg.t[:, c2, :], in_=bkG.t[:], func=AF.Silu), reads=[bkG.B], writes=[sg.b[c2]])
                        if e is not None:
                            P.op("dve", lambda E, c2=c2: E.tensor_tensor(out=sg.t[:, c2, :], in0=sg.t[:, c2, :], in1=cwrep.t[:], op=ALU.mult),
                                 reads=[sg.b[c2], cwrep.B], writes=[sg.b[c2]])
                        P.op("dve", lambda E, bkU=bkU, c2=c2, pi=pi, half=half: E.tensor_tensor(out=actb.t[:, pi, half * 2 + c2, :], in0=bkU.t[:], in1=sg.t[:, c2, :], op=ALU.mult),
                             reads=[bkU.B, sg.b[c2]], writes=[actb.b[pi]])
                wd0, wdb0 = w_get()
                wd1, wdb1 = w_get()
                for oc in range(KC):
                    bk = bank["C"] if oc % 2 == 0 else bank["Dk"]
                    for c4 in range(4):
                        wd_, wdb = (wd0, wdb0) if c4 < 2 else (wd1, wdb1)
                        mm(bk, bk.t[:], wd_[:, c4 % 2, oc * 128:(oc + 1) * 128], actb.t[:, pi, c4, :], c4 == 0, c4 == 3, [wdb, actb.b[pi]])
                    P.op("dve", lambda E, bk=bk, oc=oc: E.scalar_tensor_tensor(out=x.t[:, oc, :], in0=bk.t[:], scalar=g2[:, oc:oc + 1], in1=x.t[:, oc, :],
                                                                              op0=ALU.mult, op1=ALU.add), reads=[bk.B, mod.B, x.B], writes=[x.B])
                w_release()

        def emit_router(l, cnd, x):
            A2v = A2.t[:, l, cnd, :]
            sh2 = mvec(l, cnd, 3)
            for kc in range(KC):
                P.op("dve", lambda E, kc=kc: E.tensor_scalar(out=wrp.t[:, kc, :], in0=wr.t[:, kc, :], scalar1=A2v[:, kc:kc + 1], scalar2=None, op0=ALU.mult),
                     reads=[wr.B, A2.B], writes=[wrp.B])
            H, G = bank["H"], bank["G"]
            for kc in range(KC):
                P.op("dve", lambda E, kc=kc: E.tensor_scalar(out=gsq.t[:], in0=ident.t[:], scalar1=0.0, scalar2=sh2[:, kc:kc + 1], op0=ALU.mult, op1=ALU.add),
                     reads=[ident.B, mod.B, gsq.B], writes=[gsq.B])
                mm(G, G.t[:, 0:NE], gsq.t[:], wr.t[:, kc, :], kc == 0, kc == KC - 1, [gsq.B, wr.B])
            P.op("dve", lambda E: E.tensor_tensor(out=lg8.t[:], in0=G.t[:, 0:NE], in1=brrep.t[:], op=ALU.add), reads=[G.B, brrep.B], writes=[lg8.B])
            for blk in range(4):
                for kc in range(KC):
                    mm(H, H.t[:, 0:NE], x.t[:, kc, blk * 128:(blk + 1) * 128], wrp.t[:, kc, :], kc == 0, kc == KC - 1, [x.B, wrp.B])
                mm(G, G.t[:, 0:1], rstd.t[:, blk * 128:(blk + 1) * 128], ident.t[:, 0:1], True, True, [rstd.B, ident.B])
                P.op("dve", lambda E: E.tensor_copy(tcol.t[:, 0:1], G.t[:, 0:1]), reads=[G.B], writes=[tcol.B])
                P.op("dve", lambda E: E.scalar_tensor_tensor(out=lg.t[:], in0=H.t[:, 0:NE], scalar=tcol.t[:, 0:1], in1=lg8.t[:], op0=ALU.mult, op1=ALU.add),
                     reads=[H.B, tcol.B, lg8.B], writes=[lg.B])
                P.op("dve", lambda E: E.tensor_reduce(out=tcol.t[:, 1:2], in_=lg.t[:], axis=mybir.AxisListType.X, op=ALU.max), reads=[lg.B, tcol.B], writes=[tcol.B])
                P.op("dve", lambda E: E.tensor_scalar(out=cw.t[:], in0=lg.t[:], scalar1=tcol.t[:, 1:2], scalar2=-1e30, op0=ALU.is_ge, op1=ALU.mult),
                     reads=[lg.B, tcol.B], writes=[cw.B])
                P.op("dve", lambda E: E.tensor_tensor(out=cw.t[:], in0=cw.t[:], in1=lg.t[:], op=ALU.add), reads=[cw.B, lg.B], writes=[cw.B])
                P.op("dve", lambda E: E.tensor_reduce(out=tcol.t[:, 2:3], in_=cw.t[:], axis=mybir.AxisListType.X, op=ALU.max), reads=[cw.B, tcol.B], writes=[tcol.B])
                P.op("dve", lambda E: E.tensor_scalar(out=cw.t[:], in0=lg.t[:], scalar1=tcol.t[:, 2:3], scalar2=None, op0=ALU.is_ge), reads=[lg.B, tcol.B], writes=[cw.B])
                P.op("dve", lambda E: E.tensor_scalar(out=tcol.t[:, 3:4], in0=tcol.t[:, 1:2], scalar1=-1.0, scalar2=None, op0=ALU.mult), reads=[tcol.B], writes=[tcol.B])
                P.op("act", lambda E: E.activation(out=lg.t[:], in_=lg.t[:], func=AF.Exp, bias=tcol.t[:, 3:4], scale=1.0), reads=[lg.B, tcol.B], writes=[lg.B])
                P.op("dve", lambda E: E.tensor_tensor(out=cw.t[:], in0=cw.t[:], in1=lg.t[:], op=ALU.mult), reads=[cw.B, lg.B], writes=[cw.B])
                P.op("dve", lambda E: E.tensor_reduce(out=tcol.t[:, 0:1], in_=cw.t[:], axis=mybir.AxisListType.X, op=ALU.add), reads=[cw.B, tcol.B], writes=[tcol.B])
                P.op("dve", lambda E: E.reciprocal(out=tcol.t[:, 0:1], in_=tcol.t[:, 0:1]), reads=[tcol.B], writes=[tcol.B])
                P.op("dve", lambda E, blk=blk: E.tensor_scalar(out=cw4.t[:, blk, :], in0=cw.t[:], scalar1=tcol.t[:, 0:1], scalar2=None, op0=ALU.mult),
                     reads=[cw.B, tcol.B], writes=[cw4.B])

        def emit_cwrep(e):
            bk = bank["H"]
            for blk in range(4):
                P.op("dve", lambda E, blk=blk: E.tensor_scalar(out=cwb.t[:], in0=ident.t[:], scalar1=0.0, scalar2=cw4.t[:, blk, e:e + 1], op0=ALU.mult, op1=ALU.add),
                     reads=[ident.B, cw4.B, cwb.B], writes=[cwb.B])
                mm(bk, bk.t[:, blk * 128:(blk + 1) * 128], cwb.t[:], ident.t[:], True, True, [cwb.B, ident.B])
            P.op("act", lambda E: E.activation(out=cwrep.t[:], in_=bk.t[:], func=AF.Copy), reads=[bk.B], writes=[cwrep.B])

        def emit_ffn(l, cnd, x, h2):
            g2 = mvec(l, cnd, 5)
            norm_mod(x, h2, A2.t[:, l, cnd, :], mvec(l, cnd, 3))
            if l == 1:
                emit_router(l, cnd, x)
            emit_ffn_panels(l, x, h2, g2)

        xA = sb("xA", (128, KC, TT), F32)
        xpark = nc.dram_tensor("xpark", [128, KC, TT], F32, kind="Internal").ap()
        bpark = Buf("xpark")
        hb = sb("hb", (128, KC, TT), BF16)
        ob = sb("ob", (128, KC, TT), BF16)
        KT0 = sb("KT0", (128, 2, 2304), BF16)
        V0 = sb("V0", (128, 18, 256), BF16)
        KT1 = sb("KT1", (128, 2, 2304), BF16)
        V1 = sb("V1", (128, 18, 256), BF16)
        cstage = T(xstage.t[:, 0:512].rearrange("p (r f) -> p r f", r=2), 1)
        cstage.b = xstage.b
        cstage.B = xstage.B

        plan = []
        plan += plan_mod()
        for l in range(NL):
            plan += plan_kv(l) + plan_mixer(l) + plan_ffn(l)
        for t in range(4):
            plan += plan_kv(0)
        for t in (2, 3, 0, 1):
            plan += plan_mixer(0) + plan_ffn(0) + plan_kv(1)
            if t == 1:
                plan += plan_mixer(1) + plan_ffn(1)
        plan += plan_mixer(1) + plan_ffn(1)
        w_plan(plan)

        def load_rope(t):
            P.dma("sp", ropec.t[:], ropec_d[t], reads=[bIN], writes=[ropec.B])
            P.dma("sp", ropes.t[:], ropes_d[t], reads=[bIN], writes=[ropes.B])

        def load_cache(l, KTb, Vb):
            P.dma("sp", cstage.t, ck_d[l].rearrange("(r p) f -> p r f", p=128), reads=[bIN], writes=[cstage.B])
            H = bank["H"]
            for kvh in range(2):
                for r in range(2):
                    P.op("pe", lambda E, kvh=kvh, r=r: E.transpose(H.t[:, (kvh * 2 + r) * 128:(kvh * 2 + r + 1) * 128],
                                                                   cstage.t[:, r, kvh * 128:(kvh + 1) * 128], ident.t[:]),
                         reads=[cstage.B, ident.B], writes=[H.B])
            P.op("act", lambda E: E.activation(out=KTb.t[:, :, 0:256], in_=H.t[:].rearrange("p (k t) -> p k t", k=2), func=AF.Copy),
                 reads=[H.B], writes=[KTb.B])
            P.dma("pool", Vb.t[:, 0:2, :], cv_d[l].rearrange("(r p) f -> p r f", p=128), reads=[bIN], writes=[Vb.B])

        emit_mod()

        load_x(xp_d, xA)
        pgroups = [(0, 256, [0, 1]), (256, 512, [2, 3])]
        for l in range(NL):
            norm_mod(xA, hb, A1.t[:, l, 0, :], mvec(l, 0, 0))
            emit_kv(l, hb, KT0, V0, 0, 0, rope=False, cache_out=True)
            emit_mixer(l, 0, xA, hb, ob, KT0, V0, pgroups, rope=False)
            if debug and l == 0:
                dbg1 = nc.dram_tensor("dbg1", [128, KC, TT], F32, kind="ExternalOutput").ap()
                dbg2 = nc.dram_tensor("dbg2", [128, KC, TT], F32, kind="ExternalOutput").ap()
                dbg3 = nc.dram_tensor("dbg3", [128, KC, TT], BF16, kind="ExternalOutput").ap()
                dbg4 = nc.dram_tensor("dbg4", [128, 4, TT], F32, kind="ExternalOutput").ap()
                P.dma("sp", dbg1, xA.t[:], reads=[xA.B], writes=[Buf()])
                P.dma("sp", dbg3, ob.t[:], reads=[ob.B], writes=[Buf()])
                for i_, t_ in enumerate((ssa, sss, ssa, sss)):
                    P.dma("sp", dbg4[:, i_, :], t_.t[:], reads=[t_.B], writes=[Buf()])
            emit_ffn(l, 0, xA, hb)
            if debug and l == 0:
                P.dma("sp", dbg2, xA.t[:], reads=[xA.B], writes=[Buf()])
                P.finish()
                return nc
        store_x(xA, yp_d)

        sgroups = [(0, 512, list(range(18)))]
        load_cache(0, KT0, V0)
        load_cache(1, KT1, V1)
        for t in range(4):
            load_x(xs_d[t], xA)
            load_rope(t)
            norm_mod(xA, hb, A1.t[:, 0, 1, :], mvec(0, 1, 0))
            emit_kv(0, hb, KT0, V0, 256 + t * 512, 2 + t * 4, rope=True)
        for t in (2, 3, 0, 1):
            xt = xA
            load_x(xs_d[t], xt)
            load_rope(t)
            norm_mod(xt, hb, A1.t[:, 0, 1, :], mvec(0, 1, 0))
            emit_mixer(0, 1, xt, hb, ob, KT0, V0, sgroups, rope=True)
            emit_ffn(0, 1, xt, hb)
            norm_mod(xt, hb, A1.t[:, 1, 1, :], mvec(1, 1, 0))
            emit_kv(1, hb, KT1, V1, 256 + t * 512, 2 + t * 4, rope=True)
            if t == 1:
                emit_mixer(1, 1, xt, hb, ob, KT1, V1, sgroups, rope=True)
                emit_ffn(1, 1, xt, hb)
                store_x(xt, ys_d[1])
            if t == 0:
                P.dma("sp", xpark, xA.t[:], reads=[xA.B], writes=[bpark])
        load_rope(0)
        P.dma("sp", xA.t[:], xpark, reads=[bpark], writes=[xA.B])
        norm_mod(xA, hb, A1.t[:, 1, 1, :], mvec(1, 1, 0))
        emit_mixer(1, 1, xA, hb, ob, KT1, V1, sgroups, rope=True)
        emit_ffn(1, 1, xA, hb)
        store_x(xA, ys_d[0])

        assert wstate["taken"] == len(wstate["plan"]), (wstate["taken"], len(wstate["plan"]))
        P.finish()
        build_program.stats = (P.n_ops, P.n_wait, nc.sbuf_bytes_remaining)
    return nc


def _rope_tables():
    L_ = 2048
    rows = (np.arange(L_) // 64).astype(np.float32)
    cols = (np.arange(L_) % 64).astype(np.float32)
    inv = (10000.0 ** (-np.arange(0, 64, 2, dtype=np.float32) / 64.0)).astype(np.float32)
    ar = rows[:, None] * inv[None, :]
    ac = cols[:, None] * inv[None, :]
    ang = np.concatenate([ar, ar, ac, ac], axis=1)
    return np.cos(ang).astype(np.float32), np.sin(ang).astype(np.float32)


def _rt_matrix():
    rt = np.zeros((128, 128), np.float32)
    for base in (0, 64):
        for i in range(32):
            m = base + i
            rt[m + 32, m] = -1.0
            rt[m, m + 32] = 1.0
    return rt


def _fm(v):
    v = np.asarray(v, np.float32)
    lead = v.shape[:-1]
    return np.ascontiguousarray(np.moveaxis(v.reshape(lead + (KC, 128)), -1, 0))


_NC_CACHE = {}


def kernel(x_prompt, x_sample, cache_k, cache_v, c, c_ctx, w_ada, b_ada, norm1_g, norm2_g,
           w_in, q_norm_g, k_norm_g, sgu_norm_g, w_spatial, b_spatial, out_norm_g, w_out,
           ffn_w_gate, ffn_w_up, ffn_w_down, w_router, b_router, moe_w_gate, moe_w_up, moe_w_down):
    f32 = lambda a: np.ascontiguousarray(np.asarray(a, dtype=np.float32))
    x_prompt, x_sample, cache_k, cache_v = f32(x_prompt), f32(x_sample), f32(cache_k), f32(cache_v)
    c, c_ctx = f32(c), f32(c_ctx)
    if "nc" not in _NC_CACHE:
        _NC_CACHE["nc"] = build_program()
    nc = _NC_CACHE["nc"]
    in_maps = _prep(x_prompt, x_sample, cache_k, cache_v, c, c_ctx, w_ada, b_ada, norm1_g, norm2_g,
                    w_in, q_norm_g, k_norm_g, sgu_norm_g, w_spatial, b_spatial, out_norm_g, w_out,
                    ffn_w_gate, ffn_w_up, ffn_w_down, w_router, b_router, moe_w_gate, moe_w_up, moe_w_down)
    res = run_bass_kernel_spmd(nc, in_maps, core_ids=list(range(NCORES)))
    return _assemble(res.results)


def _prep(x_prompt, x_sample, cache_k, cache_v, c, c_ctx, w_ada, b_ada, norm1_g, norm2_g,
          w_in, q_norm_g, k_norm_g, sgu_norm_g, w_spatial, b_spatial, out_norm_g, w_out,
          ffn_w_gate, ffn_w_up, ffn_w_down, w_router, b_router, moe_w_gate, moe_w_up, moe_w_down):
    f32 = lambda a: np.ascontiguousarray(np.asarray(a, dtype=np.float32))

    cos, sin = _rope_tables()
    shared = {
        "n1g": _fm(norm1_g), "n2g": _fm(norm2_g),
        "bada": np.ascontiguousarray(np.moveaxis(f32(b_ada).reshape(NL, 96, 128), -1, 0)),
        "qg": np.ascontiguousarray(f32(q_norm_g).T), "kg": np.ascontiguousarray(f32(k_norm_g).T),
        "sgn": np.ascontiguousarray(np.moveaxis(f32(sgu_norm_g), -1, 0)),
        "bsrep": np.ascontiguousarray(np.broadcast_to(f32(b_spatial)[None], (128, NL, 8, 128))),
        "wsT": np.ascontiguousarray(np.transpose(f32(w_spatial), (3, 0, 1, 2))),
        "ong": _fm(out_norm_g),
        "wr": np.ascontiguousarray(np.transpose(f32(w_router)[0].reshape(KC, 128, NE), (1, 0, 2))),
        "brrep": np.ascontiguousarray(np.broadcast_to(f32(b_router)[0][None], (128, NE))),
        "ident": np.eye(128, dtype=np.float32), "rt": _rt_matrix(),
        "w_ada": f32(w_ada), "w_in": f32(w_in), "w_out": f32(w_out),
        "ffn_w_gate": f32(ffn_w_gate), "ffn_w_up": f32(ffn_w_up), "ffn_w_down": f32(ffn_w_down),
        "moe_w_gate": f32(moe_w_gate), "moe_w_up": f32(moe_w_up), "moe_w_down": f32(moe_w_down),
    }
    in_maps = []
    for core in range(NCORES):
        b, half = core // 2, core % 2
        own = x_sample[b, half * 1024:(half + 1) * 1024].reshape(2, TT, D)
        oth = x_sample[b, (1 - half) * 1024:(2 - half) * 1024].reshape(2, TT, D)
        pos = np.concatenate([np.arange(half * 1024, (half + 1) * 1024), np.arange((1 - half) * 1024, (2 - half) * 1024)])
        m = dict(shared)
        m["xs"] = np.ascontiguousarray(np.concatenate([own, oth], axis=0))
        m["xp"] = np.ascontiguousarray(x_prompt[2 * core:2 * core + 2].reshape(TT, D))
        m["ropec"] = np.ascontiguousarray(cos[pos].reshape(4, TT, 128).transpose(0, 2, 1))
        m["ropes"] = np.ascontiguousarray(sin[pos].reshape(4, TT, 128).transpose(0, 2, 1))
        m["ck"] = np.ascontiguousarray(cache_k[b].reshape(NL, 256, 256))
        m["cv"] = np.ascontiguousarray(cache_v[b].reshape(NL, 256, 256))
        cv2 = np.stack([c_ctx, c[b]], axis=-1)
        m["cvec"] = np.ascontiguousarray(cv2.reshape(KC, 128, 2).transpose(1, 0, 2))
        in_maps.append(m)
    return in_maps


def _assemble(R):
    y_prompt = np.empty((16, 256, D), np.float32)
    y_sample = np.empty((4, 2048, D), np.float32)
    nk = np.empty((16, NL, 256, 2, 128), np.float32)
    nv = np.empty((16, NL, 256, 2, 128), np.float32)
    for core in range(NCORES):
        b, half = core // 2, core % 2
        r = R[core]
        y_prompt[2 * core:2 * core + 2] = np.asarray(r["yp"]).reshape(2, 256, D)
        y_sample[b, half * 1024:(half + 1) * 1024] = np.asarray(r["ys"]).reshape(1024, D)
        nk[2 * core:2 * core + 2] = np.asarray(r["nk"]).reshape(2, NL, 256, 2, 128)
        nv[2 * core:2 * core + 2] = np.asarray(r["nv"]).reshape(2, NL, 256, 2, 128)
    return (y_prompt, y_sample, nk, nv)
```

```python
import numpy as np
from contextlib import ExitStack
import concourse.bass as bass
import concourse.mybir as mybir
from concourse.bass_utils import run_bass_kernel_spmd

F32 = mybir.dt.float32
BF16 = mybir.dt.bfloat16
ALU = mybir.AluOpType
AF = mybir.ActivationFunctionType

ENGS = ("pe", "act", "dve", "pool", "sp")

D = 2048
KC = 16
TT = 512
NL = 2
INW = 3584
DFF = 5632
NE = 8
DFE = 2816
EPS = 1e-6
NCORES = 8


class Buf:
    __slots__ = ("name", "w", "r", "excl")

    def __init__(self, name="", excl=False):
        self.name = name
        self.excl = excl
        self.w = None
        self.r = []


class Prog:
    NDMA = 6

    def __init__(self, nc, stack):
        self.nc = nc
        self.stack = stack
        self.ops = {e: [] for e in ENGS}
        self.sem = {}
        self.cnt = {}
        self.seen = {e: {} for e in ENGS}
        for e in ENGS:
            self._mk("c_" + e)
        self.dma_rr = {}
        for q in ("sp", "pool"):
            for i in range(self.NDMA):
                self._mk("d_%s_%d" % (q, i))
            self.dma_rr[q] = 0
        self.n_wait = 0
        self.n_ops = 0
        self.dry = False

    def _mk(self, key):
        self.sem[key] = self.stack.enter_context(self.nc.semaphore(key))
        self.cnt[key] = 0

    def _deps(self, reads, writes):
        d = {}

        def add(ev):
            if ev is None:
                return
            k, v = ev
            if d.get(k, 0) < v:
                d[k] = v
        for b in reads:
            add(b.w)
        for b in writes:
            add(b.w)
            for ev in b.r:
                add(ev)
        return d

    def _emit_waits(self, eng, deps):
        seen = self.seen[eng]
        for k, v in deps.items():
            if eng == "pe" and k == "c_pe":
                continue
            if seen.get(k, 0) >= v:
                continue
            seen[k] = v
            sem = self.sem[k]
            self.ops[eng].append(lambda E, sem=sem, v=v: E.wait_ge(sem, v))
            self.n_wait += 1

    def _record(self, ev, reads, writes):
        for b in reads:
            b.r.append(ev)
            if len(b.r) > 48:
                m = {}
                for k, v in b.r:
                    if m.get(k, 0) < v:
                        m[k] = v
                b.r = list(m.items())
        for b in writes:
            b.w = ev
            b.r = []

    def op(self, eng, fn, reads=(), writes=()):
        if self.dry:
            return
        if any(b.excl for b in reads):
            writes = list(writes) + [b for b in reads if b.excl]
            reads = [b for b in reads if not b.excl]
        deps = self._deps(reads, writes)
        self._emit_waits(eng, deps)
        key = "c_" + eng
        self.cnt[key] += 1
        v = self.cnt[key]
        sem = self.sem[key]
        self.ops[eng].append(lambda E, fn=fn, sem=sem: fn(E).then_inc(sem, 1))
        self._record((key, v), reads, writes)
        self.n_ops += 1

    def dma(self, q, out, in_, reads=(), writes=()):
        if self.dry:
            return
        deps = self._deps(reads, writes)
        i = self.dma_rr[q]
        self.dma_rr[q] = (i + 1) % self.NDMA
        key = "d_%s_%d" % (q, i)
        if self.cnt[key] > 0 and deps.get(key, 0) < self.cnt[key]:
            deps[key] = self.cnt[key]
        self._emit_waits(q, deps)
        self.cnt[key] += 16
        v = self.cnt[key]
        sem = self.sem[key]
        self.ops[q].append(lambda E, out=out, in_=in_, sem=sem: E.dma_start(out=out, in_=in_).then_inc(sem, 16))
        self._record((key, v), reads, writes)
        self.n_ops += 1

    def finish(self):
        deps = {k: v for k, v in self.cnt.items() if k.startswith("d_") and v > 0}
        self._emit_waits("sp", deps)
        ops = self.ops
        with self.nc.Block() as block:
            @block.tensor
            def _(E):
                for f in ops["pe"]:
                    f(E)

            @block.scalar
            def _(E):
                for f in ops["act"]:
                    f(E)

            @block.vector
            def _(E):
                for f in ops["dve"]:
                    f(E)

            @block.gpsimd
            def _(E):
                for f in ops["pool"]:
                    f(E)

            @block.sync
            def _(E):
                for f in ops["sp"]:
                    f(E)


class T:
    def __init__(self, t, n=1, excl=False, name=""):
        self.t = t
        self.b = [Buf(name + str(i), excl) for i in range(n)]
        self.B = self.b[0]


def build_program(debug=0):
    nc = bass.Bass("TRN2", target_bir_lowering=False)
    dt_in = lambda name, shape: nc.dram_tensor(name, list(shape), F32, kind="ExternalInput").ap()
    dt_out = lambda name, shape: nc.dram_tensor(name, list(shape), F32, kind="ExternalOutput").ap()
    xs_d = dt_in("xs", (4, TT, D))
    xp_d = dt_in("xp", (TT, D))
    ropec_d = dt_in("ropec", (4, 128, TT))
    ropes_d = dt_in("ropes", (4, 128, TT))
    ck_d = dt_in("ck", (NL, 256, 256))
    cv_d = dt_in("cv", (NL, 256, 256))
    cvec_d = dt_in("cvec", (128, KC, 2))
    n1g_d = dt_in("n1g", (128, NL, KC))
    n2g_d = dt_in("n2g", (128, NL, KC))
    bada_d = dt_in("bada", (128, NL, 96))
    qg_d = dt_in("qg", (128, NL))
    kg_d = dt_in("kg", (128, NL))
    sgn_d = dt_in("sgn", (128, NL, 8))
    bsrep_d = dt_in("bsrep", (128, NL, 8, 128))
    wsT_d = dt_in("wsT", (128, NL, 8, 128))
    ong_d = dt_in("ong", (128, NL, KC))
    wr_d = dt_in("wr", (128, KC, NE))
    brrep_d = dt_in("brrep", (128, NE))
    ident_d = dt_in("ident", (128, 128))
    rt_d = dt_in("rt", (128, 128))
    wada_d = dt_in("w_ada", (NL, D, 6 * D))
    win_d = dt_in("w_in", (NL, D, INW))
    wout_d = dt_in("w_out", (NL, D, D))
    fg_d = dt_in("ffn_w_gate", (1, D, DFF))
    fu_d = dt_in("ffn_w_up", (1, D, DFF))
    fd_d = dt_in("ffn_w_down", (1, DFF, D))
    mg_d = dt_in("moe_w_gate", (1, NE, D, DFE))
    mu_d = dt_in("moe_w_up", (1, NE, D, DFE))
    md_d = dt_in("moe_w_down", (1, NE, DFE, D))
    yp_d = dt_out("yp", (TT, D))
    ys_d = dt_out("ys", (2, TT, D))
    nk_d = dt_out("nk", (2, NL, 256, 256))
    nv_d = dt_out("nv", (2, NL, 256, 256))
    bIN = Buf("dram_in")

    with ExitStack() as st:
        P = Prog(nc, st)

        def sb(name, shape, dt, n=1, stack=st):
            return T(stack.enter_context(nc.sbuf_tensor("s_" + name, list(shape), dt)), n, name=name)

        bank = {}
        for nm in ("A0", "A1", "B0", "B1", "C", "Dk", "G", "H"):
            bank[nm] = T(st.enter_context(nc.psum_tensor("ps" + nm, [128, 512], F32)), 1, excl=True, name="ps" + nm)

        ident = sb("ident", (128, 128), F32)
        identb = sb("identb", (128, 128), BF16)
        rt = sb("rt", (128, 128), F32)
        onesb = sb("onesb", (128, 128), BF16)
        epsc = sb("epsc", (128, 1), F32)
        cvec = sb("cvec", (128, KC, 2), F32)
        scb = sb("scb", (128, KC, 2), BF16)
        n1g = sb("n1g", (128, NL, KC), F32)
        n2g = sb("n2g", (128, NL, KC), F32)
        bada = sb("bada", (128, NL, 96), F32)
        qg = sb("qg", (128, NL), F32)
        kg = sb("kg", (128, NL), F32)
        sgn = sb("sgn", (128, NL, 8), F32)
        bsrep = sb("bsrep", (128, 1, 8, 128), F32)
        wsT = sb("wsT", (128, NL, 8, 128), BF16)
        ong = sb("ong", (128, NL, KC), F32)
        wr = sb("wr", (128, KC, NE), F32)
        brrep = sb("brrep", (128, NE), F32)
        mod = sb("mod", (128, NL, 2, 96), F32)
        A1 = sb("A1", (128, NL, 2, KC), F32)
        A2 = sb("A2", (128, NL, 2, KC), F32)

        for (t_, d_) in ((ident, ident_d), (rt, rt_d), (cvec, cvec_d), (n1g, n1g_d), (n2g, n2g_d), (bada, bada_d),
                         (qg, qg_d), (kg, kg_d), (sgn, sgn_d), (ong, ong_d),
                         (wr, wr_d), (brrep, brrep_d)):
            P.dma("sp", t_.t[:], d_, reads=[bIN], writes=[t_.B])
        P.op("dve", lambda E: E.memset(onesb.t[:], 1.0), writes=[onesb.B])
        P.op("dve", lambda E: E.memset(epsc.t[:], EPS), writes=[epsc.B])
        P.op("dve", lambda E: E.tensor_copy(identb.t[:], ident.t[:]), reads=[ident.B], writes=[identb.B])
        P.dma("pool", wsT.t[:], wsT_d, reads=[bIN], writes=[wsT.B])
        P.op("act", lambda E: E.activation(out=scb.t[:], in_=cvec.t[:], func=AF.Silu), reads=[cvec.B], writes=[scb.B])

        NSLOT = 4
        wslots = [sb("wslot%d" % i, (128, 4096), BF16) for i in range(NSLOT)]
        wstate = {"plan": [], "issued": 0, "taken": 0, "released": 0}

        def w_view(i, shape):
            s_ = wslots[i % NSLOT]
            if shape[0] == "in":
                return s_.t[:, 0:KC * shape[1]].rearrange("p (k n) -> p k n", k=KC), s_.B
            return s_.t[:, 0:shape[1] * D].rearrange("p (k n) -> p k n", k=shape[1]), s_.B

        def w_issue_upto(n):
            while wstate["issued"] < min(n, len(wstate["plan"])):
                i = wstate["issued"]
                src, shape = wstate["plan"][i]
                dst, db = w_view(i, shape)
                P.dma("pool", dst, src.rearrange("(k p) n -> p k n", p=128), reads=[bIN], writes=[db])
                wstate["issued"] += 1

        def w_get(src, shape):
            i = wstate["taken"]
            wstate["taken"] += 1
            if P.dry:
                wstate["plan"].append((src, shape))
                return w_view(i, shape)
            assert i < len(wstate["plan"]) and wstate["plan"][i][1] == shape, "weight plan mismatch"
            w_issue_upto(max(wstate["released"] + NSLOT, i + 1))
            assert wstate["issued"] <= wstate["released"] + NSLOT and i - wstate["released"] < NSLOT
            return w_view(i, shape)

        def w_release():
            if P.dry:
                return
            wstate["released"] = wstate["taken"]
            w_issue_upto(wstate["released"] + NSLOT)

        def mm(out_bank, out_ap, lhsT, rhs, start, stop, reads):
            P.op("pe", lambda E: E.matmul(out_ap, lhsT=lhsT, rhs=rhs, start=start, stop=stop), reads=reads, writes=[out_bank.B])

        rr = {"act_dve": 0}

        modst = {"i": 0}

        def mod_step(n):
            for _ in range(n):
                i = modst["i"]
                if i >= NL * 48:
                    return
                modst["i"] = i + 1
                l, pc = divmod(i, 48)
                w, wb = w_get(wada_d[l, :, pc * 256:(pc + 1) * 256], ("in", 256))
                bk = bank["G"] if pc % 2 == 0 else bank["H"]
                for c2 in range(2):
                    for kc in range(KC):
                        mm(bk, bk.t[:, c2 * 2:c2 * 2 + 2], w[:, kc, c2 * 128:(c2 + 1) * 128], scb.t[:, kc, :],
                           kc == 0, kc == KC - 1, [wb, scb.B])
                w_release()
                for c2 in range(2):
                    j = pc * 2 + c2
                    P.op("dve", lambda E, l=l, j=j, c2=c2, bk=bk: E.tensor_scalar(
                        out=mod.t[:, l, :, j], in0=bk.t[:, c2 * 2:c2 * 2 + 2], scalar1=bada.t[:, l, j:j + 1], scalar2=None,
                        op0=ALU.add), reads=[bk.B, bada.B], writes=[mod.B])
                for (A, g_, off, last_pc) in ((A1, n1g, 16, 15), (A2, n2g, 64, 39)):
                    if pc == last_pc:
                        for cnd in range(2):
                            P.op("dve", lambda E, A=A, g_=g_, off=off, l=l, cnd=cnd: E.scalar_tensor_tensor(
                                out=A.t[:, l, cnd, :], in0=mod.t[:, l, cnd, off:off + 16], scalar=1.0, in1=g_.t[:, l, :],
                                op0=ALU.add, op1=ALU.mult), reads=[mod.B, g_.B], writes=[A.B])

        def need_mod(l, which):
            assert modst["i"] > l * 48 + which * 8 + 7, ("modulation not produced yet", l, which, modst["i"])

        bgc = {"hp": 0, "wo": 0, "ffn": 0}

        def mvec(l, cnd, which):
            need_mod(l, which)
            return mod.t[:, l, cnd, which * 16:(which + 1) * 16]

        xstage = sb("xstage", (128, 1024), F32, n=1)
        kvstage = sb("kvstage", (128, 4, 256), F32)
        xstg = [(xstage.t[:], xstage.B), (kvstage.t[:].rearrange("p b d -> p (b d)"), kvstage.B)]

        def load_x(src, x):
            for blk in range(4):
                for g4 in range(4):
                    st_ap, st_b = xstg[(blk * 2 + g4 // 2) % 2]
                    if g4 % 2 == 0:
                        P.dma("sp", st_ap, src[blk * 128:(blk + 1) * 128, (g4 // 2) * 1024:(g4 // 2 + 1) * 1024], reads=[bIN], writes=[st_b])
                    bk = bank["H"] if g4 % 2 == 0 else bank["G"]
                    for j in range(4):
                        kc = g4 * 4 + j
                        kl = kc % 8
                        P.op("pe", lambda E, bk=bk, j=j, kl=kl, st_ap=st_ap: E.transpose(bk.t[:, j * 128:(j + 1) * 128],
                                                                                        st_ap[:, kl * 128:(kl + 1) * 128], ident.t[:]),
                             reads=[st_b, ident.B], writes=[bk.B])
                    dst = x.t[:, g4 * 4:(g4 + 1) * 4, blk * 128:(blk + 1) * 128]
                    srcp = bk.t[:].rearrange("p (j t) -> p j t", j=4)
                    if g4 % 2 == 0:
                        P.op("dve", lambda E, dst=dst, srcp=srcp: E.tensor_copy(dst, srcp), reads=[bk.B], writes=[x.B])
                    else:
                        P.op("act", lambda E, dst=dst, srcp=srcp: E.activation(out=dst, in_=srcp, func=AF.Copy), reads=[bk.B], writes=[x.B])

        def store_x(x, dst):
            for blk in range(4):
                for g4 in range(4):
                    st_ap, st_b = xstg[(blk * 2 + g4 // 2) % 2]
                    bk = bank["H"] if g4 % 2 == 0 else bank["G"]
                    for j in range(4):
                        kc = g4 * 4 + j
                        P.op("pe", lambda E, bk=bk, j=j, kc=kc, blk=blk: E.transpose(
                            bk.t[:, j * 128:(j + 1) * 128], x.t[:, kc, blk * 128:(blk + 1) * 128], ident.t[:]),
                            reads=[x.B, ident.B], writes=[bk.B])
                    dsts = st_ap[:, (g4 % 2) * 512:(g4 % 2 + 1) * 512]
                    if g4 % 2 == 0:
                        P.op("dve", lambda E, dsts=dsts, bk=bk: E.tensor_copy(dsts, bk.t[:]), reads=[bk.B], writes=[st_b])
                    else:
                        P.op("act", lambda E, dsts=dsts, bk=bk: E.activation(out=dsts, in_=bk.t[:], func=AF.Copy), reads=[bk.B], writes=[st_b])
                        P.dma("sp", dst[blk * 128:(blk + 1) * 128, (g4 // 2) * 1024:(g4 // 2 + 1) * 1024], st_ap, reads=[st_b], writes=[Buf()])

        sqr = sb("sqr", (128, 3, TT), BF16, n=3)
        rstd = sb("rstd", (128, TT), F32)
        tmpf = sb("tmpf", (128, 2, TT), F32, n=2)

        def sum_sq_to_rstd(n_feat, dst):
            G = bank["G"]
            P.op("act", lambda E: E.activation(out=dst.t[:], in_=G.t[:], func=AF.Sqrt, bias=epsc.t[:, 0:1], scale=1.0 / n_feat),
                 reads=[G.B, epsc.B], writes=[dst.B])
            P.op("dve", lambda E: E.reciprocal(out=dst.t[:], in_=dst.t[:]), reads=[dst.B], writes=[dst.B])

        def Avec1(l, cnd):
            need_mod(l, 1)
            return A1.t[:, l, cnd, :]

        def Avec2(l, cnd):
            need_mod(l, 4)
            return A2.t[:, l, cnd, :]

        def norm_mod(x, h, Avec, Bvec):
            G = bank["G"]
            for kc in range(KC):
                i = kc % 2
                P.op("act", lambda E, kc=kc, i=i: E.activation(out=sqr.t[:, i, :], in_=x.t[:, kc, :], func=AF.Square),
                     reads=[x.B], writes=[sqr.b[i]])
                mm(G, G.t[:], onesb.t[:], sqr.t[:, i, :], kc == 0, kc == KC - 1, [onesb.B, sqr.b[i]])
            sum_sq_to_rstd(float(D), rstd)
            for kc in range(KC):
                i = kc % 2
                P.op("dve", lambda E, kc=kc, i=i: E.scalar_tensor_tensor(
                    out=tmpf.t[:, i, :], in0=x.t[:, kc, :], scalar=Avec[:, kc:kc + 1], in1=rstd.t[:], op0=ALU.mult, op1=ALU.mult),
                    reads=[x.B, rstd.B, A1.B, A2.B], writes=[tmpf.b[i]])
                P.op("act", lambda E, kc=kc, i=i: E.activation(out=h.t[:, kc, :], in_=tmpf.t[:, i, :], func=AF.Identity,
                                                              bias=Bvec[:, kc:kc + 1], scale=1.0),
                     reads=[tmpf.b[i], mod.B], writes=[h.B])

        hq = sb("hq", (128, TT), F32)
        hq1 = sb("hq1", (128, TT), F32)
        hq2 = [hq, hq1]
        hrs = sb("hrs", (128, TT), F32)
        r1 = sb("r1", (128, TT), F32)
        r2 = sb("r2", (128, TT), F32)
        qb = sb("qb", (128, TT), BF16)
        qb1 = sb("qb1", (128, TT), BF16)
        qb2 = [qb, qb1]
        ropec = sb("ropec", (128, TT), F32)
        ropes = sb("ropes", (128, TT), F32)

        def head_norm(ps, gain_ap, out_f32=None, out_bf=None):
            G = bank["G"]
            P.op("act", lambda E: E.activation(out=sqr.t[:, 0, :], in_=ps.t[:], func=AF.Square), reads=[ps.B], writes=[sqr.b[0]])
            mm(G, G.t[:], onesb.t[:], sqr.t[:, 0, :], True, True, [onesb.B, sqr.b[0]])
            sum_sq_to_rstd(128.0, hrs)
            if out_f32 is not None:
                P.op("dve", lambda E: E.scalar_tensor_tensor(out=out_f32.t[:], in0=ps.t[:], scalar=gain_ap, in1=hrs.t[:],
                                                             op0=ALU.mult, op1=ALU.mult), reads=[ps.B, hrs.B, qg.B, kg.B], writes=[out_f32.B])
            else:
                P.op("dve", lambda E: E.scalar_tensor_tensor(out=out_bf, in0=ps.t[:], scalar=gain_ap, in1=hrs.t[:],
                                                             op0=ALU.mult, op1=ALU.mult), reads=[ps.B, hrs.B, qg.B, kg.B], writes=[])

        def rope_to(src_f32, out_ap, out_buf):
            H = bank["H"]
            mm(H, H.t[:], rt.t[:], src_f32.t[:], True, True, [rt.B, src_f32.B])
            P.op("dve", lambda E: E.tensor_tensor(out=src_f32.t[:], in0=src_f32.t[:], in1=ropec.t[:], op=ALU.mult),
                 reads=[src_f32.B, ropec.B], writes=[src_f32.B])
            P.op("dve", lambda E: E.tensor_tensor(out=tmpf.t[:, 1, :], in0=H.t[:], in1=ropes.t[:], op=ALU.mult),
                 reads=[H.B, ropes.B], writes=[tmpf.b[1]])
            P.op("dve", lambda E: E.tensor_tensor(out=out_ap, in0=src_f32.t[:], in1=tmpf.t[:, 1, :], op=ALU.add),
                 reads=[src_f32.B, tmpf.b[1]], writes=[out_buf])


        def plan_kv(l):
            return [(win_d[l, :, 1024:1280], ("in", 256)), (win_d[l, :, 1280:1536], ("in", 256))]

        def emit_kv(l, h, KTb, Vb, kcol0, vblk0, rope, cache_out=None):
            wk, wkb = w_get(win_d[l, :, 1024:1280], ("in", 256))
            for kvh in range(2):
                bk = bank["A%d" % kvh]
                for kc in range(KC):
                    mm(bk, bk.t[:], wk[:, kc, kvh * 128:(kvh + 1) * 128], h.t[:, kc, :], kc == 0, kc == KC - 1, [wkb, h.B])
                dst = KTb.t[:, kvh, kcol0:kcol0 + TT]
                if rope:
                    head_norm(bk, kg.t[:, l:l + 1], out_f32=hq)
                    rope_to(hq, dst, KTb.B)
                else:
                    head_norm(bk, kg.t[:, l:l + 1], out_f32=hq)
                    P.op("act", lambda E, dst=dst: E.activation(out=dst, in_=hq.t[:], func=AF.Copy), reads=[hq.B], writes=[KTb.B])
                    if cache_out is not None:
                        H = bank["H"]
                        for blk in range(4):
                            P.op("pe", lambda E, blk=blk: E.transpose(H.t[:, blk * 128:(blk + 1) * 128], hq.t[:, blk * 128:(blk + 1) * 128], ident.t[:]),
                                 reads=[hq.B, ident.B], writes=[H.B])
                        P.op("dve", lambda E, kvh=kvh: E.tensor_copy(kvstage.t[:, :, kvh * 128:(kvh + 1) * 128],
                                                                     H.t[:].rearrange("p (b d) -> p b d", b=4)),
                             reads=[H.B], writes=[kvstage.B])
            if cache_out is not None:
                for blk in range(4):
                    P.dma("sp", nk_d[blk // 2, l, (blk % 2) * 128:(blk % 2 + 1) * 128, :], kvstage.t[:, blk, :],
                          reads=[kvstage.B], writes=[Buf()])
            w_release()
            wv, wvb = w_get(win_d[l, :, 1280:1536], ("in", 256))
            for blk in range(4):
                bk = bank["B%d" % (blk // 2)]
                o_ap = bk.t[:, (blk % 2) * 256:(blk % 2 + 1) * 256]
                for kc in range(KC):
                    mm(bk, o_ap, h.t[:, kc, blk * 128:(blk + 1) * 128], wv[:, kc, :], kc == 0, kc == KC - 1, [wvb, h.B])
                if blk % 2 == 1:
                    P.op("act", lambda E, bk=bk, blk=blk: E.activation(out=Vb.t[:, vblk0 + blk - 1:vblk0 + blk + 1, :],
                                                                      in_=bk.t[:].rearrange("p (b d) -> p b d", b=2), func=AF.Copy),
                         reads=[bk.B], writes=[Vb.B])
                    if cache_out is not None:
                        P.op("dve", lambda E, bk=bk, blk=blk: E.tensor_copy(kvstage.t[:, blk - 1:blk + 1, :],
                                                                            bk.t[:].rearrange("p (b d) -> p b d", b=2)),
                             reads=[bk.B], writes=[kvstage.B])
            w_release()
            if cache_out is not None:
                for blk in range(4):
                    P.dma("sp", nv_d[blk // 2, l, (blk % 2) * 128:(blk % 2 + 1) * 128, :], kvstage.t[:, blk, :],
                          reads=[kvstage.B], writes=[Buf()])

        PT = sb("PT", (128, 4, TT), BF16, n=4)
        uf = sb("uf", (128, TT), F32)
        ghat = sb("ghat", (128, 4, 256), BF16)
        gss = sb("gss", (128, 8), F32)
        gsq = sb("gsq", (128, 128), F32)
        ssa = sb("ssa", (128, TT), F32)
        sss = sb("sss", (128, TT), F32)
        SCALE = 1.0 / float(np.sqrt(128.0))

        def plan_mixer(l):
            items = []
            for hp in range(4):
                items.append((win_d[l, :, 2560 + hp * 256:2560 + (hp + 1) * 256], ("in", 256)))
                items.append((win_d[l, :, 1536 + hp * 256:1536 + (hp + 1) * 256], ("in", 256)))
                items.append((win_d[l, :, hp * 256:(hp + 1) * 256], ("in", 256)))
            for pc in range(8):
                items.append((wout_d[l, :, pc * 256:(pc + 1) * 256], ("in", 256)))
            return items

        deferred = []

        def flush():
            for f in deferred:
                f()
            del deferred[:]

        def accum_sumsq(src_f32_ap, src_buf, acc, first, si):
            G = bank["G"]
            if any(getattr(f, "si", None) == si for f in deferred):
                flush()
            P.op("act", lambda E: E.activation(out=sqr.t[:, si, :], in_=src_f32_ap, func=AF.Square), reads=[src_buf], writes=[sqr.b[si]])

            def part2():
                mm(G, G.t[:], onesb.t[:], sqr.t[:, si, :], True, True, [onesb.B, sqr.b[si]])
                if first:
                    P.op("dve", lambda E: E.tensor_copy(acc.t[:], G.t[:]), reads=[G.B], writes=[acc.B])
                else:
                    P.op("dve", lambda E: E.tensor_tensor(out=acc.t[:], in0=acc.t[:], in1=G.t[:], op=ALU.add), reads=[G.B, acc.B], writes=[acc.B])
            part2.si = si
            deferred.append(part2)

        def emit_mixer(l, cnd, x, h, o, KTb, Vb, groups, rope):
            P.dma("sp", bsrep.t[:, 0], bsrep_d[:, l], reads=[bIN], writes=[bsrep.B])
            for hp in range(4):
                c4 = hp
                wg_, wgb = w_get(win_d[l, :, 2560 + hp * 256:2560 + (hp + 1) * 256], ("in", 256))
                for blk in range(4):
                    bk = bank["A%d" % (blk % 2)]
                    o_ap = bk.t[:, 0:256]
                    for kc in range(KC):
                        mm(bk, o_ap, h.t[:, kc, blk * 128:(blk + 1) * 128], wg_[:, kc, :], kc == 0, kc == KC - 1, [wgb, h.B])
                    P.op("dve", lambda E, c4=c4: E.memset(gss.t[:, c4 * 2:c4 * 2 + 2], 0.0), reads=[gss.B], writes=[gss.B])
                    for hh in range(2):
                        hd = c4 * 2 + hh
                        P.op("act", lambda E, bk=bk, hh=hh, hd=hd: E.activation(out=gsq.t[:], in_=bk.t[:, hh * 128:(hh + 1) * 128], func=AF.Square,
                                                                               accum_out=gss.t[:, hd:hd + 1]),
                             reads=[bk.B], writes=[gsq.B, gss.B])
                    P.op("act", lambda E, c4=c4: E.activation(out=gss.t[:, c4 * 2:c4 * 2 + 2], in_=gss.t[:, c4 * 2:c4 * 2 + 2], func=AF.Sqrt,
                                                              bias=epsc.t[:, 0:1], scale=1.0 / 128.0), reads=[gss.B, epsc.B], writes=[gss.B])
                    P.op("dve", lambda E, c4=c4: E.reciprocal(out=gss.t[:, c4 * 2:c4 * 2 + 2], in_=gss.t[:, c4 * 2:c4 * 2 + 2]),
                         reads=[gss.B], writes=[gss.B])
                    for hh in range(2):
                        hd = c4 * 2 + hh
                        P.op("dve", lambda E, bk=bk, hh=hh, hd=hd, blk=blk: E.tensor_scalar(
                            out=ghat.t[:, blk, hh * 128:(hh + 1) * 128], in0=bk.t[:, hh * 128:(hh + 1) * 128],
                            scalar1=gss.t[:, hd:hd + 1], scalar2=None, op0=ALU.mult), reads=[bk.B, gss.B], writes=[ghat.B])
                w_release()
                flush()
                wu_, wub = w_get(win_d[l, :, 1536 + hp * 256:1536 + (hp + 1) * 256], ("in", 256))
                wq_, wqb = w_get(win_d[l, :, hp * 256:(hp + 1) * 256], ("in", 256))
                C, Dk = bank["C"], bank["Dk"]

                def sgu_head(hh):
                    hd = hp * 2 + hh
                    bkA = bank["A0"]
                    for kc in range(KC):
                        mm(bkA, bkA.t[:], wu_[:, kc, hh * 128:(hh + 1) * 128], h.t[:, kc, :], kc == 0, kc == KC - 1, [wub, h.B])
                    P.op("act", lambda E: E.activation(out=uf.t[:], in_=bkA.t[:], func=AF.Copy), reads=[bkA.B], writes=[uf.B])
                    bkB = bank["B%d" % hh]
                    for blk in range(4):
                        mm(bkB, bkB.t[:, blk * 128:(blk + 1) * 128], ghat.t[:, blk, hh * 128:(hh + 1) * 128], wsT.t[:, l, hd, :],
                           True, True, [ghat.B, wsT.B])
                    for blk in range(4):
                        P.op("dve", lambda E, blk=blk: E.scalar_tensor_tensor(
                            out=r1.t[:, blk * 128:(blk + 1) * 128], in0=bkB.t[:, blk * 128:(blk + 1) * 128], scalar=sgn.t[:, l, hd:hd + 1],
                            in1=bsrep.t[:, 0, hd, :], op0=ALU.mult, op1=ALU.add), reads=[bkB.B, sgn.B, bsrep.B], writes=[r1.B])
                    P.op("dve", lambda E: E.tensor_tensor(out=r2.t[:], in0=r1.t[:], in1=uf.t[:], op=ALU.mult), reads=[r1.B, uf.B], writes=[r2.B])
                    P.op("act", lambda E: E.activation(out=o.t[:, 8 + hd, :], in_=r2.t[:], func=AF.Copy, scale=ong.t[:, l, 8 + hd:9 + hd]),
                         reads=[r2.B, ong.B], writes=[o.B])
                    accum_sumsq(r2.t[:], r2.B, sss, hd == 0, 1)

                def q_proj_norm(hh):
                    bkQ = bank["A1"]
                    for kc in range(KC):
                        mm(bkQ, bkQ.t[:], wq_[:, kc, hh * 128:(hh + 1) * 128], h.t[:, kc, :], kc == 0, kc == KC - 1, [wqb, h.B])
                    head_norm(bkQ, qg.t[:, l:l + 1], out_f32=hq2[hh])

                def q_finish(hh):
                    if rope:
                        rope_to(hq2[hh], qb2[hh].t[:], qb2[hh].B)
                    else:
                        P.op("act", lambda E: E.activation(out=qb2[hh].t[:], in_=hq2[hh].t[:], func=AF.Copy), reads=[hq2[hh].B], writes=[qb2[hh].B])

                def attention(hh, hook=None):
                    hd = hp * 2 + hh
                    kvh = hd // 4
                    qbh = qb2[hh]
                    for gi, (q0, q1, kblocks) in enumerate(groups):
                        nkb = len(kblocks)

                        def score(ji, q0=q0, q1=q1, kblocks=kblocks):
                            j = kblocks[ji]
                            bS = bank["B%d" % (ji % 2)]
                            mm(bS, bS.t[:, q0:q1], KTb.t[:, kvh, j * 128:(j + 1) * 128], qbh.t[:, q0:q1], True, True, [KTb.B, qbh.B])
                            P.op("act", lambda E: E.activation(out=PT.t[:, ji % 4, q0:q1], in_=bS.t[:, q0:q1], func=AF.Exp, scale=SCALE),
                                 reads=[bS.B], writes=[PT.b[ji % 4]])
                        score(0)
                        for ji in range(nkb):
                            if ji + 1 < nkb:
                                score(ji + 1)
                            j = kblocks[ji]
                            mm(C, C.t[:, q0:q1], Vb.t[:, j, kvh * 128:(kvh + 1) * 128], PT.t[:, ji % 4, q0:q1], ji == 0, ji == nkb - 1,
                               [Vb.B, PT.b[ji % 4]])
                            mm(Dk, Dk.t[:, q0:q1], onesb.t[:], PT.t[:, ji % 4, q0:q1], ji == 0, ji == nkb - 1, [onesb.B, PT.b[ji % 4]])
                            if hook is not None and gi == 0 and ji == min(3, nkb - 1):
                                hook()
                    P.op("dve", lambda E: E.reciprocal(out=r1.t[:], in_=Dk.t[:]), reads=[Dk.B], writes=[r1.B])
                    P.op("dve", lambda E: E.tensor_tensor(out=r2.t[:], in0=C.t[:], in1=r1.t[:], op=ALU.mult), reads=[C.B, r1.B], writes=[r2.B])
                    P.op("act", lambda E: E.activation(out=o.t[:, hd, :], in_=r2.t[:], func=AF.Copy, scale=ong.t[:, l, hd:hd + 1]),
                         reads=[r2.B, ong.B], writes=[o.B])
                    accum_sumsq(r2.t[:], r2.B, ssa, hd == 0, 2)

                sgu_head(0)
                q_proj_norm(0)
                sgu_head(1)
                flush()
                q_finish(0)
                q_proj_norm(1)
                flush()
                attention(0, hook=lambda: q_finish(1))
                attention(1)
                w_release()
                mod_step(bgc["hp"])
            flush()
            rsa, rss = ssa, sss
            for (acc, dstr) in ((ssa, ssa), (sss, sss)):
                P.op("act", lambda E, acc=acc, dstr=dstr: E.activation(out=dstr.t[:], in_=acc.t[:], func=AF.Sqrt, bias=epsc.t[:, 0:1], scale=1.0 / 1024.0),
                     reads=[acc.B, epsc.B], writes=[dstr.B])
                P.op("dve", lambda E, dstr=dstr: E.reciprocal(out=dstr.t[:], in_=dstr.t[:]), reads=[dstr.B], writes=[dstr.B])
            g1 = mvec(l, cnd, 2)
            for pc in range(8):
                wo_, wob = w_get(wout_d[l, :, pc * 256:(pc + 1) * 256], ("in", 256))
                for c2 in range(2):
                    oc = pc * 2 + c2
                    bkA, bkB = bank["A%d" % c2], bank["B%d" % c2]
                    for kc in range(8):
                        mm(bkA, bkA.t[:], wo_[:, kc, c2 * 128:(c2 + 1) * 128], o.t[:, kc, :], kc == 0, kc == 7, [wob, o.B])
                    for kc in range(8, 16):
                        mm(bkB, bkB.t[:], wo_[:, kc, c2 * 128:(c2 + 1) * 128], o.t[:, kc, :], kc == 8, kc == 15, [wob, o.B])
                    if c2 == 1:
                        w_release()
                        mod_step(bgc["wo"])
                    P.op("dve", lambda E, bkA=bkA: E.tensor_tensor(out=r1.t[:], in0=bkA.t[:], in1=rsa.t[:], op=ALU.mult), reads=[bkA.B, rsa.B], writes=[r1.B])
                    P.op("dve", lambda E, bkB=bkB: E.tensor_tensor(out=r2.t[:], in0=bkB.t[:], in1=rss.t[:], op=ALU.mult), reads=[bkB.B, rss.B], writes=[r2.B])
                    P.op("dve", lambda E: E.tensor_tensor(out=r1.t[:], in0=r1.t[:], in1=r2.t[:], op=ALU.add), reads=[r1.B, r2.B], writes=[r1.B])
                    P.op("dve", lambda E, oc=oc: E.scalar_tensor_tensor(out=x.t[:, oc, :], in0=r1.t[:], scalar=g1[:, oc:oc + 1], in1=x.t[:, oc, :],
                                                                       op0=ALU.mult, op1=ALU.add), reads=[r1.B, mod.B, x.B], writes=[x.B])

        sg = sb("sg", (128, 2, TT), F32, n=2)
        actb = sb("actb", (128, 2, 4, TT), BF16, n=2)
        cwrep = sb("cwrep", (128, TT), F32)
        cw4 = sb("cw4", (128, 4, NE), F32)
        lg = sb("lg", (128, NE), F32)
        lg8 = sb("lg8", (128, 8), F32)
        cw = sb("cw", (128, NE), F32)
        cwb = sb("cwb", (128, 128), F32)
        tcol = sb("tcol", (128, 4), F32)
        wrp = sb("wrp", (128, KC, NE), F32)

        def ffn_panel_list(l):
            out = []
            if l == 0:
                for p in range(DFF // 256):
                    out.append((fg_d[0][:, p * 256:(p + 1) * 256], fu_d[0][:, p * 256:(p + 1) * 256], fd_d[0][p * 256:(p + 1) * 256, :], None))
            else:
                for e in range(NE):
                    for p in range(DFE // 256):
                        out.append((mg_d[0, e][:, p * 256:(p + 1) * 256], mu_d[0, e][:, p * 256:(p + 1) * 256],
                                    md_d[0, e][p * 256:(p + 1) * 256, :], e))
            assert len(out) % 2 == 0
            return out

        def plan_ffn(l):
            pl = ffn_panel_list(l)
            items = []
            for pp in range(len(pl) // 2):
                for half in range(2):
                    g_, u_, d_, e = pl[pp * 2 + half]
                    items.append((g_, ("in", 256)))
                    items.append((u_, ("in", 256)))
                for half in range(2):
                    items.append((pl[pp * 2 + half][2], ("rows", 2)))
            return items

        def emit_ffn_panels(l, x, h2, g2):
            pl = ffn_panel_list(l)
            cur_e = None
            for pp in range(len(pl) // 2):
                pi = pp % 2
                for half in range(2):
                    e = pl[pp * 2 + half][3]
                    if e is not None and e != cur_e:
                        emit_cwrep(e)
                        cur_e = e
                    wg_, wgb = w_get(pl[pp * 2 + half][0], ("in", 256))
                    for c2 in range(2):
                        bkG = bank["A%d" % c2]
                        for kc in range(KC):
                            mm(bkG, bkG.t[:], wg_[:, kc, c2 * 128:(c2 + 1) * 128], h2.t[:, kc, :], kc == 0, kc == KC - 1, [wgb, h2.B])
                    w_release()
                    wu_, wub = w_get(pl[pp * 2 + half][1], ("in", 256))
                    for c2 in range(2):
                        bkU = bank["B%d" % c2]
                        for kc in range(KC):
                            mm(bkU, bkU.t[:], wu_[:, kc, c2 * 128:(c2 + 1) * 128], h2.t[:, kc, :], kc == 0, kc == KC - 1, [wub, h2.B])
                    w_release()
                    for c2 in range(2):
                        bkG, bkU = bank["A%d" % c2], bank["B%d" % c2]
                        P.op("act", lambda E, bkG=bkG, c2=c2: E.activation(out=sg.t[:, c2, :], in_=bkG.t[:], func=AF.Silu), reads=[bkG.B], writes=[sg.b[c2]])
                        if e is not None:
                            P.op("dve", lambda E, c2=c2: E.tensor_tensor(out=sg.t[:, c2, :], in0=sg.t[:, c2, :], in1=cwrep.t[:], op=ALU.mult),
                                 reads=[sg.b[c2], cwrep.B], writes=[sg.b[c2]])
                        P.op("dve", lambda E, bkU=bkU, c2=c2, pi=pi, half=half: E.tensor_tensor(out=actb.t[:, pi, half * 2 + c2, :], in0=bkU.t[:], in1=sg.t[:, c2, :], op=ALU.mult),
                             reads=[bkU.B, sg.b[c2]], writes=[actb.b[pi]])
                wd0, wdb0 = w_get(pl[pp * 2][2], ("rows", 2))
                wd1, wdb1 = w_get(pl[pp * 2 + 1][2], ("rows", 2))
                for oc in range(KC):
                    bk = bank["C"] if oc % 2 == 0 else bank["Dk"]
                    for c4 in range(4):
                        wd_, wdb = (wd0, wdb0) if c4 < 2 else (wd1, wdb1)
                        mm(bk, bk.t[:], wd_[:, c4 % 2, oc * 128:(oc + 1) * 128], actb.t[:, pi, c4, :], c4 == 0, c4 == 3, [wdb, actb.b[pi]])
                    P.op("dve", lambda E, bk=bk, oc=oc: E.scalar_tensor_tensor(out=x.t[:, oc, :], in0=bk.t[:], scalar=g2[:, oc:oc + 1], in1=x.t[:, oc, :],
                                                                              op0=ALU.mult, op1=ALU.add), reads=[bk.B, mod.B, x.B], writes=[x.B])
                w_release()
                mod_step(bgc["ffn"])

        def emit_router(l, cnd, x):
            A2v = Avec2(l, cnd)
            sh2 = mvec(l, cnd, 3)
            for kc in range(KC):
                P.op("dve", lambda E, kc=kc: E.tensor_scalar(out=wrp.t[:, kc, :], in0=wr.t[:, kc, :], scalar1=A2v[:, kc:kc + 1], scalar2=None, op0=ALU.mult),
                     reads=[wr.B, A2.B], writes=[wrp.B])
            H, G = bank["H"], bank["G"]
            for kc in range(KC):
                P.op("dve", lambda E, kc=kc: E.tensor_scalar(out=gsq.t[:], in0=ident.t[:], scalar1=0.0, scalar2=sh2[:, kc:kc + 1], op0=ALU.mult, op1=ALU.add),
                     reads=[ident.B, mod.B, gsq.B], writes=[gsq.B])
                mm(G, G.t[:, 0:NE], gsq.t[:], wr.t[:, kc, :], kc == 0, kc == KC - 1, [gsq.B, wr.B])
            P.op("dve", lambda E: E.tensor_tensor(out=lg8.t[:], in0=G.t[:, 0:NE], in1=brrep.t[:], op=ALU.add), reads=[G.B, brrep.B], writes=[lg8.B])
            for blk in range(4):
                for kc in range(KC):
                    mm(H, H.t[:, 0:NE], x.t[:, kc, blk * 128:(blk + 1) * 128], wrp.t[:, kc, :], kc == 0, kc == KC - 1, [x.B, wrp.B])
                mm(G, G.t[:, 0:1], rstd.t[:, blk * 128:(blk + 1) * 128], ident.t[:, 0:1], True, True, [rstd.B, ident.B])
                P.op("dve", lambda E: E.tensor_copy(tcol.t[:, 0:1], G.t[:, 0:1]), reads=[G.B], writes=[tcol.B])
                P.op("dve", lambda E: E.scalar_tensor_tensor(out=lg.t[:], in0=H.t[:, 0:NE], scalar=tcol.t[:, 0:1], in1=lg8.t[:], op0=ALU.mult, op1=ALU.add),
                     reads=[H.B, tcol.B, lg8.B], writes=[lg.B])
                P.op("dve", lambda E: E.tensor_reduce(out=tcol.t[:, 1:2], in_=lg.t[:], axis=mybir.AxisListType.X, op=ALU.max), reads=[lg.B, tcol.B], writes=[tcol.B])
                P.op("dve", lambda E: E.tensor_scalar(out=cw.t[:], in0=lg.t[:], scalar1=tcol.t[:, 1:2], scalar2=-1e30, op0=ALU.is_ge, op1=ALU.mult),
                     reads=[lg.B, tcol.B], writes=[cw.B])
                P.op("dve", lambda E: E.tensor_tensor(out=cw.t[:], in0=cw.t[:], in1=lg.t[:], op=ALU.add), reads=[cw.B, lg.B], writes=[cw.B])
                P.op("dve", lambda E: E.tensor_reduce(out=tcol.t[:, 2:3], in_=cw.t[:], axis=mybir.AxisListType.X, op=ALU.max), reads=[cw.B, tcol.B], writes=[tcol.B])
                P.op("dve", lambda E: E.tensor_scalar(out=cw.t[:], in0=lg.t[:], scalar1=tcol.t[:, 2:3], scalar2=None, op0=ALU.is_ge), reads=[lg.B, tcol.B], writes=[cw.B])
                P.op("dve", lambda E: E.tensor_scalar(out=tcol.t[:, 3:4], in0=tcol.t[:, 1:2], scalar1=-1.0, scalar2=None, op0=ALU.mult), reads=[tcol.B], writes=[tcol.B])
                P.op("act", lambda E: E.activation(out=lg.t[:], in_=lg.t[:], func=AF.Exp, bias=tcol.t[:, 3:4], scale=1.0), reads=[lg.B, tcol.B], writes=[lg.B])
                P.op("dve", lambda E: E.tensor_tensor(out=cw.t[:], in0=cw.t[:], in1=lg.t[:], op=ALU.mult), reads=[cw.B, lg.B], writes=[cw.B])
                P.op("dve", lambda E: E.tensor_reduce(out=tcol.t[:, 0:1], in_=cw.t[:], axis=mybir.AxisListType.X, op=ALU.add), reads=[cw.B, tcol.B], writes=[tcol.B])
                P.op("dve", lambda E: E.reciprocal(out=tcol.t[:, 0:1], in_=tcol.t[:, 0:1]), reads=[tcol.B], writes=[tcol.B])
                P.op("dve", lambda E, blk=blk: E.tensor_scalar(out=cw4.t[:, blk, :], in0=cw.t[:], scalar1=tcol.t[:, 0:1], scalar2=None, op0=ALU.mult),
                     reads=[cw.B, tcol.B], writes=[cw4.B])

        def emit_cwrep(e):
            bk = bank["H"]
            for blk in range(4):
                P.op("dve", lambda E, blk=blk: E.tensor_scalar(out=cwb.t[:], in0=ident.t[:], scalar1=0.0, scalar2=cw4.t[:, blk, e:e + 1], op0=ALU.mult, op1=ALU.add),
                     reads=[ident.B, cw4.B, cwb.B], writes=[cwb.B])
                mm(bk, bk.t[:, blk * 128:(blk + 1) * 128], cwb.t[:], ident.t[:], True, True, [cwb.B, ident.B])
            P.op("act", lambda E: E.activation(out=cwrep.t[:], in_=bk.t[:], func=AF.Copy), reads=[bk.B], writes=[cwrep.B])

        def emit_ffn(l, cnd, x, h2):
            g2 = mvec(l, cnd, 5)
            norm_mod(x, h2, Avec2(l, cnd), mvec(l, cnd, 3))
            if l == 1:
                emit_router(l, cnd, x)
            emit_ffn_panels(l, x, h2, g2)

        xA = sb("xA", (128, KC, TT), F32)
        xpark = nc.dram_tensor("xpark", [128, KC, TT], F32, kind="Internal").ap()
        bpark = Buf("xpark")
        hb = sb("hb", (128, KC, TT), BF16)
        ob = sb("ob", (128, KC, TT), BF16)
        KT0 = sb("KT0", (128, 2, 2304), BF16)
        V0 = sb("V0", (128, 18, 256), BF16)
        KT1 = sb("KT1", (128, 2, 2304), BF16)
        V1 = sb("V1", (128, 18, 256), BF16)
        cstage = T(xstage.t[:, 0:512].rearrange("p (r f) -> p r f", r=2), 1)
        cstage.b = xstage.b
        cstage.B = xstage.B

        def load_rope(t):
            P.dma("sp", ropec.t[:], ropec_d[t], reads=[bIN], writes=[ropec.B])
            P.dma("sp", ropes.t[:], ropes_d[t], reads=[bIN], writes=[ropes.B])

        def load_cache(l, KTb, Vb):
            P.dma("sp", cstage.t, ck_d[l].rearrange("(r p) f -> p r f", p=128), reads=[bIN], writes=[cstage.B])
            H = bank["H"]
            for kvh in range(2):
                for r in range(2):
                    P.op("pe", lambda E, kvh=kvh, r=r: E.transpose(H.t[:, (kvh * 2 + r) * 128:(kvh * 2 + r + 1) * 128],
                                                                   cstage.t[:, r, kvh * 128:(kvh + 1) * 128], ident.t[:]),
                         reads=[cstage.B, ident.B], writes=[H.B])
            P.op("act", lambda E: E.activation(out=KTb.t[:, :, 0:256], in_=H.t[:].rearrange("p (k t) -> p k t", k=2), func=AF.Copy),
                 reads=[H.B], writes=[KTb.B])
            P.dma("pool", Vb.t[:, 0:2, :], cv_d[l].rearrange("(r p) f -> p r f", p=128), reads=[bIN], writes=[Vb.B])

        pgroups = [(0, 256, [0, 1]), (256, 512, [2, 3])]
        sgroups = [(0, 512, list(range(18)))]

        def schedule():
            bgc.update(hp=0, wo=0, ffn=0)
            mod_step(16)
            load_cache(0, KT0, V0)
            load_cache(1, KT1, V1)
            for t in range(4):
                load_x(xs_d[t], xA)
                load_rope(t)
                norm_mod(xA, hb, Avec1(0, 1), mvec(0, 1, 0))
                emit_kv(0, hb, KT0, V0, 256 + t * 512, 2 + t * 4, rope=True)
                mod_step(8)
            bgc.update(hp=1, wo=1, ffn=1)
            for t in (2, 3, 0, 1):
                xt = xA
                load_x(xs_d[t], xt)
                load_rope(t)
                norm_mod(xt, hb, Avec1(0, 1), mvec(0, 1, 0))
                emit_mixer(0, 1, xt, hb, ob, KT0, V0, sgroups, rope=True)
                emit_ffn(0, 1, xt, hb)
                norm_mod(xt, hb, Avec1(1, 1), mvec(1, 1, 0))
                emit_kv(1, hb, KT1, V1, 256 + t * 512, 2 + t * 4, rope=True)
                if t == 1:
                    emit_mixer(1, 1, xt, hb, ob, KT1, V1, sgroups, rope=True)
                    emit_ffn(1, 1, xt, hb)
                    store_x(xt, ys_d[1])
                if t == 0:
                    P.dma("sp", xpark, xA.t[:], reads=[xA.B], writes=[bpark])
            load_rope(0)
            P.dma("sp", xA.t[:], xpark, reads=[bpark], writes=[xA.B])
            norm_mod(xA, hb, Avec1(1, 1), mvec(1, 1, 0))
            emit_mixer(1, 1, xA, hb, ob, KT1, V1, sgroups, rope=True)
            emit_ffn(1, 1, xA, hb)
            store_x(xA, ys_d[0])
            load_x(xp_d, xA)
            for l in range(NL):
                norm_mod(xA, hb, Avec1(l, 0), mvec(l, 0, 0))
                emit_kv(l, hb, KT0, V0, 0, 0, rope=False, cache_out=True)
                emit_mixer(l, 0, xA, hb, ob, KT0, V0, pgroups, rope=False)
                emit_ffn(l, 0, xA, hb)
            store_x(xA, yp_d)
            assert modst["i"] == NL * 48 and not deferred

        P.dry = True
        schedule()
        n_plan = wstate["taken"]
        wstate.update(issued=0, taken=0, released=0)
        modst["i"] = 0
        P.dry = False
        schedule()
        assert wstate["taken"] == n_plan == len(wstate["plan"]), (wstate["taken"], n_plan, len(wstate["plan"]))
        P.finish()
        build_program.stats = (P.n_ops, P.n_wait, nc.sbuf_bytes_remaining)
    return nc


def _rope_tables():
    L_ = 2048
    rows = (np.arange(L_) // 64).astype(np.float32)
    cols = (np.arange(L_) % 64).astype(np.float32)
    inv = (10000.0 ** (-np.arange(0, 64, 2, dtype=np.float32) / 64.0)).astype(np.float32)
    ar = rows[:, None] * inv[None, :]
    ac = cols[:, None] * inv[None, :]
    ang = np.concatenate([ar, ar, ac, ac], axis=1)
    return np.cos(ang).astype(np.float32), np.sin(ang).astype(np.float32)


def _rt_matrix():
    rt = np.zeros((128, 128), np.float32)
    for base in (0, 64):
        for i in range(32):
            m = base + i
            rt[m + 32, m] = -1.0
            rt[m, m + 32] = 1.0
    return rt


def _fm(v):
    v = np.asarray(v, np.float32)
    lead = v.shape[:-1]
    return np.ascontiguousarray(np.moveaxis(v.reshape(lead + (KC, 128)), -1, 0))


_NC_CACHE = {}


def kernel(x_prompt, x_sample, cache_k, cache_v, c, c_ctx, w_ada, b_ada, norm1_g, norm2_g,
           w_in, q_norm_g, k_norm_g, sgu_norm_g, w_spatial, b_spatial, out_norm_g, w_out,
           ffn_w_gate, ffn_w_up, ffn_w_down, w_router, b_router, moe_w_gate, moe_w_up, moe_w_down):
    f32 = lambda a: np.ascontiguousarray(np.asarray(a, dtype=np.float32))
    x_prompt, x_sample, cache_k, cache_v = f32(x_prompt), f32(x_sample), f32(cache_k), f32(cache_v)
    c, c_ctx = f32(c), f32(c_ctx)
    if "nc" not in _NC_CACHE:
        _NC_CACHE["nc"] = build_program()
    nc = _NC_CACHE["nc"]
    in_maps = _prep(x_prompt, x_sample, cache_k, cache_v, c, c_ctx, w_ada, b_ada, norm1_g, norm2_g,
                    w_in, q_norm_g, k_norm_g, sgu_norm_g, w_spatial, b_spatial, out_norm_g, w_out,
                    ffn_w_gate, ffn_w_up, ffn_w_down, w_router, b_router, moe_w_gate, moe_w_up, moe_w_down)
    res = run_bass_kernel_spmd(nc, in_maps, core_ids=list(range(NCORES)))
    return _assemble(res.results)


def _prep(x_prompt, x_sample, cache_k, cache_v, c, c_ctx, w_ada, b_ada, norm1_g, norm2_g,
          w_in, q_norm_g, k_norm_g, sgu_norm_g, w_spatial, b_spatial, out_norm_g, w_out,
          ffn_w_gate, ffn_w_up, ffn_w_down, w_router, b_router, moe_w_gate, moe_w_up, moe_w_down):
    f32 = lambda a: np.ascontiguousarray(np.asarray(a, dtype=np.float32))

    cos, sin = _rope_tables()
    shared = {
        "n1g": _fm(norm1_g), "n2g": _fm(norm2_g),
        "bada": np.ascontiguousarray(np.moveaxis(f32(b_ada).reshape(NL, 96, 128), -1, 0)),
        "qg": np.ascontiguousarray(f32(q_norm_g).T), "kg": np.ascontiguousarray(f32(k_norm_g).T),
        "sgn": np.ascontiguousarray(np.moveaxis(f32(sgu_norm_g), -1, 0)),
        "bsrep": np.ascontiguousarray(np.broadcast_to(f32(b_spatial)[None], (128, NL, 8, 128))),
        "wsT": np.ascontiguousarray(np.transpose(f32(w_spatial), (3, 0, 1, 2))),
        "ong": _fm(out_norm_g),
        "wr": np.ascontiguousarray(np.transpose(f32(w_router)[0].reshape(KC, 128, NE), (1, 0, 2))),
        "brrep": np.ascontiguousarray(np.broadcast_to(f32(b_router)[0][None], (128, NE))),
        "ident": np.eye(128, dtype=np.float32), "rt": _rt_matrix(),
        "w_ada": f32(w_ada), "w_in": f32(w_in), "w_out": f32(w_out),
        "ffn_w_gate": f32(ffn_w_gate), "ffn_w_up": f32(ffn_w_up), "ffn_w_down": f32(ffn_w_down),
        "moe_w_gate": f32(moe_w_gate), "moe_w_up": f32(moe_w_up), "moe_w_down": f32(moe_w_down),
    }
    in_maps = []
    for core in range(NCORES):
        b, half = core // 2, core % 2
        own = x_sample[b, half * 1024:(half + 1) * 1024].reshape(2, TT, D)
        oth = x_sample[b, (1 - half) * 1024:(2 - half) * 1024].reshape(2, TT, D)
        pos = np.concatenate([np.arange(half * 1024, (half + 1) * 1024), np.arange((1 - half) * 1024, (2 - half) * 1024)])
        m = dict(shared)
        m["xs"] = np.ascontiguousarray(np.concatenate([own, oth], axis=0))
        m["xp"] = np.ascontiguousarray(x_prompt[2 * core:2 * core + 2].reshape(TT, D))
        m["ropec"] = np.ascontiguousarray(cos[pos].reshape(4, TT, 128).transpose(0, 2, 1))
        m["ropes"] = np.ascontiguousarray(sin[pos].reshape(4, TT, 128).transpose(0, 2, 1))
        m["ck"] = np.ascontiguousarray(cache_k[b].reshape(NL, 256, 256))
        m["cv"] = np.ascontiguousarray(cache_v[b].reshape(NL, 256, 256))
        cv2 = np.stack([c_ctx, c[b]], axis=-1)
        m["cvec"] = np.ascontiguousarray(cv2.reshape(KC, 128, 2).transpose(1, 0, 2))
        in_maps.append(m)
    return in_maps


def _assemble(R):
    y_prompt = np.empty((16, 256, D), np.float32)
    y_sample = np.empty((4, 2048, D), np.float32)
    nk = np.empty((16, NL, 256, 2, 128), np.float32)
    nv = np.empty((16, NL, 256, 2, 128), np.float32)
    for core in range(NCORES):
        b, half = core // 2, core % 2
        r = R[core]
        y_prompt[2 * core:2 * core + 2] = np.asarray(r["yp"]).reshape(2, 256, D)
        y_sample[b, half * 1024:(half + 1) * 1024] = np.asarray(r["ys"]).reshape(1024, D)
        nk[2 * core:2 * core + 2] = np.asarray(r["nk"]).reshape(2, NL, 256, 2, 128)
        nv[2 * core:2 * core + 2] = np.asarray(r["nv"]).reshape(2, NL, 256, 2, 128)
    return (y_prompt, y_sample, nk, nv)
```

```python
import numpy as np
from contextlib import ExitStack
import concourse.bass as bass
import concourse.mybir as mybir
from concourse.bass_utils import run_bass_kernel_spmd

F32 = mybir.dt.float32
BF16 = mybir.dt.bfloat16
ALU = mybir.AluOpType
AF = mybir.ActivationFunctionType

ENGS = ("pe", "act", "dve", "pool", "sp")

D = 2048
KC = 16
TT = 512
NL = 2
INW = 3584
DFF = 5632
NE = 8
DFE = 2816
EPS = 1e-6
NCORES = 8


class Buf:
    __slots__ = ("name", "w", "r", "excl")

    def __init__(self, name="", excl=False):
        self.name = name
        self.excl = excl
        self.w = None
        self.r = []


class Prog:
    NDMA = 6

    def __init__(self, nc, stack):
        self.nc = nc
        self.stack = stack
        self.ops = {e: [] for e in ENGS}
        self.sem = {}
        self.cnt = {}
        self.seen = {e: {} for e in ENGS}
        for e in ENGS:
            self._mk("c_" + e)
        self.dma_rr = {}
        for q in ("sp", "pool"):
            for i in range(self.NDMA):
                self._mk("d_%s_%d" % (q, i))
            self.dma_rr[q] = 0
        self._mk("d_cc")
        self.n_wait = 0
        self.n_ops = 0
        self.dry = False

    def _mk(self, key):
        self.sem[key] = self.stack.enter_context(self.nc.semaphore(key))
        self.cnt[key] = 0

    def _deps(self, reads, writes):
        d = {}

        def add(ev):
            if ev is None:
                return
            k, v = ev
            if d.get(k, 0) < v:
                d[k] = v
        for b in reads:
            add(b.w)
        for b in writes:
            add(b.w)
            for ev in b.r:
                add(ev)
        return d

    def _emit_waits(self, eng, deps):
        seen = self.seen[eng]
        for k, v in deps.items():
            if eng == "pe" and k == "c_pe":
                continue
            if seen.get(k, 0) >= v:
                continue
            seen[k] = v
            sem = self.sem[k]
            self.ops[eng].append(lambda E, sem=sem, v=v: E.wait_ge(sem, v))
            self.n_wait += 1

    def _record(self, ev, reads, writes):
        for b in reads:
            b.r.append(ev)
            if len(b.r) > 48:
                m = {}
                for k, v in b.r:
                    if m.get(k, 0) < v:
                        m[k] = v
                b.r = list(m.items())
        for b in writes:
            b.w = ev
            b.r = []

    def op(self, eng, fn, reads=(), writes=()):
        if self.dry:
            return
        if any(b.excl for b in reads):
            writes = list(writes) + [b for b in reads if b.excl]
            reads = [b for b in reads if not b.excl]
        deps = self._deps(reads, writes)
        self._emit_waits(eng, deps)
        key = "c_" + eng
        self.cnt[key] += 1
        v = self.cnt[key]
        sem = self.sem[key]
        self.ops[eng].append(lambda E, fn=fn, sem=sem: fn(E).then_inc(sem, 1))
        self._record((key, v), reads, writes)
        self.n_ops += 1

    def dma(self, q, out, in_, reads=(), writes=()):
        if self.dry:
            return
        deps = self._deps(reads, writes)
        i = self.dma_rr[q]
        self.dma_rr[q] = (i + 1) % self.NDMA
        key = "d_%s_%d" % (q, i)
        if self.cnt[key] > 0 and deps.get(key, 0) < self.cnt[key]:
            deps[key] = self.cnt[key]
        self._emit_waits(q, deps)
        self.cnt[key] += 16
        v = self.cnt[key]
        sem = self.sem[key]
        self.ops[q].append(lambda E, out=out, in_=in_, sem=sem: E.dma_start(out=out, in_=in_).then_inc(sem, 16))
        self._record((key, v), reads, writes)
        self.n_ops += 1

    def coll(self, in_ap, out_ap, groups, reads=(), writes=()):
        if self.dry:
            return
        deps = self._deps(reads, writes)
        self._emit_waits("pool", deps)
        key = "d_cc"
        self.cnt[key] += 1
        v = self.cnt[key]
        sem = self.sem[key]
        self.ops["pool"].append(lambda E: E.collective_compute(
            "AllGather", ALU.bypass, replica_groups=groups, ins=[in_ap], outs=[out_ap]).then_inc(sem, 1))
        self._record((key, v), reads, writes)
        self.n_ops += 1

    def finish(self):
        deps = {k: v for k, v in self.cnt.items() if k.startswith("d_") and v > 0}
        self._emit_waits("sp", deps)
        ops = self.ops
        with self.nc.Block() as block:
            @block.tensor
            def _(E):
                for f in ops["pe"]:
                    f(E)

            @block.scalar
            def _(E):
                for f in ops["act"]:
                    f(E)

            @block.vector
            def _(E):
                for f in ops["dve"]:
                    f(E)

            @block.gpsimd
            def _(E):
                for f in ops["pool"]:
                    f(E)

            @block.sync
            def _(E):
                for f in ops["sp"]:
                    f(E)


class T:
    def __init__(self, t, n=1, excl=False, name=""):
        self.t = t
        self.b = [Buf(name + str(i), excl) for i in range(n)]
        self.B = self.b[0]


def build_program(debug=0):
    nc = bass.Bass("TRN2", target_bir_lowering=False, num_devices=NCORES)
    dt_in = lambda name, shape: nc.dram_tensor(name, list(shape), F32, kind="ExternalInput").ap()
    dt_out = lambda name, shape: nc.dram_tensor(name, list(shape), F32, kind="ExternalOutput").ap()
    xs_d = dt_in("xs", (4, TT, D))
    xp_d = dt_in("xp", (TT, D))
    ropec_d = dt_in("ropec", (4, 128, TT))
    ropes_d = dt_in("ropes", (4, 128, TT))
    ck_d = dt_in("ck", (NL, 256, 256))
    cv_d = dt_in("cv", (NL, 256, 256))
    cvec_d = dt_in("cvec", (128, KC, 2))
    n1g_d = dt_in("n1g", (128, NL, KC))
    n2g_d = dt_in("n2g", (128, NL, KC))
    bada_d = dt_in("bada", (128, NL, 96))
    qg_d = dt_in("qg", (128, NL))
    kg_d = dt_in("kg", (128, NL))
    sgn_d = dt_in("sgn", (128, NL, 8))
    bsrep_d = dt_in("bsrep", (128, NL, 8, 128))
    wsT_d = dt_in("wsT", (128, NL, 8, 128))
    ong_d = dt_in("ong", (128, NL, KC))
    wr_d = dt_in("wr", (128, KC, NE))
    brrep_d = dt_in("brrep", (128, NE))
    ident_d = dt_in("ident", (128, 128))
    rt_d = dt_in("rt", (128, 128))
    wada_d = dt_in("w_ada", (NL, D, 6 * D))
    win_d = dt_in("w_in", (NL, D, INW))
    wout_d = dt_in("w_out", (NL, D, D))
    fg_d = dt_in("ffn_w_gate", (1, D, DFF))
    fu_d = dt_in("ffn_w_up", (1, D, DFF))
    fd_d = dt_in("ffn_w_down", (1, DFF, D))
    mg_d = dt_in("moe_w_gate", (1, NE, D, DFE))
    mu_d = dt_in("moe_w_up", (1, NE, D, DFE))
    md_d = dt_in("moe_w_down", (1, NE, DFE, D))
    yp_d = dt_out("yp", (TT, D))
    ys_d = dt_out("ys", (2, TT, D))
    nk_d = dt_out("nk", (2, NL, 256, 256))
    nv_d = dt_out("nv", (2, NL, 256, 256))
    bIN = Buf("dram_in")

    with ExitStack() as st:
        P = Prog(nc, st)

        def sb(name, shape, dt, n=1, stack=st):
            return T(stack.enter_context(nc.sbuf_tensor("s_" + name, list(shape), dt)), n, name=name)

        bank = {}
        for nm in ("A0", "A1", "B0", "B1", "C", "Dk", "G", "H"):
            bank[nm] = T(st.enter_context(nc.psum_tensor("ps" + nm, [128, 512], F32)), 1, excl=True, name="ps" + nm)

        ident = sb("ident", (128, 128), F32)
        identb = sb("identb", (128, 128), BF16)
        rt = sb("rt", (128, 128), F32)
        onesb = sb("onesb", (128, 128), BF16)
        epsc = sb("epsc", (128, 1), F32)
        cvec = sb("cvec", (128, KC, 2), F32)
        scb = sb("scb", (128, KC, 2), BF16)
        n1g = sb("n1g", (128, NL, KC), F32)
        n2g = sb("n2g", (128, NL, KC), F32)
        bada = sb("bada", (128, NL, 96), F32)
        qg = sb("qg", (128, NL), F32)
        kg = sb("kg", (128, NL), F32)
        sgn = sb("sgn", (128, NL, 8), F32)
        bsrep = sb("bsrep", (128, 1, 8, 128), F32)
        wsT = sb("wsT", (128, NL, 8, 128), BF16)
        ong = sb("ong", (128, NL, KC), F32)
        wr = sb("wr", (128, KC, NE), F32)
        brrep = sb("brrep", (128, NE), F32)
        mod = sb("mod", (128, NL, 2, 96), F32)
        A1 = sb("A1", (128, NL, 2, KC), F32)
        A2 = sb("A2", (128, NL, 2, KC), F32)

        for (t_, d_) in ((ident, ident_d), (rt, rt_d), (cvec, cvec_d), (n1g, n1g_d), (n2g, n2g_d), (bada, bada_d),
                         (qg, qg_d), (kg, kg_d), (sgn, sgn_d), (ong, ong_d),
                         (wr, wr_d), (brrep, brrep_d)):
            P.dma("sp", t_.t[:], d_, reads=[bIN], writes=[t_.B])
        P.op("dve", lambda E: E.memset(onesb.t[:], 1.0), writes=[onesb.B])
        P.op("dve", lambda E: E.memset(epsc.t[:], EPS), writes=[epsc.B])
        P.op("dve", lambda E: E.tensor_copy(identb.t[:], ident.t[:]), reads=[ident.B], writes=[identb.B])
        P.dma("pool", wsT.t[:], wsT_d, reads=[bIN], writes=[wsT.B])
        P.op("act", lambda E: E.activation(out=scb.t[:], in_=cvec.t[:], func=AF.Silu), reads=[cvec.B], writes=[scb.B])

        NSLOT = 4
        wslots = [sb("wslot%d" % i, (128, 4096), BF16) for i in range(NSLOT)]
        wstate = {"plan": [], "issued": 0, "taken": 0, "released": 0}

        def w_view(i, shape):
            s_ = wslots[i % NSLOT]
            if shape[0] == "in":
                return s_.t[:, 0:KC * shape[1]].rearrange("p (k n) -> p k n", k=KC), s_.B
            return s_.t[:, 0:shape[1] * D].rearrange("p (k n) -> p k n", k=shape[1]), s_.B

        def w_issue_upto(n):
            while wstate["issued"] < min(n, len(wstate["plan"])):
                i = wstate["issued"]
                src, shape = wstate["plan"][i]
                dst, db = w_view(i, shape)
                P.dma("pool", dst, src.rearrange("(k p) n -> p k n", p=128), reads=[bIN], writes=[db])
                wstate["issued"] += 1

        def w_get(src, shape):
            i = wstate["taken"]
            wstate["taken"] += 1
            if P.dry:
                wstate["plan"].append((src, shape))
                return w_view(i, shape)
            assert i < len(wstate["plan"]) and wstate["plan"][i][1] == shape, "weight plan mismatch"
            w_issue_upto(max(wstate["released"] + NSLOT, i + 1))
            assert wstate["issued"] <= wstate["released"] + NSLOT and i - wstate["released"] < NSLOT
            return w_view(i, shape)

        def w_release():
            if P.dry:
                return
            wstate["released"] = wstate["taken"]
            w_issue_upto(wstate["released"] + NSLOT)

        def mm(out_bank, out_ap, lhsT, rhs, start, stop, reads):
            P.op("pe", lambda E: E.matmul(out_ap, lhsT=lhsT, rhs=rhs, start=start, stop=stop), reads=reads, writes=[out_bank.B])

        rr = {"act_dve": 0}

        modst = {"i": 0}

        def mod_step(n):
            for _ in range(n):
                i = modst["i"]
                if i >= NL * 48:
                    return
                modst["i"] = i + 1
                l, pc = divmod(i, 48)
                w, wb = w_get(wada_d[l, :, pc * 256:(pc + 1) * 256], ("in", 256))
                bk = bank["G"] if pc % 2 == 0 else bank["H"]
                for c2 in range(2):
                    for kc in range(KC):
                        mm(bk, bk.t[:, c2 * 2:c2 * 2 + 2], w[:, kc, c2 * 128:(c2 + 1) * 128], scb.t[:, kc, :],
                           kc == 0, kc == KC - 1, [wb, scb.B])
                w_release()
                for c2 in range(2):
                    j = pc * 2 + c2
                    P.op("dve", lambda E, l=l, j=j, c2=c2, bk=bk: E.tensor_scalar(
                        out=mod.t[:, l, :, j], in0=bk.t[:, c2 * 2:c2 * 2 + 2], scalar1=bada.t[:, l, j:j + 1], scalar2=None,
                        op0=ALU.add), reads=[bk.B, bada.B], writes=[mod.B])
                for (A, g_, off, last_pc) in ((A1, n1g, 16, 15), (A2, n2g, 64, 39)):
                    if pc == last_pc:
                        for cnd in range(2):
                            P.op("dve", lambda E, A=A, g_=g_, off=off, l=l, cnd=cnd: E.scalar_tensor_tensor(
                                out=A.t[:, l, cnd, :], in0=mod.t[:, l, cnd, off:off + 16], scalar=1.0, in1=g_.t[:, l, :],
                                op0=ALU.add, op1=ALU.mult), reads=[mod.B, g_.B], writes=[A.B])

        def need_mod(l, which):
            assert modst["i"] > l * 48 + which * 8 + 7, ("modulation not produced yet", l, which, modst["i"])

        bgc = {"hp": 0, "wo": 0, "ffn": 0}

        def mvec(l, cnd, which):
            need_mod(l, which)
            return mod.t[:, l, cnd, which * 16:(which + 1) * 16]

        xstage = sb("xstage", (128, 1024), F32, n=1)
        kvstage = sb("kvstage", (128, 4, 256), F32)
        xstg = [(xstage.t[:], xstage.B), (kvstage.t[:].rearrange("p b d -> p (b d)"), kvstage.B)]

        def load_x(src, x):
            for blk in range(4):
                for g4 in range(4):
                    st_ap, st_b = xstg[(blk * 2 + g4 // 2) % 2]
                    if g4 % 2 == 0:
                        P.dma("sp", st_ap, src[blk * 128:(blk + 1) * 128, (g4 // 2) * 1024:(g4 // 2 + 1) * 1024], reads=[bIN], writes=[st_b])
                    bk = bank["H"] if g4 % 2 == 0 else bank["G"]
                    for j in range(4):
                        kc = g4 * 4 + j
                        kl = kc % 8
                        P.op("pe", lambda E, bk=bk, j=j, kl=kl, st_ap=st_ap: E.transpose(bk.t[:, j * 128:(j + 1) * 128],
                                                                                        st_ap[:, kl * 128:(kl + 1) * 128], ident.t[:]),
                             reads=[st_b, ident.B], writes=[bk.B])
                    dst = x.t[:, g4 * 4:(g4 + 1) * 4, blk * 128:(blk + 1) * 128]
                    srcp = bk.t[:].rearrange("p (j t) -> p j t", j=4)
                    if g4 % 2 == 0:
                        P.op("dve", lambda E, dst=dst, srcp=srcp: E.tensor_copy(dst, srcp), reads=[bk.B], writes=[x.B])
                    else:
                        P.op("act", lambda E, dst=dst, srcp=srcp: E.activation(out=dst, in_=srcp, func=AF.Copy), reads=[bk.B], writes=[x.B])

        def store_x(x, dst):
            for blk in range(4):
                for g4 in range(4):
                    st_ap, st_b = xstg[(blk * 2 + g4 // 2) % 2]
                    bk = bank["H"] if g4 % 2 == 0 else bank["G"]
                    for j in range(4):
                        kc = g4 * 4 + j
                        P.op("pe", lambda E, bk=bk, j=j, kc=kc, blk=blk: E.transpose(
                            bk.t[:, j * 128:(j + 1) * 128], x.t[:, kc, blk * 128:(blk + 1) * 128], ident.t[:]),
                            reads=[x.B, ident.B], writes=[bk.B])
                    dsts = st_ap[:, (g4 % 2) * 512:(g4 % 2 + 1) * 512]
                    if g4 % 2 == 0:
                        P.op("dve", lambda E, dsts=dsts, bk=bk: E.tensor_copy(dsts, bk.t[:]), reads=[bk.B], writes=[st_b])
                    else:
                        P.op("act", lambda E, dsts=dsts, bk=bk: E.activation(out=dsts, in_=bk.t[:], func=AF.Copy), reads=[bk.B], writes=[st_b])
                        P.dma("sp", dst[blk * 128:(blk + 1) * 128, (g4 // 2) * 1024:(g4 // 2 + 1) * 1024], st_ap, reads=[st_b], writes=[Buf()])

        sqr = sb("sqr", (128, 3, TT), BF16, n=3)
        rstd = sb("rstd", (128, TT), F32)
        tmpf = sb("tmpf", (128, 2, TT), F32, n=2)

        def sum_sq_to_rstd(n_feat, dst):
            G = bank["G"]
            P.op("act", lambda E: E.activation(out=dst.t[:], in_=G.t[:], func=AF.Sqrt, bias=epsc.t[:, 0:1], scale=1.0 / n_feat),
                 reads=[G.B, epsc.B], writes=[dst.B])
            P.op("dve", lambda E: E.reciprocal(out=dst.t[:], in_=dst.t[:]), reads=[dst.B], writes=[dst.B])

        def Avec1(l, cnd):
            need_mod(l, 1)
            return A1.t[:, l, cnd, :]

        def Avec2(l, cnd):
            need_mod(l, 4)
            return A2.t[:, l, cnd, :]

        def norm_mod(x, h, Avec, Bvec):
            G = bank["G"]
            for kc in range(KC):
                i = kc % 2
                P.op("act", lambda E, kc=kc, i=i: E.activation(out=sqr.t[:, i, :], in_=x.t[:, kc, :], func=AF.Square),
                     reads=[x.B], writes=[sqr.b[i]])
                mm(G, G.t[:], onesb.t[:], sqr.t[:, i, :], kc == 0, kc == KC - 1, [onesb.B, sqr.b[i]])
            sum_sq_to_rstd(float(D), rstd)
            for kc in range(KC):
                i = kc % 2
                P.op("dve", lambda E, kc=kc, i=i: E.scalar_tensor_tensor(
                    out=tmpf.t[:, i, :], in0=x.t[:, kc, :], scalar=Avec[:, kc:kc + 1], in1=rstd.t[:], op0=ALU.mult, op1=ALU.mult),
                    reads=[x.B, rstd.B, A1.B, A2.B], writes=[tmpf.b[i]])
                P.op("act", lambda E, kc=kc, i=i: E.activation(out=h.t[:, kc, :], in_=tmpf.t[:, i, :], func=AF.Identity,
                                                              bias=Bvec[:, kc:kc + 1], scale=1.0),
                     reads=[tmpf.b[i], mod.B], writes=[h.B])

        hq = sb("hq", (128, TT), F32)
        hq1 = sb("hq1", (128, TT), F32)
        hq2 = [hq, hq1]
        hrs = sb("hrs", (128, TT), F32)
        r1 = sb("r1", (128, TT), F32)
        r2 = sb("r2", (128, TT), F32)
        qb = sb("qb", (128, TT), BF16)
        qb1 = sb("qb1", (128, TT), BF16)
        qb2 = [qb, qb1]
        ropec = sb("ropec", (128, TT), F32)
        ropes = sb("ropes", (128, TT), F32)

        def head_norm(ps, gain_ap, out_f32=None, out_bf=None):
            G = bank["G"]
            P.op("act", lambda E: E.activation(out=sqr.t[:, 0, :], in_=ps.t[:], func=AF.Square), reads=[ps.B], writes=[sqr.b[0]])
            mm(G, G.t[:], onesb.t[:], sqr.t[:, 0, :], True, True, [onesb.B, sqr.b[0]])
            sum_sq_to_rstd(128.0, hrs)
            if out_f32 is not None:
                P.op("dve", lambda E: E.scalar_tensor_tensor(out=out_f32.t[:], in0=ps.t[:], scalar=gain_ap, in1=hrs.t[:],
                                                             op0=ALU.mult, op1=ALU.mult), reads=[ps.B, hrs.B, qg.B, kg.B], writes=[out_f32.B])
            else:
                P.op("dve", lambda E: E.scalar_tensor_tensor(out=out_bf, in0=ps.t[:], scalar=gain_ap, in1=hrs.t[:],
                                                             op0=ALU.mult, op1=ALU.mult), reads=[ps.B, hrs.B, qg.B, kg.B], writes=[])

        def rope_to(src_f32, out_ap, out_buf):
            H = bank["H"]
            mm(H, H.t[:], rt.t[:], src_f32.t[:], True, True, [rt.B, src_f32.B])
            P.op("dve", lambda E: E.tensor_tensor(out=src_f32.t[:], in0=src_f32.t[:], in1=ropec.t[:], op=ALU.mult),
                 reads=[src_f32.B, ropec.B], writes=[src_f32.B])
            P.op("dve", lambda E: E.tensor_tensor(out=tmpf.t[:, 1, :], in0=H.t[:], in1=ropes.t[:], op=ALU.mult),
                 reads=[H.B, ropes.B], writes=[tmpf.b[1]])
            P.op("dve", lambda E: E.tensor_tensor(out=out_ap, in0=src_f32.t[:], in1=tmpf.t[:, 1, :], op=ALU.add),
                 reads=[src_f32.B, tmpf.b[1]], writes=[out_buf])


        def plan_kv(l):
            return [(win_d[l, :, 1024:1280], ("in", 256)), (win_d[l, :, 1280:1536], ("in", 256))]

        def emit_kv(l, h, KTb, Vb, kcol0, vblk0, rope, cache_out=None):
            wk, wkb = w_get(win_d[l, :, 1024:1280], ("in", 256))
            for kvh in range(2):
                bk = bank["A%d" % kvh]
                for kc in range(KC):
                    mm(bk, bk.t[:], wk[:, kc, kvh * 128:(kvh + 1) * 128], h.t[:, kc, :], kc == 0, kc == KC - 1, [wkb, h.B])
                dst = KTb.t[:, kvh, kcol0:kcol0 + TT]
                if rope:
                    head_norm(bk, kg.t[:, l:l + 1], out_f32=hq)
                    rope_to(hq, dst, KTb.B)
                else:
                    head_norm(bk, kg.t[:, l:l + 1], out_f32=hq)
                    P.op("act", lambda E, dst=dst: E.activation(out=dst, in_=hq.t[:], func=AF.Copy), reads=[hq.B], writes=[KTb.B])
                    if cache_out is not None:
                        H = bank["H"]
                        for blk in range(4):
                            P.op("pe", lambda E, blk=blk: E.transpose(H.t[:, blk * 128:(blk + 1) * 128], hq.t[:, blk * 128:(blk + 1) * 128], ident.t[:]),
                                 reads=[hq.B, ident.B], writes=[H.B])
                        P.op("dve", lambda E, kvh=kvh: E.tensor_copy(kvstage.t[:, :, kvh * 128:(kvh + 1) * 128],
                                                                     H.t[:].rearrange("p (b d) -> p b d", b=4)),
                             reads=[H.B], writes=[kvstage.B])
            if cache_out is not None:
                for blk in range(4):
                    P.dma("sp", nk_d[blk // 2, l, (blk % 2) * 128:(blk % 2 + 1) * 128, :], kvstage.t[:, blk, :],
                          reads=[kvstage.B], writes=[Buf()])
            w_release()
            wv, wvb = w_get(win_d[l, :, 1280:1536], ("in", 256))
            for blk in range(4):
                bk = bank["B%d" % (blk // 2)]
                o_ap = bk.t[:, (blk % 2) * 256:(blk % 2 + 1) * 256]
                for kc in range(KC):
                    mm(bk, o_ap, h.t[:, kc, blk * 128:(blk + 1) * 128], wv[:, kc, :], kc == 0, kc == KC - 1, [wvb, h.B])
                if blk % 2 == 1:
                    P.op("act", lambda E, bk=bk, blk=blk: E.activation(out=Vb.t[:, vblk0 + blk - 1:vblk0 + blk + 1, :],
                                                                      in_=bk.t[:].rearrange("p (b d) -> p b d", b=2), func=AF.Copy),
                         reads=[bk.B], writes=[Vb.B])
                    if cache_out is not None:
                        P.op("dve", lambda E, bk=bk, blk=blk: E.tensor_copy(kvstage.t[:, blk - 1:blk + 1, :],
                                                                            bk.t[:].rearrange("p (b d) -> p b d", b=2)),
                             reads=[bk.B], writes=[kvstage.B])
            w_release()
            if cache_out is not None:
                for blk in range(4):
                    P.dma("sp", nv_d[blk // 2, l, (blk % 2) * 128:(blk % 2 + 1) * 128, :], kvstage.t[:, blk, :],
                          reads=[kvstage.B], writes=[Buf()])

        PT = sb("PT", (128, 4, TT), BF16, n=4)
        uf = sb("uf", (128, TT), F32)
        ghat = sb("ghat", (128, 4, 256), BF16)
        gss = sb("gss", (128, 8), F32)
        gsq = sb("gsq", (128, 128), F32)
        ssa = sb("ssa", (128, TT), F32)
        sss = sb("sss", (128, TT), F32)
        SCALE = 1.0 / float(np.sqrt(128.0))

        def plan_mixer(l):
            items = []
            for hp in range(4):
                items.append((win_d[l, :, 2560 + hp * 256:2560 + (hp + 1) * 256], ("in", 256)))
                items.append((win_d[l, :, 1536 + hp * 256:1536 + (hp + 1) * 256], ("in", 256)))
                items.append((win_d[l, :, hp * 256:(hp + 1) * 256], ("in", 256)))
            for pc in range(8):
                items.append((wout_d[l, :, pc * 256:(pc + 1) * 256], ("in", 256)))
            return items

        deferred = []

        def flush():
            for f in deferred:
                f()
            del deferred[:]

        def accum_sumsq(src_f32_ap, src_buf, acc, first, si):
            G = bank["G"]
            if any(getattr(f, "si", None) == si for f in deferred):
                flush()
            P.op("act", lambda E: E.activation(out=sqr.t[:, si, :], in_=src_f32_ap, func=AF.Square), reads=[src_buf], writes=[sqr.b[si]])

            def part2():
                mm(G, G.t[:], onesb.t[:], sqr.t[:, si, :], True, True, [onesb.B, sqr.b[si]])
                if first:
                    P.op("dve", lambda E: E.tensor_copy(acc.t[:], G.t[:]), reads=[G.B], writes=[acc.B])
                else:
                    P.op("dve", lambda E: E.tensor_tensor(out=acc.t[:], in0=acc.t[:], in1=G.t[:], op=ALU.add), reads=[G.B, acc.B], writes=[acc.B])
            part2.si = si
            deferred.append(part2)

        def emit_mixer(l, cnd, x, h, o, KTb, Vb, groups, rope):
            P.dma("sp", bsrep.t[:, 0], bsrep_d[:, l], reads=[bIN], writes=[bsrep.B])
            for hp in range(4):
                c4 = hp
                wg_, wgb = w_get(win_d[l, :, 2560 + hp * 256:2560 + (hp + 1) * 256], ("in", 256))
                for blk in range(4):
                    bk = bank["A%d" % (blk % 2)]
                    o_ap = bk.t[:, 0:256]
                    for kc in range(KC):
                        mm(bk, o_ap, h.t[:, kc, blk * 128:(blk + 1) * 128], wg_[:, kc, :], kc == 0, kc == KC - 1, [wgb, h.B])
                    P.op("dve", lambda E, c4=c4: E.memset(gss.t[:, c4 * 2:c4 * 2 + 2], 0.0), reads=[gss.B], writes=[gss.B])
                    for hh in range(2):
                        hd = c4 * 2 + hh
                        P.op("act", lambda E, bk=bk, hh=hh, hd=hd: E.activation(out=gsq.t[:], in_=bk.t[:, hh * 128:(hh + 1) * 128], func=AF.Square,
                                                                               accum_out=gss.t[:, hd:hd + 1]),
                             reads=[bk.B], writes=[gsq.B, gss.B])
                    P.op("act", lambda E, c4=c4: E.activation(out=gss.t[:, c4 * 2:c4 * 2 + 2], in_=gss.t[:, c4 * 2:c4 * 2 + 2], func=AF.Sqrt,
                                                              bias=epsc.t[:, 0:1], scale=1.0 / 128.0), reads=[gss.B, epsc.B], writes=[gss.B])
                    P.op("dve", lambda E, c4=c4: E.reciprocal(out=gss.t[:, c4 * 2:c4 * 2 + 2], in_=gss.t[:, c4 * 2:c4 * 2 + 2]),
                         reads=[gss.B], writes=[gss.B])
                    for hh in range(2):
                        hd = c4 * 2 + hh
                        P.op("dve", lambda E, bk=bk, hh=hh, hd=hd, blk=blk: E.tensor_scalar(
                            out=ghat.t[:, blk, hh * 128:(hh + 1) * 128], in0=bk.t[:, hh * 128:(hh + 1) * 128],
                            scalar1=gss.t[:, hd:hd + 1], scalar2=None, op0=ALU.mult), reads=[bk.B, gss.B], writes=[ghat.B])
                w_release()
                flush()
                wu_, wub = w_get(win_d[l, :, 1536 + hp * 256:1536 + (hp + 1) * 256], ("in", 256))
                wq_, wqb = w_get(win_d[l, :, hp * 256:(hp + 1) * 256], ("in", 256))
                C, Dk = bank["C"], bank["Dk"]

                def sgu_head(hh):
                    hd = hp * 2 + hh
                    bkA = bank["A0"]
                    for kc in range(KC):
                        mm(bkA, bkA.t[:], wu_[:, kc, hh * 128:(hh + 1) * 128], h.t[:, kc, :], kc == 0, kc == KC - 1, [wub, h.B])
                    P.op("act", lambda E: E.activation(out=uf.t[:], in_=bkA.t[:], func=AF.Copy), reads=[bkA.B], writes=[uf.B])
                    bkB = bank["B%d" % hh]
                    for blk in range(4):
                        mm(bkB, bkB.t[:, blk * 128:(blk + 1) * 128], ghat.t[:, blk, hh * 128:(hh + 1) * 128], wsT.t[:, l, hd, :],
                           True, True, [ghat.B, wsT.B])
                    for blk in range(4):
                        P.op("dve", lambda E, blk=blk: E.scalar_tensor_tensor(
                            out=r1.t[:, blk * 128:(blk + 1) * 128], in0=bkB.t[:, blk * 128:(blk + 1) * 128], scalar=sgn.t[:, l, hd:hd + 1],
                            in1=bsrep.t[:, 0, hd, :], op0=ALU.mult, op1=ALU.add), reads=[bkB.B, sgn.B, bsrep.B], writes=[r1.B])
                    P.op("dve", lambda E: E.tensor_tensor(out=r2.t[:], in0=r1.t[:], in1=uf.t[:], op=ALU.mult), reads=[r1.B, uf.B], writes=[r2.B])
                    P.op("act", lambda E: E.activation(out=o.t[:, 8 + hd, :], in_=r2.t[:], func=AF.Copy, scale=ong.t[:, l, 8 + hd:9 + hd]),
                         reads=[r2.B, ong.B], writes=[o.B])
                    accum_sumsq(r2.t[:], r2.B, sss, hd == 0, 1)

                def q_proj_norm(hh):
                    bkQ = bank["A1"]
                    for kc in range(KC):
                        mm(bkQ, bkQ.t[:], wq_[:, kc, hh * 128:(hh + 1) * 128], h.t[:, kc, :], kc == 0, kc == KC - 1, [wqb, h.B])
                    head_norm(bkQ, qg.t[:, l:l + 1], out_f32=hq2[hh])

                def q_finish(hh):
                    if rope:
                        rope_to(hq2[hh], qb2[hh].t[:], qb2[hh].B)
                    else:
                        P.op("act", lambda E: E.activation(out=qb2[hh].t[:], in_=hq2[hh].t[:], func=AF.Copy), reads=[hq2[hh].B], writes=[qb2[hh].B])

                def attention(hh, hook=None):
                    hd = hp * 2 + hh
                    kvh = hd // 4
                    qbh = qb2[hh]
                    for gi, (q0, q1, kblocks) in enumerate(groups):
                        nkb = len(kblocks)

                        def score(ji, q0=q0, q1=q1, kblocks=kblocks):
                            j = kblocks[ji]
                            bS = bank["B%d" % (ji % 2)]
                            mm(bS, bS.t[:, q0:q1], KTb.t[:, kvh, j * 128:(j + 1) * 128], qbh.t[:, q0:q1], True, True, [KTb.B, qbh.B])
                            P.op("act", lambda E: E.activation(out=PT.t[:, ji % 4, q0:q1], in_=bS.t[:, q0:q1], func=AF.Exp, scale=SCALE),
                                 reads=[bS.B], writes=[PT.b[ji % 4]])
                        score(0)
                        for ji in range(nkb):
                            if ji + 1 < nkb:
                                score(ji + 1)
                            j = kblocks[ji]
                            mm(C, C.t[:, q0:q1], Vb.t[:, j, kvh * 128:(kvh + 1) * 128], PT.t[:, ji % 4, q0:q1], ji == 0, ji == nkb - 1,
                               [Vb.B, PT.b[ji % 4]])
                            mm(Dk, Dk.t[:, q0:q1], onesb.t[:], PT.t[:, ji % 4, q0:q1], ji == 0, ji == nkb - 1, [onesb.B, PT.b[ji % 4]])
                            if hook is not None and gi == 0 and ji == min(3, nkb - 1):
                                hook()
                    P.op("dve", lambda E: E.reciprocal(out=r1.t[:], in_=Dk.t[:]), reads=[Dk.B], writes=[r1.B])
                    P.op("dve", lambda E: E.tensor_tensor(out=r2.t[:], in0=C.t[:], in1=r1.t[:], op=ALU.mult), reads=[C.B, r1.B], writes=[r2.B])
                    P.op("act", lambda E: E.activation(out=o.t[:, hd, :], in_=r2.t[:], func=AF.Copy, scale=ong.t[:, l, hd:hd + 1]),
                         reads=[r2.B, ong.B], writes=[o.B])
                    accum_sumsq(r2.t[:], r2.B, ssa, hd == 0, 2)

                sgu_head(0)
                q_proj_norm(0)
                sgu_head(1)
                flush()
                q_finish(0)
                q_proj_norm(1)
                flush()
                attention(0, hook=lambda: q_finish(1))
                attention(1)
                w_release()
                mod_step(bgc["hp"])
            flush()
            rsa, rss = ssa, sss
            for (acc, dstr) in ((ssa, ssa), (sss, sss)):
                P.op("act", lambda E, acc=acc, dstr=dstr: E.activation(out=dstr.t[:], in_=acc.t[:], func=AF.Sqrt, bias=epsc.t[:, 0:1], scale=1.0 / 1024.0),
                     reads=[acc.B, epsc.B], writes=[dstr.B])
                P.op("dve", lambda E, dstr=dstr: E.reciprocal(out=dstr.t[:], in_=dstr.t[:]), reads=[dstr.B], writes=[dstr.B])
            g1 = mvec(l, cnd, 2)
            for pc in range(8):
                wo_, wob = w_get(wout_d[l, :, pc * 256:(pc + 1) * 256], ("in", 256))
                for c2 in range(2):
                    oc = pc * 2 + c2
                    bkA, bkB = bank["A%d" % c2], bank["B%d" % c2]
                    for kc in range(8):
                        mm(bkA, bkA.t[:], wo_[:, kc, c2 * 128:(c2 + 1) * 128], o.t[:, kc, :], kc == 0, kc == 7, [wob, o.B])
                    for kc in range(8, 16):
                        mm(bkB, bkB.t[:], wo_[:, kc, c2 * 128:(c2 + 1) * 128], o.t[:, kc, :], kc == 8, kc == 15, [wob, o.B])
                    if c2 == 1:
                        w_release()
                        mod_step(bgc["wo"])
                    P.op("dve", lambda E, bkA=bkA: E.tensor_tensor(out=r1.t[:], in0=bkA.t[:], in1=rsa.t[:], op=ALU.mult), reads=[bkA.B, rsa.B], writes=[r1.B])
                    P.op("dve", lambda E, bkB=bkB: E.tensor_tensor(out=r2.t[:], in0=bkB.t[:], in1=rss.t[:], op=ALU.mult), reads=[bkB.B, rss.B], writes=[r2.B])
                    P.op("dve", lambda E: E.tensor_tensor(out=r1.t[:], in0=r1.t[:], in1=r2.t[:], op=ALU.add), reads=[r1.B, r2.B], writes=[r1.B])
                    P.op("dve", lambda E, oc=oc: E.scalar_tensor_tensor(out=x.t[:, oc, :], in0=r1.t[:], scalar=g1[:, oc:oc + 1], in1=x.t[:, oc, :],
                                                                       op0=ALU.mult, op1=ALU.add), reads=[r1.B, mod.B, x.B], writes=[x.B])

        sg = sb("sg", (128, 2, TT), F32, n=2)
        actb = sb("actb", (128, 2, 4, TT), BF16, n=2)
        cwrep = sb("cwrep", (128, TT), F32)
        cw4 = sb("cw4", (128, 4, NE), F32)
        lg = sb("lg", (128, NE), F32)
        lg8 = sb("lg8", (128, 8), F32)
        cw = sb("cw", (128, NE), F32)
        cwb = sb("cwb", (128, 128), F32)
        tcol = sb("tcol", (128, 4), F32)
        wrp = sb("wrp", (128, KC, NE), F32)

        def ffn_panel_list(l):
            out = []
            if l == 0:
                for p in range(DFF // 256):
                    out.append((fg_d[0][:, p * 256:(p + 1) * 256], fu_d[0][:, p * 256:(p + 1) * 256], fd_d[0][p * 256:(p + 1) * 256, :], None))
            else:
                for e in range(NE):
                    for p in range(DFE // 256):
                        out.append((mg_d[0, e][:, p * 256:(p + 1) * 256], mu_d[0, e][:, p * 256:(p + 1) * 256],
                                    md_d[0, e][p * 256:(p + 1) * 256, :], e))
            assert len(out) % 2 == 0
            return out

        def plan_ffn(l):
            pl = ffn_panel_list(l)
            items = []
            for pp in range(len(pl) // 2):
                for half in range(2):
                    g_, u_, d_, e = pl[pp * 2 + half]
                    items.append((g_, ("in", 256)))
                    items.append((u_, ("in", 256)))
                for half in range(2):
                    items.append((pl[pp * 2 + half][2], ("rows", 2)))
            return items

        def emit_ffn_panels(l, x, h2, g2):
            pl = ffn_panel_list(l)
            cur_e = None
            for pp in range(len(pl) // 2):
                pi = pp % 2
                for half in range(2):
                    e = pl[pp * 2 + half][3]
                    if e is not None and e != cur_e:
                        emit_cwrep(e)
                        cur_e = e
                    wg_, wgb = w_get(pl[pp * 2 + half][0], ("in", 256))
                    for c2 in range(2):
                        bkG = bank["A%d" % c2]
                        for kc in range(KC):
                            mm(bkG, bkG.t[:], wg_[:, kc, c2 * 128:(c2 + 1) * 128], h2.t[:, kc, :], kc == 0, kc == KC - 1, [wgb, h2.B])
                    w_release()
                    wu_, wub = w_get(pl[pp * 2 + half][1], ("in", 256))
                    for c2 in range(2):
                        bkU = bank["B%d" % c2]
                        for kc in range(KC):
                            mm(bkU, bkU.t[:], wu_[:, kc, c2 * 128:(c2 + 1) * 128], h2.t[:, kc, :], kc == 0, kc == KC - 1, [wub, h2.B])
                    w_release()
                    for c2 in range(2):
                        bkG, bkU = bank["A%d" % c2], bank["B%d" % c2]
                        P.op("act", lambda E, bkG=bkG, c2=c2: E.activation(out=sg.t[:, c2, :], in_=bkG.t[:], func=AF.Silu), reads=[bkG.B], writes=[sg.b[c2]])
                        if e is not None:
                            P.op("dve", lambda E, c2=c2: E.tensor_tensor(out=sg.t[:, c2, :], in0=sg.t[:, c2, :], in1=cwrep.t[:], op=ALU.mult),
                                 reads=[sg.b[c2], cwrep.B], writes=[sg.b[c2]])
                        P.op("dve", lambda E, bkU=bkU, c2=c2, pi=pi, half=half: E.tensor_tensor(out=actb.t[:, pi, half * 2 + c2, :], in0=bkU.t[:], in1=sg.t[:, c2, :], op=ALU.mult),
                             reads=[bkU.B, sg.b[c2]], writes=[actb.b[pi]])
                wd0, wdb0 = w_get(pl[pp * 2][2], ("rows", 2))
                wd1, wdb1 = w_get(pl[pp * 2 + 1][2], ("rows", 2))
                for oc in range(KC):
                    bk = bank["C"] if oc % 2 == 0 else bank["Dk"]
                    for c4 in range(4):
                        wd_, wdb = (wd0, wdb0) if c4 < 2 else (wd1, wdb1)
                        mm(bk, bk.t[:], wd_[:, c4 % 2, oc * 128:(oc + 1) * 128], actb.t[:, pi, c4, :], c4 == 0, c4 == 3, [wdb, actb.b[pi]])
                    P.op("dve", lambda E, bk=bk, oc=oc: E.scalar_tensor_tensor(out=x.t[:, oc, :], in0=bk.t[:], scalar=g2[:, oc:oc + 1], in1=x.t[:, oc, :],
                                                                              op0=ALU.mult, op1=ALU.add), reads=[bk.B, mod.B, x.B], writes=[x.B])
                w_release()
                mod_step(bgc["ffn"])

        def emit_router(l, cnd, x):
            A2v = Avec2(l, cnd)
            sh2 = mvec(l, cnd, 3)
            for kc in range(KC):
                P.op("dve", lambda E, kc=kc: E.tensor_scalar(out=wrp.t[:, kc, :], in0=wr.t[:, kc, :], scalar1=A2v[:, kc:kc + 1], scalar2=None, op0=ALU.mult),
                     reads=[wr.B, A2.B], writes=[wrp.B])
            H, G = bank["H"], bank["G"]
            for kc in range(KC):
                P.op("dve", lambda E, kc=kc: E.tensor_scalar(out=gsq.t[:], in0=ident.t[:], scalar1=0.0, scalar2=sh2[:, kc:kc + 1], op0=ALU.mult, op1=ALU.add),
                     reads=[ident.B, mod.B, gsq.B], writes=[gsq.B])
                mm(G, G.t[:, 0:NE], gsq.t[:], wr.t[:, kc, :], kc == 0, kc == KC - 1, [gsq.B, wr.B])
            P.op("dve", lambda E: E.tensor_tensor(out=lg8.t[:], in0=G.t[:, 0:NE], in1=brrep.t[:], op=ALU.add), reads=[G.B, brrep.B], writes=[lg8.B])
            for blk in range(4):
                for kc in range(KC):
                    mm(H, H.t[:, 0:NE], x.t[:, kc, blk * 128:(blk + 1) * 128], wrp.t[:, kc, :], kc == 0, kc == KC - 1, [x.B, wrp.B])
                mm(G, G.t[:, 0:1], rstd.t[:, blk * 128:(blk + 1) * 128], ident.t[:, 0:1], True, True, [rstd.B, ident.B])
                P.op("dve", lambda E: E.tensor_copy(tcol.t[:, 0:1], G.t[:, 0:1]), reads=[G.B], writes=[tcol.B])
                P.op("dve", lambda E: E.scalar_tensor_tensor(out=lg.t[:], in0=H.t[:, 0:NE], scalar=tcol.t[:, 0:1], in1=lg8.t[:], op0=ALU.mult, op1=ALU.add),
                     reads=[H.B, tcol.B, lg8.B], writes=[lg.B])
                P.op("dve", lambda E: E.tensor_reduce(out=tcol.t[:, 1:2], in_=lg.t[:], axis=mybir.AxisListType.X, op=ALU.max), reads=[lg.B, tcol.B], writes=[tcol.B])
                P.op("dve", lambda E: E.tensor_scalar(out=cw.t[:], in0=lg.t[:], scalar1=tcol.t[:, 1:2], scalar2=-1e30, op0=ALU.is_ge, op1=ALU.mult),
                     reads=[lg.B, tcol.B], writes=[cw.B])
                P.op("dve", lambda E: E.tensor_tensor(out=cw.t[:], in0=cw.t[:], in1=lg.t[:], op=ALU.add), reads=[cw.B, lg.B], writes=[cw.B])
                P.op("dve", lambda E: E.tensor_reduce(out=tcol.t[:, 2:3], in_=cw.t[:], axis=mybir.AxisListType.X, op=ALU.max), reads=[cw.B, tcol.B], writes=[tcol.B])
                P.op("dve", lambda E: E.tensor_scalar(out=cw.t[:], in0=lg.t[:], scalar1=tcol.t[:, 2:3], scalar2=None, op0=ALU.is_ge), reads=[lg.B, tcol.B], writes=[cw.B])
                P.op("dve", lambda E: E.tensor_scalar(out=tcol.t[:, 3:4], in0=tcol.t[:, 1:2], scalar1=-1.0, scalar2=None, op0=ALU.mult), reads=[tcol.B], writes=[tcol.B])
                P.op("act", lambda E: E.activation(out=lg.t[:], in_=lg.t[:], func=AF.Exp, bias=tcol.t[:, 3:4], scale=1.0), reads=[lg.B, tcol.B], writes=[lg.B])
                P.op("dve", lambda E: E.tensor_tensor(out=cw.t[:], in0=cw.t[:], in1=lg.t[:], op=ALU.mult), reads=[cw.B, lg.B], writes=[cw.B])
                P.op("dve", lambda E: E.tensor_reduce(out=tcol.t[:, 0:1], in_=cw.t[:], axis=mybir.AxisListType.X, op=ALU.add), reads=[cw.B, tcol.B], writes=[tcol.B])
                P.op("dve", lambda E: E.reciprocal(out=tcol.t[:, 0:1], in_=tcol.t[:, 0:1]), reads=[tcol.B], writes=[tcol.B])
                P.op("dve", lambda E, blk=blk: E.tensor_scalar(out=cw4.t[:, blk, :], in0=cw.t[:], scalar1=tcol.t[:, 0:1], scalar2=None, op0=ALU.mult),
                     reads=[cw.B, tcol.B], writes=[cw4.B])

        def emit_cwrep(e):
            bk = bank["H"]
            for blk in range(4):
                P.op("dve", lambda E, blk=blk: E.tensor_scalar(out=cwb.t[:], in0=ident.t[:], scalar1=0.0, scalar2=cw4.t[:, blk, e:e + 1], op0=ALU.mult, op1=ALU.add),
                     reads=[ident.B, cw4.B, cwb.B], writes=[cwb.B])
                mm(bk, bk.t[:, blk * 128:(blk + 1) * 128], cwb.t[:], ident.t[:], True, True, [cwb.B, ident.B])
            P.op("act", lambda E: E.activation(out=cwrep.t[:], in_=bk.t[:], func=AF.Copy), reads=[bk.B], writes=[cwrep.B])

        def emit_ffn(l, cnd, x, h2):
            g2 = mvec(l, cnd, 5)
            norm_mod(x, h2, Avec2(l, cnd), mvec(l, cnd, 3))
            if l == 1:
                emit_router(l, cnd, x)
            emit_ffn_panels(l, x, h2, g2)

        xA = sb("xA", (128, KC, TT), F32)
        xpark = nc.dram_tensor("xpark", [2, 128, KC, TT], F32, kind="Internal").ap()
        bpark = [Buf("xpark0"), Buf("xpark1")]
        kvsend = nc.dram_tensor("kvsend", [128, 4096], BF16, kind="Internal").ap()
        kvrecv = nc.dram_tensor("kvrecv", [256, 4096], BF16, addr_space="Local", kind="Internal").ap()
        bsend, brecv = Buf("kvsend"), Buf("kvrecv")
        hb = sb("hb", (128, KC, TT), BF16)
        ob = sb("ob", (128, KC, TT), BF16)
        KT0 = sb("KT0", (128, 2, 2304), BF16)
        V0 = sb("V0", (128, 18, 256), BF16)
        KT1 = sb("KT1", (128, 2, 2304), BF16)
        V1 = sb("V1", (128, 18, 256), BF16)
        cstage = T(xstage.t[:, 0:512].rearrange("p (r f) -> p r f", r=2), 1)
        cstage.b = xstage.b
        cstage.B = xstage.B

        def load_rope(t):
            P.dma("sp", ropec.t[:], ropec_d[t], reads=[bIN], writes=[ropec.B])
            P.dma("sp", ropes.t[:], ropes_d[t], reads=[bIN], writes=[ropes.B])

        def load_cache(l, KTb, Vb):
            P.dma("sp", cstage.t, ck_d[l].rearrange("(r p) f -> p r f", p=128), reads=[bIN], writes=[cstage.B])
            H = bank["H"]
            for kvh in range(2):
                for r in range(2):
                    P.op("pe", lambda E, kvh=kvh, r=r: E.transpose(H.t[:, (kvh * 2 + r) * 128:(kvh * 2 + r + 1) * 128],
                                                                   cstage.t[:, r, kvh * 128:(kvh + 1) * 128], ident.t[:]),
                         reads=[cstage.B, ident.B], writes=[H.B])
            P.op("act", lambda E: E.activation(out=KTb.t[:, :, 0:256], in_=H.t[:].rearrange("p (k t) -> p k t", k=2), func=AF.Copy),
                 reads=[H.B], writes=[KTb.B])
            P.dma("pool", Vb.t[:, 0:2, :], cv_d[l].rearrange("(r p) f -> p r f", p=128), reads=[bIN], writes=[Vb.B])

        pgroups = [(0, 256, [0, 1]), (256, 512, [2, 3])]
        sgroups = [(0, 512, list(range(18)))]

        def schedule():
            bgc.update(hp=0, wo=0, ffn=0)
            mod_step(16)
            load_cache(0, KT0, V0)
            load_cache(1, KT1, V1)
            for t in range(4):
                load_x(xs_d[t], xA)
                load_rope(t)
                norm_mod(xA, hb, Avec1(0, 1), mvec(0, 1, 0))
                emit_kv(0, hb, KT0, V0, 256 + t * 512, 2 + t * 4, rope=True)
                mod_step(8)
            bgc.update(hp=1, wo=1, ffn=1)
            for t in (0, 1):
                load_x(xs_d[t], xA)
                load_rope(t)
                norm_mod(xA, hb, Avec1(0, 1), mvec(0, 1, 0))
                emit_mixer(0, 1, xA, hb, ob, KT0, V0, sgroups, rope=True)
                emit_ffn(0, 1, xA, hb)
                norm_mod(xA, hb, Avec1(1, 1), mvec(1, 1, 0))
                emit_kv(1, hb, KT1, V1, 256 + t * 512, 2 + t * 4, rope=True)
                P.dma("sp", xpark[t], xA.t[:], reads=[xA.B], writes=[bpark[t]])
            P.dma("sp", kvsend[:, 0:2048].rearrange("p (k n) -> p k n", k=2), KT1.t[:, :, 256:1280], reads=[KT1.B], writes=[bsend])
            P.dma("sp", kvsend[:, 2048:4096].rearrange("p (b d) -> p b d", b=8), V1.t[:, 2:10, :], reads=[V1.B], writes=[bsend])
            P.coll(kvsend, kvrecv, [[0, 1], [2, 3], [4, 5], [6, 7]], reads=[bsend], writes=[brecv])
            load_x(xp_d, xA)
            for l in range(NL):
                norm_mod(xA, hb, Avec1(l, 0), mvec(l, 0, 0))
                emit_kv(l, hb, KT0, V0, 0, 0, rope=False, cache_out=True)
                emit_mixer(l, 0, xA, hb, ob, KT0, V0, pgroups, rope=False)
                emit_ffn(l, 0, xA, hb)
            store_x(xA, yp_d)
            for r in range(2):
                P.dma("sp", KT1.t[:, :, 256 + r * 1024:1280 + r * 1024],
                      kvrecv[r * 128:(r + 1) * 128, 0:2048].rearrange("p (k n) -> p k n", k=2), reads=[brecv], writes=[KT1.B])
                P.dma("sp", V1.t[:, 2 + r * 8:10 + r * 8, :],
                      kvrecv[r * 128:(r + 1) * 128, 2048:4096].rearrange("p (b d) -> p b d", b=8), reads=[brecv], writes=[V1.B])
            for t in (0, 1):
                load_rope(t)
                P.dma("sp", xA.t[:], xpark[t], reads=[bpark[t]], writes=[xA.B])
                norm_mod(xA, hb, Avec1(1, 1), mvec(1, 1, 0))
                emit_mixer(1, 1, xA, hb, ob, KT1, V1, sgroups, rope=True)
                emit_ffn(1, 1, xA, hb)
                store_x(xA, ys_d[t])
            assert modst["i"] == NL * 48 and not deferred

        P.dry = True
        schedule()
        n_plan = wstate["taken"]
        wstate.update(issued=0, taken=0, released=0)
        modst["i"] = 0
        P.dry = False
        schedule()
        assert wstate["taken"] == n_plan == len(wstate["plan"]), (wstate["taken"], n_plan, len(wstate["plan"]))
        P.finish()
        build_program.stats = (P.n_ops, P.n_wait, nc.sbuf_bytes_remaining)
    return nc


def _rope_tables():
    L_ = 2048
    rows = (np.arange(L_) // 64).astype(np.float32)
    cols = (np.arange(L_) % 64).astype(np.float32)
    inv = (10000.0 ** (-np.arange(0, 64, 2, dtype=np.float32) / 64.0)).astype(np.float32)
    ar = rows[:, None] * inv[None, :]
    ac = cols[:, None] * inv[None, :]
    ang = np.concatenate([ar, ar, ac, ac], axis=1)
    return np.cos(ang).astype(np.float32), np.sin(ang).astype(np.float32)


def _rt_matrix():
    rt = np.zeros((128, 128), np.float32)
    for base in (0, 64):
        for i in range(32):
            m = base + i
            rt[m + 32, m] = -1.0
            rt[m, m + 32] = 1.0
    return rt


def _fm(v):
    v = np.asarray(v, np.float32)
    lead = v.shape[:-1]
    return np.ascontiguousarray(np.moveaxis(v.reshape(lead + (KC, 128)), -1, 0))


_NC_CACHE = {}


def kernel(x_prompt, x_sample, cache_k, cache_v, c, c_ctx, w_ada, b_ada, norm1_g, norm2_g,
           w_in, q_norm_g, k_norm_g, sgu_norm_g, w_spatial, b_spatial, out_norm_g, w_out,
           ffn_w_gate, ffn_w_up, ffn_w_down, w_router, b_router, moe_w_gate, moe_w_up, moe_w_down):
    f32 = lambda a: np.ascontiguousarray(np.asarray(a, dtype=np.float32))
    x_prompt, x_sample, cache_k, cache_v = f32(x_prompt), f32(x_sample), f32(cache_k), f32(cache_v)
    c, c_ctx = f32(c), f32(c_ctx)
    if "nc" not in _NC_CACHE:
        _NC_CACHE["nc"] = build_program()
    nc = _NC_CACHE["nc"]
    in_maps = _prep(x_prompt, x_sample, cache_k, cache_v, c, c_ctx, w_ada, b_ada, norm1_g, norm2_g,
                    w_in, q_norm_g, k_norm_g, sgu_norm_g, w_spatial, b_spatial, out_norm_g, w_out,
                    ffn_w_gate, ffn_w_up, ffn_w_down, w_router, b_router, moe_w_gate, moe_w_up, moe_w_down)
    res = run_bass_kernel_spmd(nc, in_maps, core_ids=list(range(NCORES)))
    return _assemble(res.results)


def _prep(x_prompt, x_sample, cache_k, cache_v, c, c_ctx, w_ada, b_ada, norm1_g, norm2_g,
          w_in, q_norm_g, k_norm_g, sgu_norm_g, w_spatial, b_spatial, out_norm_g, w_out,
          ffn_w_gate, ffn_w_up, ffn_w_down, w_router, b_router, moe_w_gate, moe_w_up, moe_w_down):
    f32 = lambda a: np.ascontiguousarray(np.asarray(a, dtype=np.float32))

    cos, sin = _rope_tables()
    shared = {
        "n1g": _fm(norm1_g), "n2g": _fm(norm2_g),
        "bada": np.ascontiguousarray(np.moveaxis(f32(b_ada).reshape(NL, 96, 128), -1, 0)),
        "qg": np.ascontiguousarray(f32(q_norm_g).T), "kg": np.ascontiguousarray(f32(k_norm_g).T),
        "sgn": np.ascontiguousarray(np.moveaxis(f32(sgu_norm_g), -1, 0)),
        "bsrep": np.ascontiguousarray(np.broadcast_to(f32(b_spatial)[None], (128, NL, 8, 128))),
        "wsT": np.ascontiguousarray(np.transpose(f32(w_spatial), (3, 0, 1, 2))),
        "ong": _fm(out_norm_g),
        "wr": np.ascontiguousarray(np.transpose(f32(w_router)[0].reshape(KC, 128, NE), (1, 0, 2))),
        "brrep": np.ascontiguousarray(np.broadcast_to(f32(b_router)[0][None], (128, NE))),
        "ident": np.eye(128, dtype=np.float32), "rt": _rt_matrix(),
        "w_ada": f32(w_ada), "w_in": f32(w_in), "w_out": f32(w_out),
        "ffn_w_gate": f32(ffn_w_gate), "ffn_w_up": f32(ffn_w_up), "ffn_w_down": f32(ffn_w_down),
        "moe_w_gate": f32(moe_w_gate), "moe_w_up": f32(moe_w_up), "moe_w_down": f32(moe_w_down),
    }
    in_maps = []
    for core in range(NCORES):
        b, half = core // 2, core % 2
        own = x_sample[b, half * 1024:(half + 1) * 1024].reshape(2, TT, D)
        oth = x_sample[b, (1 - half) * 1024:(2 - half) * 1024].reshape(2, TT, D)
        pos = np.concatenate([np.arange(half * 1024, (half + 1) * 1024), np.arange((1 - half) * 1024, (2 - half) * 1024)])
        m = dict(shared)
        m["xs"] = np.ascontiguousarray(np.concatenate([own, oth], axis=0))
        m["xp"] = np.ascontiguousarray(x_prompt[2 * core:2 * core + 2].reshape(TT, D))
        m["ropec"] = np.ascontiguousarray(cos[pos].reshape(4, TT, 128).transpose(0, 2, 1))
        m["ropes"] = np.ascontiguousarray(sin[pos].reshape(4, TT, 128).transpose(0, 2, 1))
        m["ck"] = np.ascontiguousarray(cache_k[b].reshape(NL, 256, 256))
        m["cv"] = np.ascontiguousarray(cache_v[b].reshape(NL, 256, 256))
        cv2 = np.stack([c_ctx, c[b]], axis=-1)
        m["cvec"] = np.ascontiguousarray(cv2.reshape(KC, 128, 2).transpose(1, 0, 2))
        in_maps.append(m)
    return in_maps


def _assemble(R):
    y_prompt = np.empty((16, 256, D), np.float32)
    y_sample = np.empty((4, 2048, D), np.float32)
    nk = np.empty((16, NL, 256, 2, 128), np.float32)
    nv = np.empty((16, NL, 256, 2, 128), np.float32)
    for core in range(NCORES):
        b, half = core // 2, core % 2
        r = R[core]
        y_prompt[2 * core:2 * core + 2] = np.asarray(r["yp"]).reshape(2, 256, D)
        y_sample[b, half * 1024:(half + 1) * 1024] = np.asarray(r["ys"]).reshape(1024, D)
        nk[2 * core:2 * core + 2] = np.asarray(r["nk"]).reshape(2, NL, 256, 2, 128)
        nv[2 * core:2 * core + 2] = np.asarray(r["nv"]).reshape(2, NL, 256, 2, 128)
    return (y_prompt, y_sample, nk, nv)
```

```python
import numpy as np
from contextlib import ExitStack
import concourse.bass as bass
import concourse.mybir as mybir
from concourse.bass_utils import run_bass_kernel_spmd

F32 = mybir.dt.float32
BF16 = mybir.dt.bfloat16
ALU = mybir.AluOpType
AF = mybir.ActivationFunctionType

ENGS = ("pe", "act", "dve", "pool", "sp")

D = 2048
KC = 16
TT = 512
NL = 2
INW = 3584
DFF = 5632
NE = 8
DFE = 2816
EPS = 1e-6
NCORES = 8


class Buf:
    __slots__ = ("name", "w", "r", "excl")

    def __init__(self, name="", excl=False):
        self.name = name
        self.excl = excl
        self.w = None
        self.r = []


class Prog:
    NDMA = 6

    def __init__(self, nc, stack):
        self.nc = nc
        self.stack = stack
        self.ops = {e: [] for e in ENGS}
        self.sem = {}
        self.cnt = {}
        self.seen = {e: {} for e in ENGS}
        for e in ENGS:
            self._mk("c_" + e)
        self.dma_rr = {}
        for q in ("sp", "pool"):
            for i in range(self.NDMA):
                self._mk("d_%s_%d" % (q, i))
            self.dma_rr[q] = 0
        self._mk("d_cc")
        self.n_wait = 0
        self.n_ops = 0
        self.dry = False

    def _mk(self, key):
        self.sem[key] = self.stack.enter_context(self.nc.semaphore(key))
        self.cnt[key] = 0

    def _deps(self, reads, writes):
        d = {}

        def add(ev):
            if ev is None:
                return
            k, v = ev
            if d.get(k, 0) < v:
                d[k] = v
        for b in reads:
            add(b.w)
        for b in writes:
            add(b.w)
            for ev in b.r:
                add(ev)
        return d

    def _emit_waits(self, eng, deps):
        seen = self.seen[eng]
        for k, v in deps.items():
            if eng == "pe" and k == "c_pe":
                continue
            if seen.get(k, 0) >= v:
                continue
            seen[k] = v
            sem = self.sem[k]
            self.ops[eng].append(lambda E, sem=sem, v=v: E.wait_ge(sem, v))
            self.n_wait += 1

    def _record(self, ev, reads, writes):
        for b in reads:
            b.r.append(ev)
            if len(b.r) > 48:
                m = {}
                for k, v in b.r:
                    if m.get(k, 0) < v:
                        m[k] = v
                b.r = list(m.items())
        for b in writes:
            b.w = ev
            b.r = []

    def op(self, eng, fn, reads=(), writes=()):
        if self.dry:
            return
        if any(b.excl for b in reads):
            writes = list(writes) + [b for b in reads if b.excl]
            reads = [b for b in reads if not b.excl]
        deps = self._deps(reads, writes)
        self._emit_waits(eng, deps)
        key = "c_" + eng
        self.cnt[key] += 1
        v = self.cnt[key]
        sem = self.sem[key]
        self.ops[eng].append(lambda E, fn=fn, sem=sem: fn(E).then_inc(sem, 1))
        self._record((key, v), reads, writes)
        self.n_ops += 1

    def dma(self, q, out, in_, reads=(), writes=()):
        if self.dry:
            return
        deps = self._deps(reads, writes)
        i = self.dma_rr[q]
        self.dma_rr[q] = (i + 1) % self.NDMA
        key = "d_%s_%d" % (q, i)
        if self.cnt[key] > 0 and deps.get(key, 0) < self.cnt[key]:
            deps[key] = self.cnt[key]
        self._emit_waits(q, deps)
        self.cnt[key] += 16
        v = self.cnt[key]
        sem = self.sem[key]
        self.ops[q].append(lambda E, out=out, in_=in_, sem=sem: E.dma_start(out=out, in_=in_).then_inc(sem, 16))
        self._record((key, v), reads, writes)
        self.n_ops += 1

    def coll(self, in_ap, out_ap, groups, reads=(), writes=()):
        if self.dry:
            return
        deps = self._deps(reads, writes)
        self._emit_waits("pool", deps)
        key = "d_cc"
        self.cnt[key] += 1
        v = self.cnt[key]
        sem = self.sem[key]
        self.ops["pool"].append(lambda E: E.collective_compute(
            "AllGather", ALU.bypass, replica_groups=groups, ins=[in_ap], outs=[out_ap]).then_inc(sem, 1))
        self._record((key, v), reads, writes)
        self.n_ops += 1

    def finish(self):
        deps = {k: v for k, v in self.cnt.items() if k.startswith("d_") and v > 0}
        self._emit_waits("sp", deps)
        ops = self.ops
        with self.nc.Block() as block:
            @block.tensor
            def _(E):
                for f in ops["pe"]:
                    f(E)

            @block.scalar
            def _(E):
                for f in ops["act"]:
                    f(E)

            @block.vector
            def _(E):
                for f in ops["dve"]:
                    f(E)

            @block.gpsimd
            def _(E):
                for f in ops["pool"]:
                    f(E)

            @block.sync
            def _(E):
                for f in ops["sp"]:
                    f(E)


class T:
    def __init__(self, t, n=1, excl=False, name=""):
        self.t = t
        self.b = [Buf(name + str(i), excl) for i in range(n)]
        self.B = self.b[0]


def build_program(debug=0):
    nc = bass.Bass("TRN2", target_bir_lowering=False, num_devices=NCORES)
    dt_in = lambda name, shape: nc.dram_tensor(name, list(shape), F32, kind="ExternalInput").ap()
    dt_out = lambda name, shape: nc.dram_tensor(name, list(shape), F32, kind="ExternalOutput").ap()
    xs_d = dt_in("xs", (4, TT, D))
    xp_d = dt_in("xp", (TT, D))
    ropec_d = dt_in("ropec", (4, 128, TT))
    ropes_d = dt_in("ropes", (4, 128, TT))
    ck_d = dt_in("ck", (NL, 256, 256))
    cv_d = dt_in("cv", (NL, 256, 256))
    cvec_d = dt_in("cvec", (128, KC, 2))
    n1g_d = dt_in("n1g", (128, NL, KC))
    n2g_d = dt_in("n2g", (128, NL, KC))
    bada_d = dt_in("bada", (128, NL, 96))
    qg_d = dt_in("qg", (128, NL))
    kg_d = dt_in("kg", (128, NL))
    sgn_d = dt_in("sgn", (128, NL, 8))
    bsrep_d = dt_in("bsrep", (128, NL, 8, 128))
    wsT_d = dt_in("wsT", (128, NL, 8, 128))
    ong_d = dt_in("ong", (128, NL, KC))
    wr_d = dt_in("wr", (128, KC, NE))
    brrep_d = dt_in("brrep", (128, NE))
    ident_d = dt_in("ident", (128, 128))
    rt_d = dt_in("rt", (128, 128))
    wada_d = dt_in("w_ada", (NL, D, 3 * D))
    win_d = dt_in("w_in", (NL, D, INW))
    wout_d = dt_in("w_out", (NL, D, D))
    fg_d = dt_in("ffn_w_gate", (1, D, DFF))
    fu_d = dt_in("ffn_w_up", (1, D, DFF))
    fd_d = dt_in("ffn_w_down", (1, DFF, D))
    mg_d = dt_in("moe_w_gate", (1, NE, D, DFE))
    mu_d = dt_in("moe_w_up", (1, NE, D, DFE))
    md_d = dt_in("moe_w_down", (1, NE, DFE, D))
    yp_d = dt_out("yp", (TT, D))
    ys_d = dt_out("ys", (2, TT, D))
    nk_d = dt_out("nk", (2, NL, 256, 256))
    nv_d = dt_out("nv", (2, NL, 256, 256))
    bIN = Buf("dram_in")

    with ExitStack() as st:
        P = Prog(nc, st)

        def sb(name, shape, dt, n=1, stack=st):
            return T(stack.enter_context(nc.sbuf_tensor("s_" + name, list(shape), dt)), n, name=name)

        bank = {}
        for nm in ("A0", "A1", "B0", "B1", "C", "Dk", "G", "H"):
            bank[nm] = T(st.enter_context(nc.psum_tensor("ps" + nm, [128, 512], F32)), 1, excl=True, name="ps" + nm)

        ident = sb("ident", (128, 128), F32)
        identb = sb("identb", (128, 128), BF16)
        rt = sb("rt", (128, 128), F32)
        onesb = sb("onesb", (128, 128), BF16)
        epsc = sb("epsc", (128, 1), F32)
        cvec = sb("cvec", (128, KC, 2), F32)
        scb = sb("scb", (128, KC, 2), BF16)
        modraw = sb("modraw", (128, 48, 2), F32)
        modg = sb("modg", (128, 2, 96), F32)
        n1g = sb("n1g", (128, NL, KC), F32)
        n2g = sb("n2g", (128, NL, KC), F32)
        bada = sb("bada", (128, NL, 96), F32)
        qg = sb("qg", (128, NL), F32)
        kg = sb("kg", (128, NL), F32)
        sgn = sb("sgn", (128, NL, 8), F32)
        bsrep = sb("bsrep", (128, 1, 8, 128), F32)
        wsT = sb("wsT", (128, NL, 8, 128), BF16)
        ong = sb("ong", (128, NL, KC), F32)
        wr = sb("wr", (128, KC, NE), F32)
        brrep = sb("brrep", (128, NE), F32)
        mod = sb("mod", (128, NL, 2, 96), F32)
        A1 = sb("A1", (128, NL, 2, KC), F32)
        A2 = sb("A2", (128, NL, 2, KC), F32)

        for (t_, d_) in ((ident, ident_d), (rt, rt_d), (cvec, cvec_d), (n1g, n1g_d), (n2g, n2g_d), (bada, bada_d),
                         (qg, qg_d), (kg, kg_d), (sgn, sgn_d), (ong, ong_d),
                         (wr, wr_d), (brrep, brrep_d)):
            P.dma("sp", t_.t[:], d_, reads=[bIN], writes=[t_.B])
        P.op("dve", lambda E: E.memset(onesb.t[:], 1.0), writes=[onesb.B])
        P.op("dve", lambda E: E.memset(epsc.t[:], EPS), writes=[epsc.B])
        P.op("dve", lambda E: E.tensor_copy(identb.t[:], ident.t[:]), reads=[ident.B], writes=[identb.B])
        P.dma("pool", wsT.t[:], wsT_d, reads=[bIN], writes=[wsT.B])
        P.op("act", lambda E: E.activation(out=scb.t[:], in_=cvec.t[:], func=AF.Silu), reads=[cvec.B], writes=[scb.B])

        NSLOT = 4
        wslots = [sb("wslot%d" % i, (128, 4096), BF16) for i in range(NSLOT)]
        wstate = {"plan": [], "issued": 0, "taken": 0, "released": 0}

        def w_view(i, shape):
            s_ = wslots[i % NSLOT]
            if shape[0] == "in":
                return s_.t[:, 0:KC * shape[1]].rearrange("p (k n) -> p k n", k=KC), s_.B
            return s_.t[:, 0:shape[1] * D].rearrange("p (k n) -> p k n", k=shape[1]), s_.B

        def w_issue_upto(n):
            while wstate["issued"] < min(n, len(wstate["plan"])):
                i = wstate["issued"]
                src, shape = wstate["plan"][i]
                dst, db = w_view(i, shape)
                P.dma("pool", dst, src.rearrange("(k p) n -> p k n", p=128), reads=[bIN], writes=[db])
                wstate["issued"] += 1

        def w_get(src, shape):
            i = wstate["taken"]
            wstate["taken"] += 1
            if P.dry:
                wstate["plan"].append((src, shape))
                return w_view(i, shape)
            assert i < len(wstate["plan"]) and wstate["plan"][i][1] == shape, "weight plan mismatch"
            w_issue_upto(max(wstate["released"] + NSLOT, i + 1))
            assert wstate["issued"] <= wstate["released"] + NSLOT and i - wstate["released"] < NSLOT
            return w_view(i, shape)

        def w_release():
            if P.dry:
                return
            wstate["released"] = wstate["taken"]
            w_issue_upto(wstate["released"] + NSLOT)

        def mm(out_bank, out_ap, lhsT, rhs, start, stop, reads):
            P.op("pe", lambda E: E.matmul(out_ap, lhsT=lhsT, rhs=rhs, start=start, stop=stop), reads=reads, writes=[out_bank.B])

        rr = {"act_dve": 0}

        modst = {"i": 0, "rounds": 0}
        ROUNDS = [(0, 8), (8, 24), (24, 48)]
        PAIRS = [[0, 1], [2, 3], [4, 5], [6, 7]]

        def mod_exchange(ri):
            lo, hi = ROUNDS[ri]
            n = (hi - lo) * 4
            l = lo // 24
            q0, q1 = lo % 24, (hi - 1) % 24 + 1
            P.dma("sp", msend[ri], modraw.t[:, 0:(hi - lo) * 2, :].rearrange("p m k -> p (m k)"), reads=[modraw.B], writes=[bmsend[ri]])
            P.coll(msend[ri], mrecv[ri], PAIRS, reads=[bmsend[ri]], writes=[bmrecv[ri]])
            P.dma("sp", modg.t[:, :, 0:n], mrecv[ri].rearrange("(r p) f -> p r f", p=128), reads=[bmrecv[ri]], writes=[modg.B])
            for r in range(2):
                for cnd in range(2):
                    o_ = mod.t[:, l, cnd, :].rearrange("p (q r c) -> p q r c", q=24, r=2)[:, q0:q1, r, :]
                    b_ = bada.t[:, l, :].rearrange("p (q r c) -> p q r c", q=24, r=2)[:, q0:q1, r, :]
                    i_ = modg.t[:, r, 0:n].rearrange("p (q c k) -> p q c k", c=2, k=2)[:, :, :, cnd]
                    P.op("dve", lambda E, o_=o_, i_=i_, b_=b_: E.tensor_tensor(out=o_, in0=i_, in1=b_, op=ALU.add),
                         reads=[modg.B, bada.B], writes=[mod.B])
            for (A, g_, off, rr_) in ((A1, n1g, 16, (0, 2)), (A2, n2g, 64, (1, 2))):
                if ri in rr_:
                    for cnd in range(2):
                        P.op("dve", lambda E, A=A, g_=g_, off=off, l=l, cnd=cnd: E.scalar_tensor_tensor(
                            out=A.t[:, l, cnd, :], in0=mod.t[:, l, cnd, off:off + 16], scalar=1.0, in1=g_.t[:, l, :],
                            op0=ALU.add, op1=ALU.mult), reads=[mod.B, g_.B], writes=[A.B])
            modst["rounds"] = ri + 1

        def mod_step(n):
            for _ in range(n):
                i = modst["i"]
                if i >= 48:
                    return
                modst["i"] = i + 1
                l, q = divmod(i, 24)
                ri = [k for k, (lo, hi) in enumerate(ROUNDS) if lo <= i < hi][0]
                mi = i - ROUNDS[ri][0]
                w, wb = w_get(wada_d[l, :, q * 256:(q + 1) * 256], ("in", 256))
                bk = bank["G"] if i % 2 == 0 else bank["H"]
                for c2 in range(2):
                    for kc in range(KC):
                        mm(bk, bk.t[:, c2 * 2:c2 * 2 + 2], w[:, kc, c2 * 128:(c2 + 1) * 128], scb.t[:, kc, :],
                           kc == 0, kc == KC - 1, [wb, scb.B])
                w_release()
                P.op("dve", lambda E, mi=mi, bk=bk: E.tensor_copy(modraw.t[:, mi * 2:mi * 2 + 2, :],
                                                                 bk.t[:, 0:4].rearrange("p (c k) -> p c k", c=2)),
                     reads=[bk.B], writes=[modraw.B])
                if i + 1 == ROUNDS[ri][1]:
                    mod_exchange(ri)

        def need_mod(l, which):
            req = 3 if l == 1 else (1 if which < 2 else 2)
            assert modst["rounds"] >= req, ("modulation not exchanged yet", l, which, modst)

        bgc = {"hp": 0, "wo": 0, "ffn": 0}

        def mvec(l, cnd, which):
            need_mod(l, which)
            return mod.t[:, l, cnd, which * 16:(which + 1) * 16]

        xstage = sb("xstage", (128, 1024), F32, n=1)
        kvstage = sb("kvstage", (128, 4, 256), F32)
        xstg = [(xstage.t[:], xstage.B), (kvstage.t[:].rearrange("p b d -> p (b d)"), kvstage.B)]

        def load_x(src, x):
            for blk in range(4):
                for g4 in range(4):
                    st_ap, st_b = xstg[(blk * 2 + g4 // 2) % 2]
                    if g4 % 2 == 0:
                        P.dma("sp", st_ap, src[blk * 128:(blk + 1) * 128, (g4 // 2) * 1024:(g4 // 2 + 1) * 1024], reads=[bIN], writes=[st_b])
                    bk = bank["H"] if g4 % 2 == 0 else bank["G"]
                    for j in range(4):
                        kc = g4 * 4 + j
                        kl = kc % 8
                        P.op("pe", lambda E, bk=bk, j=j, kl=kl, st_ap=st_ap: E.transpose(bk.t[:, j * 128:(j + 1) * 128],
                                                                                        st_ap[:, kl * 128:(kl + 1) * 128], ident.t[:]),
                             reads=[st_b, ident.B], writes=[bk.B])
                    dst = x.t[:, g4 * 4:(g4 + 1) * 4, blk * 128:(blk + 1) * 128]
                    srcp = bk.t[:].rearrange("p (j t) -> p j t", j=4)
                    if g4 % 2 == 0:
                        P.op("dve", lambda E, dst=dst, srcp=srcp: E.tensor_copy(dst, srcp), reads=[bk.B], writes=[x.B])
                    else:
                        P.op("act", lambda E, dst=dst, srcp=srcp: E.activation(out=dst, in_=srcp, func=AF.Copy), reads=[bk.B], writes=[x.B])

        def store_x(x, dst):
            for blk in range(4):
                for g4 in range(4):
                    st_ap, st_b = xstg[(blk * 2 + g4 // 2) % 2]
                    bk = bank["H"] if g4 % 2 == 0 else bank["G"]
                    for j in range(4):
                        kc = g4 * 4 + j
                        P.op("pe", lambda E, bk=bk, j=j, kc=kc, blk=blk: E.transpose(
                            bk.t[:, j * 128:(j + 1) * 128], x.t[:, kc, blk * 128:(blk + 1) * 128], ident.t[:]),
                            reads=[x.B, ident.B], writes=[bk.B])
                    dsts = st_ap[:, (g4 % 2) * 512:(g4 % 2 + 1) * 512]
                    if g4 % 2 == 0:
                        P.op("dve", lambda E, dsts=dsts, bk=bk: E.tensor_copy(dsts, bk.t[:]), reads=[bk.B], writes=[st_b])
                    else:
                        P.op("act", lambda E, dsts=dsts, bk=bk: E.activation(out=dsts, in_=bk.t[:], func=AF.Copy), reads=[bk.B], writes=[st_b])
                        P.dma("sp", dst[blk * 128:(blk + 1) * 128, (g4 // 2) * 1024:(g4 // 2 + 1) * 1024], st_ap, reads=[st_b], writes=[Buf()])

        sqr = sb("sqr", (128, 3, TT), BF16, n=3)
        rstd = sb("rstd", (128, TT), F32)
        tmpf = sb("tmpf", (128, 2, TT), F32, n=2)

        def sum_sq_to_rstd(n_feat, dst):
            G = bank["G"]
            P.op("act", lambda E: E.activation(out=dst.t[:], in_=G.t[:], func=AF.Sqrt, bias=epsc.t[:, 0:1], scale=1.0 / n_feat),
                 reads=[G.B, epsc.B], writes=[dst.B])
            P.op("dve", lambda E: E.reciprocal(out=dst.t[:], in_=dst.t[:]), reads=[dst.B], writes=[dst.B])

        def Avec1(l, cnd):
            need_mod(l, 1)
            return A1.t[:, l, cnd, :]

        def Avec2(l, cnd):
            need_mod(l, 4)
            return A2.t[:, l, cnd, :]

        def norm_mod(x, h, Avec, Bvec):
            G = bank["G"]
            for kc in range(KC):
                i = kc % 2
                P.op("act", lambda E, kc=kc, i=i: E.activation(out=sqr.t[:, i, :], in_=x.t[:, kc, :], func=AF.Square),
                     reads=[x.B], writes=[sqr.b[i]])
                mm(G, G.t[:], onesb.t[:], sqr.t[:, i, :], kc == 0, kc == KC - 1, [onesb.B, sqr.b[i]])
            sum_sq_to_rstd(float(D), rstd)
            for kc in range(KC):
                i = kc % 2
                P.op("dve", lambda E, kc=kc, i=i: E.scalar_tensor_tensor(
                    out=tmpf.t[:, i, :], in0=x.t[:, kc, :], scalar=Avec[:, kc:kc + 1], in1=rstd.t[:], op0=ALU.mult, op1=ALU.mult),
                    reads=[x.B, rstd.B, A1.B, A2.B], writes=[tmpf.b[i]])
                P.op("act", lambda E, kc=kc, i=i: E.activation(out=h.t[:, kc, :], in_=tmpf.t[:, i, :], func=AF.Identity,
                                                              bias=Bvec[:, kc:kc + 1], scale=1.0),
                     reads=[tmpf.b[i], mod.B], writes=[h.B])

        hq = sb("hq", (128, TT), F32)
        hq1 = sb("hq1", (128, TT), F32)
        hq2 = [hq, hq1]
        hrs = sb("hrs", (128, TT), F32)
        r1 = sb("r1", (128, TT), F32)
        r2 = sb("r2", (128, TT), F32)
        qb = sb("qb", (128, TT), BF16)
        qb1 = sb("qb1", (128, TT), BF16)
        qb2 = [qb, qb1]
        ropec = sb("ropec", (128, TT), F32)
        ropes = sb("ropes", (128, TT), F32)

        def head_norm(ps, gain_ap, out_f32=None, out_bf=None):
            G = bank["G"]
            P.op("act", lambda E: E.activation(out=sqr.t[:, 0, :], in_=ps.t[:], func=AF.Square), reads=[ps.B], writes=[sqr.b[0]])
            mm(G, G.t[:], onesb.t[:], sqr.t[:, 0, :], True, True, [onesb.B, sqr.b[0]])
            sum_sq_to_rstd(128.0, hrs)
            if out_f32 is not None:
                P.op("dve", lambda E: E.scalar_tensor_tensor(out=out_f32.t[:], in0=ps.t[:], scalar=gain_ap, in1=hrs.t[:],
                                                             op0=ALU.mult, op1=ALU.mult), reads=[ps.B, hrs.B, qg.B, kg.B], writes=[out_f32.B])
            else:
                P.op("dve", lambda E: E.scalar_tensor_tensor(out=out_bf, in0=ps.t[:], scalar=gain_ap, in1=hrs.t[:],
                                                             op0=ALU.mult, op1=ALU.mult), reads=[ps.B, hrs.B, qg.B, kg.B], writes=[])

        def rope_to(src_f32, out_ap, out_buf):
            H = bank["H"]
            mm(H, H.t[:], rt.t[:], src_f32.t[:], True, True, [rt.B, src_f32.B])
            P.op("dve", lambda E: E.tensor_tensor(out=src_f32.t[:], in0=src_f32.t[:], in1=ropec.t[:], op=ALU.mult),
                 reads=[src_f32.B, ropec.B], writes=[src_f32.B])
            P.op("dve", lambda E: E.tensor_tensor(out=tmpf.t[:, 1, :], in0=H.t[:], in1=ropes.t[:], op=ALU.mult),
                 reads=[H.B, ropes.B], writes=[tmpf.b[1]])
            P.op("dve", lambda E: E.tensor_tensor(out=out_ap, in0=src_f32.t[:], in1=tmpf.t[:, 1, :], op=ALU.add),
                 reads=[src_f32.B, tmpf.b[1]], writes=[out_buf])


        def plan_kv(l):
            return [(win_d[l, :, 1024:1280], ("in", 256)), (win_d[l, :, 1280:1536], ("in", 256))]

        def emit_kv(l, h, KTb, Vb, kcol0, vblk0, rope, cache_out=None):
            wk, wkb = w_get(win_d[l, :, 1024:1280], ("in", 256))
            for kvh in range(2):
                bk = bank["A%d" % kvh]
                for kc in range(KC):
                    mm(bk, bk.t[:], wk[:, kc, kvh * 128:(kvh + 1) * 128], h.t[:, kc, :], kc == 0, kc == KC - 1, [wkb, h.B])
                dst = KTb.t[:, kvh, kcol0:kcol0 + TT]
                if rope:
                    head_norm(bk, kg.t[:, l:l + 1], out_f32=hq)
                    rope_to(hq, dst, KTb.B)
                else:
                    head_norm(bk, kg.t[:, l:l + 1], out_f32=hq)
                    P.op("act", lambda E, dst=dst: E.activation(out=dst, in_=hq.t[:], func=AF.Copy), reads=[hq.B], writes=[KTb.B])
                    if cache_out is not None:
                        H = bank["H"]
                        for blk in range(4):
                            P.op("pe", lambda E, blk=blk: E.transpose(H.t[:, blk * 128:(blk + 1) * 128], hq.t[:, blk * 128:(blk + 1) * 128], ident.t[:]),
                                 reads=[hq.B, ident.B], writes=[H.B])
                        P.op("dve", lambda E, kvh=kvh: E.tensor_copy(kvstage.t[:, :, kvh * 128:(kvh + 1) * 128],
                                                                     H.t[:].rearrange("p (b d) -> p b d", b=4)),
                             reads=[H.B], writes=[kvstage.B])
            if cache_out is not None:
                for blk in range(4):
                    P.dma("sp", nk_d[blk // 2, l, (blk % 2) * 128:(blk % 2 + 1) * 128, :], kvstage.t[:, blk, :],
                          reads=[kvstage.B], writes=[Buf()])
            w_release()
            wv, wvb = w_get(win_d[l, :, 1280:1536], ("in", 256))
            for blk in range(4):
                bk = bank["B%d" % (blk // 2)]
                o_ap = bk.t[:, (blk % 2) * 256:(blk % 2 + 1) * 256]
                for kc in range(KC):
                    mm(bk, o_ap, h.t[:, kc, blk * 128:(blk + 1) * 128], wv[:, kc, :], kc == 0, kc == KC - 1, [wvb, h.B])
                if blk % 2 == 1:
                    P.op("act", lambda E, bk=bk, blk=blk: E.activation(out=Vb.t[:, vblk0 + blk - 1:vblk0 + blk + 1, :],
                                                                      in_=bk.t[:].rearrange("p (b d) -> p b d", b=2), func=AF.Copy),
                         reads=[bk.B], writes=[Vb.B])
                    if cache_out is not None:
                        P.op("dve", lambda E, bk=bk, blk=blk: E.tensor_copy(kvstage.t[:, blk - 1:blk + 1, :],
                                                                            bk.t[:].rearrange("p (b d) -> p b d", b=2)),
                             reads=[bk.B], writes=[kvstage.B])
            w_release()
            if cache_out is not None:
                for blk in range(4):
                    P.dma("sp", nv_d[blk // 2, l, (blk % 2) * 128:(blk % 2 + 1) * 128, :], kvstage.t[:, blk, :],
                          reads=[kvstage.B], writes=[Buf()])

        PT = sb("PT", (128, 4, TT), BF16, n=4)
        uf = sb("uf", (128, TT), F32)
        ghat = sb("ghat", (128, 4, 256), BF16)
        gss = sb("gss", (128, 8), F32)
        gsq = sb("gsq", (128, 128), F32)
        ssa = sb("ssa", (128, TT), F32)
        sss = sb("sss", (128, TT), F32)
        SCALE = 1.0 / float(np.sqrt(128.0))

        def plan_mixer(l):
            items = []
            for hp in range(4):
                items.append((win_d[l, :, 2560 + hp * 256:2560 + (hp + 1) * 256], ("in", 256)))
                items.append((win_d[l, :, 1536 + hp * 256:1536 + (hp + 1) * 256], ("in", 256)))
                items.append((win_d[l, :, hp * 256:(hp + 1) * 256], ("in", 256)))
            for pc in range(8):
                items.append((wout_d[l, :, pc * 256:(pc + 1) * 256], ("in", 256)))
            return items

        deferred = []

        def flush():
            for f in deferred:
                f()
            del deferred[:]

        def accum_sumsq(src_f32_ap, src_buf, acc, first, si):
            G = bank["G"]
            if any(getattr(f, "si", None) == si for f in deferred):
                flush()
            P.op("act", lambda E: E.activation(out=sqr.t[:, si, :], in_=src_f32_ap, func=AF.Square), reads=[src_buf], writes=[sqr.b[si]])

            def part2():
                mm(G, G.t[:], onesb.t[:], sqr.t[:, si, :], True, True, [onesb.B, sqr.b[si]])
                if first:
                    P.op("dve", lambda E: E.tensor_copy(acc.t[:], G.t[:]), reads=[G.B], writes=[acc.B])
                else:
                    P.op("dve", lambda E: E.tensor_tensor(out=acc.t[:], in0=acc.t[:], in1=G.t[:], op=ALU.add), reads=[G.B, acc.B], writes=[acc.B])
            part2.si = si
            deferred.append(part2)

        def emit_mixer(l, cnd, x, h, o, KTb, Vb, groups, rope):
            P.dma("sp", bsrep.t[:, 0], bsrep_d[:, l], reads=[bIN], writes=[bsrep.B])
            for hp in range(4):
                c4 = hp
                wg_, wgb = w_get(win_d[l, :, 2560 + hp * 256:2560 + (hp + 1) * 256], ("in", 256))
                for blk in range(4):
                    bk = bank["A%d" % (blk % 2)]
                    o_ap = bk.t[:, 0:256]
                    for kc in range(KC):
                        mm(bk, o_ap, h.t[:, kc, blk * 128:(blk + 1) * 128], wg_[:, kc, :], kc == 0, kc == KC - 1, [wgb, h.B])
                    P.op("dve", lambda E, c4=c4: E.memset(gss.t[:, c4 * 2:c4 * 2 + 2], 0.0), reads=[gss.B], writes=[gss.B])
                    for hh in range(2):
                        hd = c4 * 2 + hh
                        P.op("act", lambda E, bk=bk, hh=hh, hd=hd: E.activation(out=gsq.t[:], in_=bk.t[:, hh * 128:(hh + 1) * 128], func=AF.Square,
                                                                               accum_out=gss.t[:, hd:hd + 1]),
                             reads=[bk.B], writes=[gsq.B, gss.B])
                    P.op("act", lambda E, c4=c4: E.activation(out=gss.t[:, c4 * 2:c4 * 2 + 2], in_=gss.t[:, c4 * 2:c4 * 2 + 2], func=AF.Sqrt,
                                                              bias=epsc.t[:, 0:1], scale=1.0 / 128.0), reads=[gss.B, epsc.B], writes=[gss.B])
                    P.op("dve", lambda E, c4=c4: E.reciprocal(out=gss.t[:, c4 * 2:c4 * 2 + 2], in_=gss.t[:, c4 * 2:c4 * 2 + 2]),
                         reads=[gss.B], writes=[gss.B])
                    for hh in range(2):
                        hd = c4 * 2 + hh
                        P.op("dve", lambda E, bk=bk, hh=hh, hd=hd, blk=blk: E.tensor_scalar(
                            out=ghat.t[:, blk, hh * 128:(hh + 1) * 128], in0=bk.t[:, hh * 128:(hh + 1) * 128],
                            scalar1=gss.t[:, hd:hd + 1], scalar2=None, op0=ALU.mult), reads=[bk.B, gss.B], writes=[ghat.B])
                w_release()
                flush()
                wu_, wub = w_get(win_d[l, :, 1536 + hp * 256:1536 + (hp + 1) * 256], ("in", 256))
                wq_, wqb = w_get(win_d[l, :, hp * 256:(hp + 1) * 256], ("in", 256))
                C, Dk = bank["C"], bank["Dk"]

                def sgu_head(hh):
                    hd = hp * 2 + hh
                    bkA = bank["A0"]
                    for kc in range(KC):
                        mm(bkA, bkA.t[:], wu_[:, kc, hh * 128:(hh + 1) * 128], h.t[:, kc, :], kc == 0, kc == KC - 1, [wub, h.B])
                    P.op("act", lambda E: E.activation(out=uf.t[:], in_=bkA.t[:], func=AF.Copy), reads=[bkA.B], writes=[uf.B])
                    bkB = bank["B%d" % hh]
                    for blk in range(4):
                        mm(bkB, bkB.t[:, blk * 128:(blk + 1) * 128], ghat.t[:, blk, hh * 128:(hh + 1) * 128], wsT.t[:, l, hd, :],
                           True, True, [ghat.B, wsT.B])
                    for blk in range(4):
                        P.op("dve", lambda E, blk=blk: E.scalar_tensor_tensor(
                            out=r1.t[:, blk * 128:(blk + 1) * 128], in0=bkB.t[:, blk * 128:(blk + 1) * 128], scalar=sgn.t[:, l, hd:hd + 1],
                            in1=bsrep.t[:, 0, hd, :], op0=ALU.mult, op1=ALU.add), reads=[bkB.B, sgn.B, bsrep.B], writes=[r1.B])
                    P.op("dve", lambda E: E.tensor_tensor(out=r2.t[:], in0=r1.t[:], in1=uf.t[:], op=ALU.mult), reads=[r1.B, uf.B], writes=[r2.B])
                    P.op("act", lambda E: E.activation(out=o.t[:, 8 + hd, :], in_=r2.t[:], func=AF.Copy, scale=ong.t[:, l, 8 + hd:9 + hd]),
                         reads=[r2.B, ong.B], writes=[o.B])
                    accum_sumsq(r2.t[:], r2.B, sss, hd == 0, 1)

                def q_proj_norm(hh):
                    bkQ = bank["A1"]
                    for kc in range(KC):
                        mm(bkQ, bkQ.t[:], wq_[:, kc, hh * 128:(hh + 1) * 128], h.t[:, kc, :], kc == 0, kc == KC - 1, [wqb, h.B])
                    head_norm(bkQ, qg.t[:, l:l + 1], out_f32=hq2[hh])

                def q_finish(hh):
                    if rope:
                        rope_to(hq2[hh], qb2[hh].t[:], qb2[hh].B)
                    else:
                        P.op("act", lambda E: E.activation(out=qb2[hh].t[:], in_=hq2[hh].t[:], func=AF.Copy), reads=[hq2[hh].B], writes=[qb2[hh].B])

                def attention(hh, hook=None):
                    hd = hp * 2 + hh
                    kvh = hd // 4
                    qbh = qb2[hh]
                    for gi, (q0, q1, kblocks) in enumerate(groups):
                        nkb = len(kblocks)

                        def score(ji, q0=q0, q1=q1, kblocks=kblocks):
                            j = kblocks[ji]
                            bS = bank["B%d" % (ji % 2)]
                            mm(bS, bS.t[:, q0:q1], KTb.t[:, kvh, j * 128:(j + 1) * 128], qbh.t[:, q0:q1], True, True, [KTb.B, qbh.B])
                            P.op("act", lambda E: E.activation(out=PT.t[:, ji % 4, q0:q1], in_=bS.t[:, q0:q1], func=AF.Exp, scale=SCALE),
                                 reads=[bS.B], writes=[PT.b[ji % 4]])
                        score(0)
                        for ji in range(nkb):
                            if ji + 1 < nkb:
                                score(ji + 1)
                            j = kblocks[ji]
                            mm(C, C.t[:, q0:q1], Vb.t[:, j, kvh * 128:(kvh + 1) * 128], PT.t[:, ji % 4, q0:q1], ji == 0, ji == nkb - 1,
                               [Vb.B, PT.b[ji % 4]])
                            mm(Dk, Dk.t[:, q0:q1], onesb.t[:], PT.t[:, ji % 4, q0:q1], ji == 0, ji == nkb - 1, [onesb.B, PT.b[ji % 4]])
                            if hook is not None and gi == 0 and ji == min(3, nkb - 1):
                                hook()
                    P.op("dve", lambda E: E.reciprocal(out=r1.t[:], in_=Dk.t[:]), reads=[Dk.B], writes=[r1.B])
                    P.op("dve", lambda E: E.tensor_tensor(out=r2.t[:], in0=C.t[:], in1=r1.t[:], op=ALU.mult), reads=[C.B, r1.B], writes=[r2.B])
                    P.op("act", lambda E: E.activation(out=o.t[:, hd, :], in_=r2.t[:], func=AF.Copy, scale=ong.t[:, l, hd:hd + 1]),
                         reads=[r2.B, ong.B], writes=[o.B])
                    accum_sumsq(r2.t[:], r2.B, ssa, hd == 0, 2)

                sgu_head(0)
                q_proj_norm(0)
                sgu_head(1)
                flush()
                q_finish(0)
                q_proj_norm(1)
                flush()
                attention(0, hook=lambda: q_finish(1))
                attention(1)
                w_release()
                mod_step(bgc["hp"])
            flush()
            rsa, rss = ssa, sss
            for (acc, dstr) in ((ssa, ssa), (sss, sss)):
                P.op("act", lambda E, acc=acc, dstr=dstr: E.activation(out=dstr.t[:], in_=acc.t[:], func=AF.Sqrt, bias=epsc.t[:, 0:1], scale=1.0 / 1024.0),
                     reads=[acc.B, epsc.B], writes=[dstr.B])
                P.op("dve", lambda E, dstr=dstr: E.reciprocal(out=dstr.t[:], in_=dstr.t[:]), reads=[dstr.B], writes=[dstr.B])
            g1 = mvec(l, cnd, 2)
            for pc in range(8):
                wo_, wob = w_get(wout_d[l, :, pc * 256:(pc + 1) * 256], ("in", 256))
                for c2 in range(2):
                    oc = pc * 2 + c2
                    bkA, bkB = bank["A%d" % c2], bank["B%d" % c2]
                    for kc in range(8):
                        mm(bkA, bkA.t[:], wo_[:, kc, c2 * 128:(c2 + 1) * 128], o.t[:, kc, :], kc == 0, kc == 7, [wob, o.B])
                    for kc in range(8, 16):
                        mm(bkB, bkB.t[:], wo_[:, kc, c2 * 128:(c2 + 1) * 128], o.t[:, kc, :], kc == 8, kc == 15, [wob, o.B])
                    if c2 == 1:
                        w_release()
                        mod_step(bgc["wo"])
                    P.op("dve", lambda E, bkA=bkA: E.tensor_tensor(out=r1.t[:], in0=bkA.t[:], in1=rsa.t[:], op=ALU.mult), reads=[bkA.B, rsa.B], writes=[r1.B])
                    P.op("dve", lambda E, bkB=bkB: E.tensor_tensor(out=r2.t[:], in0=bkB.t[:], in1=rss.t[:], op=ALU.mult), reads=[bkB.B, rss.B], writes=[r2.B])
                    P.op("dve", lambda E: E.tensor_tensor(out=r1.t[:], in0=r1.t[:], in1=r2.t[:], op=ALU.add), reads=[r1.B, r2.B], writes=[r1.B])
                    P.op("dve", lambda E, oc=oc: E.scalar_tensor_tensor(out=x.t[:, oc, :], in0=r1.t[:], scalar=g1[:, oc:oc + 1], in1=x.t[:, oc, :],
                                                                       op0=ALU.mult, op1=ALU.add), reads=[r1.B, mod.B, x.B], writes=[x.B])

        sg = sb("sg", (128, 2, TT), F32, n=2)
        actb = sb("actb", (128, 2, 4, TT), BF16, n=2)
        cwrep = sb("cwrep", (128, TT), F32)
        cw4 = sb("cw4", (128, 4, NE), F32)
        lg = sb("lg", (128, NE), F32)
        lg8 = sb("lg8", (128, 8), F32)
        cw = sb("cw", (128, NE), F32)
        cwb = sb("cwb", (128, 128), F32)
        tcol = sb("tcol", (128, 4), F32)
        wrp = sb("wrp", (128, KC, NE), F32)

        def ffn_panel_list(l):
            out = []
            if l == 0:
                for p in range(DFF // 256):
                    out.append((fg_d[0][:, p * 256:(p + 1) * 256], fu_d[0][:, p * 256:(p + 1) * 256], fd_d[0][p * 256:(p + 1) * 256, :], None))
            else:
                for e in range(NE):
                    for p in range(DFE // 256):
                        out.append((mg_d[0, e][:, p * 256:(p + 1) * 256], mu_d[0, e][:, p * 256:(p + 1) * 256],
                                    md_d[0, e][p * 256:(p + 1) * 256, :], e))
            assert len(out) % 2 == 0
            return out

        def plan_ffn(l):
            pl = ffn_panel_list(l)
            items = []
            for pp in range(len(pl) // 2):
                for half in range(2):
                    g_, u_, d_, e = pl[pp * 2 + half]
                    items.append((g_, ("in", 256)))
                    items.append((u_, ("in", 256)))
                for half in range(2):
                    items.append((pl[pp * 2 + half][2], ("rows", 2)))
            return items

        def emit_ffn_panels(l, x, h2, g2):
            pl = ffn_panel_list(l)
            cur_e = None
            for pp in range(len(pl) // 2):
                pi = pp % 2
                for half in range(2):
                    e = pl[pp * 2 + half][3]
                    if e is not None and e != cur_e:
                        emit_cwrep(e)
                        cur_e = e
                    wg_, wgb = w_get(pl[pp * 2 + half][0], ("in", 256))
                    for c2 in range(2):
                        bkG = bank["A%d" % c2]
                        for kc in range(KC):
                            mm(bkG, bkG.t[:], wg_[:, kc, c2 * 128:(c2 + 1) * 128], h2.t[:, kc, :], kc == 0, kc == KC - 1, [wgb, h2.B])
                    w_release()
                    wu_, wub = w_get(pl[pp * 2 + half][1], ("in", 256))
                    for c2 in range(2):
                        bkU = bank["B%d" % c2]
                        for kc in range(KC):
                            mm(bkU, bkU.t[:], wu_[:, kc, c2 * 128:(c2 + 1) * 128], h2.t[:, kc, :], kc == 0, kc == KC - 1, [wub, h2.B])
                    w_release()
                    for c2 in range(2):
                        bkG, bkU = bank["A%d" % c2], bank["B%d" % c2]
                        P.op("act", lambda E, bkG=bkG, c2=c2: E.activation(out=sg.t[:, c2, :], in_=bkG.t[:], func=AF.Silu), reads=[bkG.B], writes=[sg.b[c2]])
                        if e is not None:
                            P.op("dve", lambda E, c2=c2: E.tensor_tensor(out=sg.t[:, c2, :], in0=sg.t[:, c2, :], in1=cwrep.t[:], op=ALU.mult),
                                 reads=[sg.b[c2], cwrep.B], writes=[sg.b[c2]])
                        P.op("dve", lambda E, bkU=bkU, c2=c2, pi=pi, half=half: E.tensor_tensor(out=actb.t[:, pi, half * 2 + c2, :], in0=bkU.t[:], in1=sg.t[:, c2, :], op=ALU.mult),
                             reads=[bkU.B, sg.b[c2]], writes=[actb.b[pi]])
                wd0, wdb0 = w_get(pl[pp * 2][2], ("rows", 2))
                wd1, wdb1 = w_get(pl[pp * 2 + 1][2], ("rows", 2))
                for oc in range(KC):
                    bk = bank["C"] if oc % 2 == 0 else bank["Dk"]
                    for c4 in range(4):
                        wd_, wdb = (wd0, wdb0) if c4 < 2 else (wd1, wdb1)
                        mm(bk, bk.t[:], wd_[:, c4 % 2, oc * 128:(oc + 1) * 128], actb.t[:, pi, c4, :], c4 == 0, c4 == 3, [wdb, actb.b[pi]])
                    P.op("dve", lambda E, bk=bk, oc=oc: E.scalar_tensor_tensor(out=x.t[:, oc, :], in0=bk.t[:], scalar=g2[:, oc:oc + 1], in1=x.t[:, oc, :],
                                                                              op0=ALU.mult, op1=ALU.add), reads=[bk.B, mod.B, x.B], writes=[x.B])
                w_release()
                mod_step(bgc["ffn"])

        def emit_router(l, cnd, x):
            A2v = Avec2(l, cnd)
            sh2 = mvec(l, cnd, 3)
            for kc in range(KC):
                P.op("dve", lambda E, kc=kc: E.tensor_scalar(out=wrp.t[:, kc, :], in0=wr.t[:, kc, :], scalar1=A2v[:, kc:kc + 1], scalar2=None, op0=ALU.mult),
                     reads=[wr.B, A2.B], writes=[wrp.B])
            H, G = bank["H"], bank["G"]
            for kc in range(KC):
                P.op("dve", lambda E, kc=kc: E.tensor_scalar(out=gsq.t[:], in0=ident.t[:], scalar1=0.0, scalar2=sh2[:, kc:kc + 1], op0=ALU.mult, op1=ALU.add),
                     reads=[ident.B, mod.B, gsq.B], writes=[gsq.B])
                mm(G, G.t[:, 0:NE], gsq.t[:], wr.t[:, kc, :], kc == 0, kc == KC - 1, [gsq.B, wr.B])
            P.op("dve", lambda E: E.tensor_tensor(out=lg8.t[:], in0=G.t[:, 0:NE], in1=brrep.t[:], op=ALU.add), reads=[G.B, brrep.B], writes=[lg8.B])
            for blk in range(4):
                for kc in range(KC):
                    mm(H, H.t[:, 0:NE], x.t[:, kc, blk * 128:(blk + 1) * 128], wrp.t[:, kc, :], kc == 0, kc == KC - 1, [x.B, wrp.B])
                mm(G, G.t[:, 0:1], rstd.t[:, blk * 128:(blk + 1) * 128], ident.t[:, 0:1], True, True, [rstd.B, ident.B])
                P.op("dve", lambda E: E.tensor_copy(tcol.t[:, 0:1], G.t[:, 0:1]), reads=[G.B], writes=[tcol.B])
                P.op("dve", lambda E: E.scalar_tensor_tensor(out=lg.t[:], in0=H.t[:, 0:NE], scalar=tcol.t[:, 0:1], in1=lg8.t[:], op0=ALU.mult, op1=ALU.add),
                     reads=[H.B, tcol.B, lg8.B], writes=[lg.B])
                P.op("dve", lambda E: E.tensor_reduce(out=tcol.t[:, 1:2], in_=lg.t[:], axis=mybir.AxisListType.X, op=ALU.max), reads=[lg.B, tcol.B], writes=[tcol.B])
                P.op("dve", lambda E: E.tensor_scalar(out=cw.t[:], in0=lg.t[:], scalar1=tcol.t[:, 1:2], scalar2=-1e30, op0=ALU.is_ge, op1=ALU.mult),
                     reads=[lg.B, tcol.B], writes=[cw.B])
                P.op("dve", lambda E: E.tensor_tensor(out=cw.t[:], in0=cw.t[:], in1=lg.t[:], op=ALU.add), reads=[cw.B, lg.B], writes=[cw.B])
                P.op("dve", lambda E: E.tensor_reduce(out=tcol.t[:, 2:3], in_=cw.t[:], axis=mybir.AxisListType.X, op=ALU.max), reads=[cw.B, tcol.B], writes=[tcol.B])
                P.op("dve", lambda E: E.tensor_scalar(out=cw.t[:], in0=lg.t[:], scalar1=tcol.t[:, 2:3], scalar2=None, op0=ALU.is_ge), reads=[lg.B, tcol.B], writes=[cw.B])
                P.op("dve", lambda E: E.tensor_scalar(out=tcol.t[:, 3:4], in0=tcol.t[:, 1:2], scalar1=-1.0, scalar2=None, op0=ALU.mult), reads=[tcol.B], writes=[tcol.B])
                P.op("act", lambda E: E.activation(out=lg.t[:], in_=lg.t[:], func=AF.Exp, bias=tcol.t[:, 3:4], scale=1.0), reads=[lg.B, tcol.B], writes=[lg.B])
                P.op("dve", lambda E: E.tensor_tensor(out=cw.t[:], in0=cw.t[:], in1=lg.t[:], op=ALU.mult), reads=[cw.B, lg.B], writes=[cw.B])
                P.op("dve", lambda E: E.tensor_reduce(out=tcol.t[:, 0:1], in_=cw.t[:], axis=mybir.AxisListType.X, op=ALU.add), reads=[cw.B, tcol.B], writes=[tcol.B])
                P.op("dve", lambda E: E.reciprocal(out=tcol.t[:, 0:1], in_=tcol.t[:, 0:1]), reads=[tcol.B], writes=[tcol.B])
                P.op("dve", lambda E, blk=blk: E.tensor_scalar(out=cw4.t[:, blk, :], in0=cw.t[:], scalar1=tcol.t[:, 0:1], scalar2=None, op0=ALU.mult),
                     reads=[cw.B, tcol.B], writes=[cw4.B])

        def emit_cwrep(e):
            bk = bank["H"]
            for blk in range(4):
                P.op("dve", lambda E, blk=blk: E.tensor_scalar(out=cwb.t[:], in0=ident.t[:], scalar1=0.0, scalar2=cw4.t[:, blk, e:e + 1], op0=ALU.mult, op1=ALU.add),
                     reads=[ident.B, cw4.B, cwb.B], writes=[cwb.B])
                mm(bk, bk.t[:, blk * 128:(blk + 1) * 128], cwb.t[:], ident.t[:], True, True, [cwb.B, ident.B])
            P.op("act", lambda E: E.activation(out=cwrep.t[:], in_=bk.t[:], func=AF.Copy), reads=[bk.B], writes=[cwrep.B])

        def emit_ffn(l, cnd, x, h2):
            g2 = mvec(l, cnd, 5)
            norm_mod(x, h2, Avec2(l, cnd), mvec(l, cnd, 3))
            if l == 1:
                emit_router(l, cnd, x)
            emit_ffn_panels(l, x, h2, g2)

        xA = sb("xA", (128, KC, TT), F32)
        xpark = nc.dram_tensor("xpark", [2, 128, KC, TT], F32, kind="Internal").ap()
        bpark = [Buf("xpark0"), Buf("xpark1")]
        kvsend = nc.dram_tensor("kvsend", [128, 4096], BF16, kind="Internal").ap()
        kvrecv = nc.dram_tensor("kvrecv", [256, 4096], BF16, addr_space="Local", kind="Internal").ap()
        bsend, brecv = Buf("kvsend"), Buf("kvrecv")
        msend = [nc.dram_tensor("msend%d" % k, [128, (hi - lo) * 4], F32, kind="Internal").ap() for k, (lo, hi) in enumerate(ROUNDS)]
        mrecv = [nc.dram_tensor("mrecv%d" % k, [256, (hi - lo) * 4], F32, addr_space="Local", kind="Internal").ap()
                 for k, (lo, hi) in enumerate(ROUNDS)]
        bmsend = [Buf("msend%d" % k) for k in range(3)]
        bmrecv = [Buf("mrecv%d" % k) for k in range(3)]
        hb = sb("hb", (128, KC, TT), BF16)
        ob = sb("ob", (128, KC, TT), BF16)
        KT0 = sb("KT0", (128, 2, 2304), BF16)
        V0 = sb("V0", (128, 18, 256), BF16)
        KT1 = sb("KT1", (128, 2, 2304), BF16)
        V1 = sb("V1", (128, 18, 256), BF16)
        cstage = T(xstage.t[:, 0:512].rearrange("p (r f) -> p r f", r=2), 1)
        cstage.b = xstage.b
        cstage.B = xstage.B

        def load_rope(t):
            P.dma("sp", ropec.t[:], ropec_d[t], reads=[bIN], writes=[ropec.B])
            P.dma("sp", ropes.t[:], ropes_d[t], reads=[bIN], writes=[ropes.B])

        def load_cache(l, KTb, Vb):
            P.dma("sp", cstage.t, ck_d[l].rearrange("(r p) f -> p r f", p=128), reads=[bIN], writes=[cstage.B])
            H = bank["H"]
            for kvh in range(2):
                for r in range(2):
                    P.op("pe", lambda E, kvh=kvh, r=r: E.transpose(H.t[:, (kvh * 2 + r) * 128:(kvh * 2 + r + 1) * 128],
                                                                   cstage.t[:, r, kvh * 128:(kvh + 1) * 128], ident.t[:]),
                         reads=[cstage.B, ident.B], writes=[H.B])
            P.op("act", lambda E: E.activation(out=KTb.t[:, :, 0:256], in_=H.t[:].rearrange("p (k t) -> p k t", k=2), func=AF.Copy),
                 reads=[H.B], writes=[KTb.B])
            P.dma("pool", Vb.t[:, 0:2, :], cv_d[l].rearrange("(r p) f -> p r f", p=128), reads=[bIN], writes=[Vb.B])

        pgroups = [(0, 256, [0, 1]), (256, 512, [2, 3])]
        sgroups = [(0, 512, list(range(18)))]

        def schedule():
            bgc.update(hp=0, wo=0, ffn=0)
            mod_step(8)
            load_cache(0, KT0, V0)
            load_cache(1, KT1, V1)
            for t in range(4):
                load_x(xs_d[t], xA)
                load_rope(t)
                norm_mod(xA, hb, Avec1(0, 1), mvec(0, 1, 0))
                emit_kv(0, hb, KT0, V0, 256 + t * 512, 2 + t * 4, rope=True)
                mod_step(4)
            bgc.update(hp=2, wo=1, ffn=1)
            for t in (0, 1):
                load_x(xs_d[t], xA)
                load_rope(t)
                norm_mod(xA, hb, Avec1(0, 1), mvec(0, 1, 0))
                emit_mixer(0, 1, xA, hb, ob, KT0, V0, sgroups, rope=True)
                emit_ffn(0, 1, xA, hb)
                norm_mod(xA, hb, Avec1(1, 1), mvec(1, 1, 0))
                emit_kv(1, hb, KT1, V1, 256 + t * 512, 2 + t * 4, rope=True)
                P.dma("sp", xpark[t], xA.t[:], reads=[xA.B], writes=[bpark[t]])
            P.dma("sp", kvsend[:, 0:2048].rearrange("p (k n) -> p k n", k=2), KT1.t[:, :, 256:1280], reads=[KT1.B], writes=[bsend])
            P.dma("sp", kvsend[:, 2048:4096].rearrange("p (b d) -> p b d", b=8), V1.t[:, 2:10, :], reads=[V1.B], writes=[bsend])
            P.coll(kvsend, kvrecv, [[0, 1], [2, 3], [4, 5], [6, 7]], reads=[bsend], writes=[brecv])
            load_x(xp_d, xA)
            for l in range(NL):
                norm_mod(xA, hb, Avec1(l, 0), mvec(l, 0, 0))
                emit_kv(l, hb, KT0, V0, 0, 0, rope=False, cache_out=True)
                emit_mixer(l, 0, xA, hb, ob, KT0, V0, pgroups, rope=False)
                emit_ffn(l, 0, xA, hb)
            store_x(xA, yp_d)
            for r in range(2):
                P.dma("sp", KT1.t[:, :, 256 + r * 1024:1280 + r * 1024],
                      kvrecv[r * 128:(r + 1) * 128, 0:2048].rearrange("p (k n) -> p k n", k=2), reads=[brecv], writes=[KT1.B])
                P.dma("sp", V1.t[:, 2 + r * 8:10 + r * 8, :],
                      kvrecv[r * 128:(r + 1) * 128, 2048:4096].rearrange("p (b d) -> p b d", b=8), reads=[brecv], writes=[V1.B])
            for t in (0, 1):
                load_rope(t)
                P.dma("sp", xA.t[:], xpark[t], reads=[bpark[t]], writes=[xA.B])
                norm_mod(xA, hb, Avec1(1, 1), mvec(1, 1, 0))
                emit_mixer(1, 1, xA, hb, ob, KT1, V1, sgroups, rope=True)
                emit_ffn(1, 1, xA, hb)
                store_x(xA, ys_d[t])
            assert modst["i"] == 48 and modst["rounds"] == 3 and not deferred

        P.dry = True
        schedule()
        n_plan = wstate["taken"]
        wstate.update(issued=0, taken=0, released=0)
        modst.update(i=0, rounds=0)
        P.dry = False
        schedule()
        assert wstate["taken"] == n_plan == len(wstate["plan"]), (wstate["taken"], n_plan, len(wstate["plan"]))
        P.finish()
        build_program.stats = (P.n_ops, P.n_wait, nc.sbuf_bytes_remaining)
    return nc


def _rope_tables():
    L_ = 2048
    rows = (np.arange(L_) // 64).astype(np.float32)
    cols = (np.arange(L_) % 64).astype(np.float32)
    inv = (10000.0 ** (-np.arange(0, 64, 2, dtype=np.float32) / 64.0)).astype(np.float32)
    ar = rows[:, None] * inv[None, :]
    ac = cols[:, None] * inv[None, :]
    ang = np.concatenate([ar, ar, ac, ac], axis=1)
    return np.cos(ang).astype(np.float32), np.sin(ang).astype(np.float32)


def _rt_matrix():
    rt = np.zeros((128, 128), np.float32)
    for base in (0, 64):
        for i in range(32):
            m = base + i
            rt[m + 32, m] = -1.0
            rt[m, m + 32] = 1.0
    return rt


def _fm(v):
    v = np.asarray(v, np.float32)
    lead = v.shape[:-1]
    return np.ascontiguousarray(np.moveaxis(v.reshape(lead + (KC, 128)), -1, 0))


_NC_CACHE = {}


def kernel(x_prompt, x_sample, cache_k, cache_v, c, c_ctx, w_ada, b_ada, norm1_g, norm2_g,
           w_in, q_norm_g, k_norm_g, sgu_norm_g, w_spatial, b_spatial, out_norm_g, w_out,
           ffn_w_gate, ffn_w_up, ffn_w_down, w_router, b_router, moe_w_gate, moe_w_up, moe_w_down):
    f32 = lambda a: np.ascontiguousarray(np.asarray(a, dtype=np.float32))
    x_prompt, x_sample, cache_k, cache_v = f32(x_prompt), f32(x_sample), f32(cache_k), f32(cache_v)
    c, c_ctx = f32(c), f32(c_ctx)
    if "nc" not in _NC_CACHE:
        _NC_CACHE["nc"] = build_program()
    nc = _NC_CACHE["nc"]
    in_maps = _prep(x_prompt, x_sample, cache_k, cache_v, c, c_ctx, w_ada, b_ada, norm1_g, norm2_g,
                    w_in, q_norm_g, k_norm_g, sgu_norm_g, w_spatial, b_spatial, out_norm_g, w_out,
                    ffn_w_gate, ffn_w_up, ffn_w_down, w_router, b_router, moe_w_gate, moe_w_up, moe_w_down)
    res = run_bass_kernel_spmd(nc, in_maps, core_ids=list(range(NCORES)))
    return _assemble(res.results)


def _prep(x_prompt, x_sample, cache_k, cache_v, c, c_ctx, w_ada, b_ada, norm1_g, norm2_g,
          w_in, q_norm_g, k_norm_g, sgu_norm_g, w_spatial, b_spatial, out_norm_g, w_out,
          ffn_w_gate, ffn_w_up, ffn_w_down, w_router, b_router, moe_w_gate, moe_w_up, moe_w_down):
    f32 = lambda a: np.ascontiguousarray(np.asarray(a, dtype=np.float32))

    cos, sin = _rope_tables()
    w_ada_f = f32(w_ada)
    shared = {
        "n1g": _fm(norm1_g), "n2g": _fm(norm2_g),
        "bada": np.ascontiguousarray(np.moveaxis(f32(b_ada).reshape(NL, 96, 128), -1, 0)),
        "qg": np.ascontiguousarray(f32(q_norm_g).T), "kg": np.ascontiguousarray(f32(k_norm_g).T),
        "sgn": np.ascontiguousarray(np.moveaxis(f32(sgu_norm_g), -1, 0)),
        "bsrep": np.ascontiguousarray(np.broadcast_to(f32(b_spatial)[None], (128, NL, 8, 128))),
        "wsT": np.ascontiguousarray(np.transpose(f32(w_spatial), (3, 0, 1, 2))),
        "ong": _fm(out_norm_g),
        "wr": np.ascontiguousarray(np.transpose(f32(w_router)[0].reshape(KC, 128, NE), (1, 0, 2))),
        "brrep": np.ascontiguousarray(np.broadcast_to(f32(b_router)[0][None], (128, NE))),
        "ident": np.eye(128, dtype=np.float32), "rt": _rt_matrix(),
        "w_in": f32(w_in), "w_out": f32(w_out),
        "ffn_w_gate": f32(ffn_w_gate), "ffn_w_up": f32(ffn_w_up), "ffn_w_down": f32(ffn_w_down),
        "moe_w_gate": f32(moe_w_gate), "moe_w_up": f32(moe_w_up), "moe_w_down": f32(moe_w_down),
    }
    in_maps = []
    for core in range(NCORES):
        b, half = core // 2, core % 2
        own = x_sample[b, half * 1024:(half + 1) * 1024].reshape(2, TT, D)
        oth = x_sample[b, (1 - half) * 1024:(2 - half) * 1024].reshape(2, TT, D)
        pos = np.concatenate([np.arange(half * 1024, (half + 1) * 1024), np.arange((1 - half) * 1024, (2 - half) * 1024)])
        m = dict(shared)
        m["xs"] = np.ascontiguousarray(np.concatenate([own, oth], axis=0))
        m["xp"] = np.ascontiguousarray(x_prompt[2 * core:2 * core + 2].reshape(TT, D))
        m["ropec"] = np.ascontiguousarray(cos[pos].reshape(4, TT, 128).transpose(0, 2, 1))
        m["ropes"] = np.ascontiguousarray(sin[pos].reshape(4, TT, 128).transpose(0, 2, 1))
        m["ck"] = np.ascontiguousarray(cache_k[b].reshape(NL, 256, 256))
        m["cv"] = np.ascontiguousarray(cache_v[b].reshape(NL, 256, 256))
        cv2 = np.stack([c_ctx, c[b]], axis=-1)
        m["cvec"] = np.ascontiguousarray(cv2.reshape(KC, 128, 2).transpose(1, 0, 2))
        m["w_ada"] = np.ascontiguousarray(w_ada_f.reshape(NL, D, 24, 2, 256)[:, :, :, half, :].reshape(NL, D, 3 * D))
        in_maps.append(m)
    return in_maps


def _assemble(R):
    y_prompt = np.empty((16, 256, D), np.float32)
    y_sample = np.empty((4, 2048, D), np.float32)
    nk = np.empty((16, NL, 256, 2, 128), np.float32)
    nv = np.empty((16, NL, 256, 2, 128), np.float32)
    for core in range(NCORES):
        b, half = core // 2, core % 2
        r = R[core]
        y_prompt[2 * core:2 * core + 2] = np.asarray(r["yp"]).reshape(2, 256, D)
        y_sample[b, half * 1024:(half + 1) * 1024] = np.asarray(r["ys"]).reshape(1024, D)
        nk[2 * core:2 * core + 2] = np.asarray(r["nk"]).reshape(2, NL, 256, 2, 128)
        nv[2 * core:2 * core + 2] = np.asarray(r["nv"]).reshape(2, NL, 256, 2, 128)
    return (y_prompt, y_sample, nk, nv)
```

```python
import numpy as np
from contextlib import ExitStack
import concourse.bass as bass
import concourse.mybir as mybir
from concourse.bass_utils import run_bass_kernel_spmd

F32 = mybir.dt.float32
BF16 = mybir.dt.bfloat16
ALU = mybir.AluOpType
AF = mybir.ActivationFunctionType

ENGS = ("pe", "act", "dve", "pool", "sp")

D = 2048
KC = 16
TT = 512
NL = 2
INW = 3584
DFF = 5632
NE = 8
DFE = 2816
EPS = 1e-6
NCORES = 8


class Buf:
    __slots__ = ("name", "w", "r", "excl")

    def __init__(self, name="", excl=False):
        self.name = name
        self.excl = excl
        self.w = None
        self.r = []


class Prog:
    NDMA = 6

    def __init__(self, nc, stack):
        self.nc = nc
        self.stack = stack
        self.ops = {e: [] for e in ENGS}
        self.sem = {}
        self.cnt = {}
        self.seen = {e: {} for e in ENGS}
        for e in ENGS:
            self._mk("c_" + e)
        self.dma_rr = {}
        for q in ("sp", "pool"):
            for i in range(self.NDMA):
                self._mk("d_%s_%d" % (q, i))
            self.dma_rr[q] = 0
        self._mk("d_cc")
        self.n_wait = 0
        self.n_ops = 0
        self.dry = False

    def _mk(self, key):
        self.sem[key] = self.stack.enter_context(self.nc.semaphore(key))
        self.cnt[key] = 0

    def _deps(self, reads, writes):
        d = {}

        def add(ev):
            if ev is None:
                return
            k, v = ev
            if d.get(k, 0) < v:
                d[k] = v
        for b in reads:
            add(b.w)
        for b in writes:
            add(b.w)
            for ev in b.r:
                add(ev)
        return d

    def _emit_waits(self, eng, deps):
        seen = self.seen[eng]
        for k, v in deps.items():
            if eng == "pe" and k == "c_pe":
                continue
            if seen.get(k, 0) >= v:
                continue
            seen[k] = v
            sem = self.sem[k]
            self.ops[eng].append(lambda E, sem=sem, v=v: E.wait_ge(sem, v))
            self.n_wait += 1

    def _record(self, ev, reads, writes):
        for b in reads:
            b.r.append(ev)
            if len(b.r) > 48:
                m = {}
                for k, v in b.r:
                    if m.get(k, 0) < v:
                        m[k] = v
                b.r = list(m.items())
        for b in writes:
            b.w = ev
            b.r = []

    def op(self, eng, fn, reads=(), writes=()):
        if self.dry:
            return
        if any(b.excl for b in reads):
            writes = list(writes) + [b for b in reads if b.excl]
            reads = [b for b in reads if not b.excl]
        deps = self._deps(reads, writes)
        self._emit_waits(eng, deps)
        key = "c_" + eng
        self.cnt[key] += 1
        v = self.cnt[key]
        sem = self.sem[key]
        self.ops[eng].append(lambda E, fn=fn, sem=sem: fn(E).then_inc(sem, 1))
        self._record((key, v), reads, writes)
        self.n_ops += 1

    def dma(self, q, out, in_, reads=(), writes=()):
        if self.dry:
            return
        deps = self._deps(reads, writes)
        i = self.dma_rr[q]
        self.dma_rr[q] = (i + 1) % self.NDMA
        key = "d_%s_%d" % (q, i)
        if self.cnt[key] > 0 and deps.get(key, 0) < self.cnt[key]:
            deps[key] = self.cnt[key]
        self._emit_waits(q, deps)
        self.cnt[key] += 16
        v = self.cnt[key]
        sem = self.sem[key]
        self.ops[q].append(lambda E, out=out, in_=in_, sem=sem: E.dma_start(out=out, in_=in_).then_inc(sem, 16))
        self._record((key, v), reads, writes)
        self.n_ops += 1

    def coll(self, in_ap, out_ap, groups, reads=(), writes=()):
        if self.dry:
            return
        deps = self._deps(reads, writes)
        self._emit_waits("pool", deps)
        key = "d_cc"
        self.cnt[key] += 1
        v = self.cnt[key]
        sem = self.sem[key]
        self.ops["pool"].append(lambda E: E.collective_compute(
            "AllGather", ALU.bypass, replica_groups=groups, ins=[in_ap], outs=[out_ap]).then_inc(sem, 1))
        self._record((key, v), reads, writes)
        self.n_ops += 1

    def finish(self):
        deps = {k: v for k, v in self.cnt.items() if k.startswith("d_") and v > 0}
        self._emit_waits("sp", deps)
        ops = self.ops
        with self.nc.Block() as block:
            @block.tensor
            def _(E):
                for f in ops["pe"]:
                    f(E)

            @block.scalar
            def _(E):
                for f in ops["act"]:
                    f(E)

            @block.vector
            def _(E):
                for f in ops["dve"]:
                    f(E)

            @block.gpsimd
            def _(E):
                for f in ops["pool"]:
                    f(E)

            @block.sync
            def _(E):
                for f in ops["sp"]:
                    f(E)


class T:
    def __init__(self, t, n=1, excl=False, name=""):
        self.t = t
        self.b = [Buf(name + str(i), excl) for i in range(n)]
        self.B = self.b[0]


def build_program(debug=0):
    nc = bass.Bass("TRN2", target_bir_lowering=False, num_devices=NCORES)
    dt_in = lambda name, shape: nc.dram_tensor(name, list(shape), F32, kind="ExternalInput").ap()
    dt_out = lambda name, shape: nc.dram_tensor(name, list(shape), F32, kind="ExternalOutput").ap()
    xs_d = dt_in("xs", (4, TT, D))
    xp_d = dt_in("xp", (TT, D))
    ropec_d = dt_in("ropec", (4, 128, TT))
    ropes_d = dt_in("ropes", (4, 128, TT))
    ck_d = dt_in("ck", (NL, 256, 256))
    cv_d = dt_in("cv", (NL, 256, 256))
    cvec_d = dt_in("cvec", (128, KC, 2))
    n1g_d = dt_in("n1g", (128, NL, KC))
    n2g_d = dt_in("n2g", (128, NL, KC))
    bada_d = dt_in("bada", (128, NL, 96))
    qg_d = dt_in("qg", (128, NL))
    kg_d = dt_in("kg", (128, NL))
    sgn_d = dt_in("sgn", (128, NL, 8))
    bsrep_d = dt_in("bsrep", (128, NL, 8, 128))
    wsT_d = dt_in("wsT", (128, NL, 8, 128))
    ong_d = dt_in("ong", (128, NL, KC))
    wr_d = dt_in("wr", (128, KC, NE))
    brrep_d = dt_in("brrep", (128, NE))
    ident_d = dt_in("ident", (128, 128))
    rt_d = dt_in("rt", (128, 128))
    wada_d = dt_in("w_ada", (NL, D, 3 * D))
    win_d = dt_in("w_in", (NL, D, INW))
    wout_d = dt_in("w_out", (NL, D, D))
    fg_d = dt_in("ffn_w_gate", (1, D, DFF))
    fu_d = dt_in("ffn_w_up", (1, D, DFF))
    fd_d = dt_in("ffn_w_down", (1, DFF, D))
    mg_d = dt_in("moe_w_gate", (1, NE, D, DFE))
    mu_d = dt_in("moe_w_up", (1, NE, D, DFE))
    md_d = dt_in("moe_w_down", (1, NE, DFE, D))
    yp_d = dt_out("yp", (TT, D))
    ys_d = dt_out("ys", (2, TT, D))
    nk_d = dt_out("nk", (2, NL, 256, 256))
    nv_d = dt_out("nv", (2, NL, 256, 256))
    bIN = Buf("dram_in")

    with ExitStack() as st:
        P = Prog(nc, st)

        def sb(name, shape, dt, n=1, stack=st):
            return T(stack.enter_context(nc.sbuf_tensor("s_" + name, list(shape), dt)), n, name=name)

        bank = {}
        for nm in ("A0", "A1", "B0", "B1", "C", "Dk", "G", "H"):
            bank[nm] = T(st.enter_context(nc.psum_tensor("ps" + nm, [128, 512], F32)), 1, excl=True, name="ps" + nm)

        ident = sb("ident", (128, 128), F32)
        identb = sb("identb", (128, 128), BF16)
        rt = sb("rt", (128, 128), F32)
        onesb = sb("onesb", (128, 128), BF16)
        epsc = sb("epsc", (128, 1), F32)
        cvec = sb("cvec", (128, KC, 2), F32)
        scb = sb("scb", (128, KC, 2), BF16)
        modraw = sb("modraw", (128, 48, 2), F32)
        modg = sb("modg", (128, 2, 96), F32)
        n1g = sb("n1g", (128, NL, KC), F32)
        n2g = sb("n2g", (128, NL, KC), F32)
        bada = sb("bada", (128, NL, 96), F32)
        qg = sb("qg", (128, NL), F32)
        kg = sb("kg", (128, NL), F32)
        sgn = sb("sgn", (128, NL, 8), F32)
        bsrep = sb("bsrep", (128, 1, 8, 128), F32)
        wsT = sb("wsT", (128, NL, 8, 128), BF16)
        ong = sb("ong", (128, NL, KC), F32)
        wr = sb("wr", (128, KC, NE), F32)
        brrep = sb("brrep", (128, NE), F32)
        mod = sb("mod", (128, NL, 2, 96), F32)
        A1 = sb("A1", (128, NL, 2, KC), F32)
        A2 = sb("A2", (128, NL, 2, KC), F32)

        for (t_, d_) in ((ident, ident_d), (rt, rt_d), (cvec, cvec_d), (n1g, n1g_d), (n2g, n2g_d), (bada, bada_d),
                         (qg, qg_d), (kg, kg_d), (sgn, sgn_d), (ong, ong_d),
                         (wr, wr_d), (brrep, brrep_d)):
            P.dma("sp", t_.t[:], d_, reads=[bIN], writes=[t_.B])
        P.op("dve", lambda E: E.memset(onesb.t[:], 1.0), writes=[onesb.B])
        P.op("dve", lambda E: E.memset(epsc.t[:], EPS), writes=[epsc.B])
        P.op("dve", lambda E: E.tensor_copy(identb.t[:], ident.t[:]), reads=[ident.B], writes=[identb.B])
        P.dma("pool", wsT.t[:], wsT_d, reads=[bIN], writes=[wsT.B])
        P.op("act", lambda E: E.activation(out=scb.t[:], in_=cvec.t[:], func=AF.Silu), reads=[cvec.B], writes=[scb.B])

        NSLOT = 4
        wslots = [sb("wslot%d" % i, (128, 4096), BF16) for i in range(NSLOT)]
        wstate = {"plan": [], "issued": 0, "taken": 0, "released": 0}

        def w_view(i, shape):
            s_ = wslots[i % NSLOT]
            if shape[0] == "in":
                return s_.t[:, 0:KC * shape[1]].rearrange("p (k n) -> p k n", k=KC), s_.B
            return s_.t[:, 0:shape[1] * D].rearrange("p (k n) -> p k n", k=shape[1]), s_.B

        def w_issue_upto(n):
            while wstate["issued"] < min(n, len(wstate["plan"])):
                i = wstate["issued"]
                src, shape = wstate["plan"][i]
                dst, db = w_view(i, shape)
                P.dma("pool", dst, src.rearrange("(k p) n -> p k n", p=128), reads=[bIN], writes=[db])
                wstate["issued"] += 1

        def w_get(src, shape):
            i = wstate["taken"]
            wstate["taken"] += 1
            if P.dry:
                wstate["plan"].append((src, shape))
                return w_view(i, shape)
            assert i < len(wstate["plan"]) and wstate["plan"][i][1] == shape, "weight plan mismatch"
            w_issue_upto(max(wstate["released"] + NSLOT, i + 1))
            assert wstate["issued"] <= wstate["released"] + NSLOT and i - wstate["released"] < NSLOT
            return w_view(i, shape)

        def w_release():
            if P.dry:
                return
            wstate["released"] = wstate["taken"]
            w_issue_upto(wstate["released"] + NSLOT)

        def mm(out_bank, out_ap, lhsT, rhs, start, stop, reads):
            P.op("pe", lambda E: E.matmul(out_ap, lhsT=lhsT, rhs=rhs, start=start, stop=stop), reads=reads, writes=[out_bank.B])

        rr = {"act_dve": 0}

        modst = {"i": 0, "rounds": 0}
        ROUNDS = [(0, 8), (8, 24), (24, 48)]
        PAIRS = [[0, 1], [2, 3], [4, 5], [6, 7]]

        def mod_exchange(ri):
            lo, hi = ROUNDS[ri]
            n = (hi - lo) * 4
            l = lo // 24
            q0, q1 = lo % 24, (hi - 1) % 24 + 1
            P.dma("sp", msend[ri], modraw.t[:, 0:(hi - lo) * 2, :].rearrange("p m k -> p (m k)"), reads=[modraw.B], writes=[bmsend[ri]])
            P.coll(msend[ri], mrecv[ri], PAIRS, reads=[bmsend[ri]], writes=[bmrecv[ri]])
            P.dma("sp", modg.t[:, :, 0:n], mrecv[ri].rearrange("(r p) f -> p r f", p=128), reads=[bmrecv[ri]], writes=[modg.B])
            for r in range(2):
                for cnd in range(2):
                    o_ = mod.t[:, l, cnd, :].rearrange("p (q r c) -> p q r c", q=24, r=2)[:, q0:q1, r, :]
                    b_ = bada.t[:, l, :].rearrange("p (q r c) -> p q r c", q=24, r=2)[:, q0:q1, r, :]
                    i_ = modg.t[:, r, 0:n].rearrange("p (q c k) -> p q c k", c=2, k=2)[:, :, :, cnd]
                    P.op("dve", lambda E, o_=o_, i_=i_, b_=b_: E.tensor_tensor(out=o_, in0=i_, in1=b_, op=ALU.add),
                         reads=[modg.B, bada.B], writes=[mod.B])
            for (A, g_, off, rr_) in ((A1, n1g, 16, (0, 2)), (A2, n2g, 64, (1, 2))):
                if ri in rr_:
                    for cnd in range(2):
                        P.op("dve", lambda E, A=A, g_=g_, off=off, l=l, cnd=cnd: E.scalar_tensor_tensor(
                            out=A.t[:, l, cnd, :], in0=mod.t[:, l, cnd, off:off + 16], scalar=1.0, in1=g_.t[:, l, :],
                            op0=ALU.add, op1=ALU.mult), reads=[mod.B, g_.B], writes=[A.B])
            modst["rounds"] = ri + 1

        def mod_step(n):
            for _ in range(n):
                i = modst["i"]
                if i >= 48:
                    return
                modst["i"] = i + 1
                l, q = divmod(i, 24)
                ri = [k for k, (lo, hi) in enumerate(ROUNDS) if lo <= i < hi][0]
                mi = i - ROUNDS[ri][0]
                w, wb = w_get(wada_d[l, :, q * 256:(q + 1) * 256], ("in", 256))
                bk = bank["G"] if i % 2 == 0 else bank["H"]
                for c2 in range(2):
                    for kc in range(KC):
                        mm(bk, bk.t[:, c2 * 2:c2 * 2 + 2], w[:, kc, c2 * 128:(c2 + 1) * 128], scb.t[:, kc, :],
                           kc == 0, kc == KC - 1, [wb, scb.B])
                w_release()
                P.op("dve", lambda E, mi=mi, bk=bk: E.tensor_copy(modraw.t[:, mi * 2:mi * 2 + 2, :],
                                                                 bk.t[:, 0:4].rearrange("p (c k) -> p c k", c=2)),
                     reads=[bk.B], writes=[modraw.B])
                if i + 1 == ROUNDS[ri][1]:
                    mod_exchange(ri)

        def need_mod(l, which):
            req = 3 if l == 1 else (1 if which < 2 else 2)
            assert modst["rounds"] >= req, ("modulation not exchanged yet", l, which, modst)

        bgc = {"hp": 0, "wo": 0, "ffn": 0}

        def mvec(l, cnd, which):
            need_mod(l, which)
            return mod.t[:, l, cnd, which * 16:(which + 1) * 16]

        xstage = sb("xstage", (128, 1024), F32, n=1)
        kvstage = sb("kvstage", (128, 4, 256), F32)
        xstg = [(xstage.t[:], xstage.B), (kvstage.t[:].rearrange("p b d -> p (b d)"), kvstage.B)]

        def load_x(src, x):
            for blk in range(4):
                for g4 in range(4):
                    st_ap, st_b = xstg[(blk * 2 + g4 // 2) % 2]
                    if g4 % 2 == 0:
                        P.dma("sp", st_ap, src[blk * 128:(blk + 1) * 128, (g4 // 2) * 1024:(g4 // 2 + 1) * 1024], reads=[bIN], writes=[st_b])
                    bk = bank["H"] if g4 % 2 == 0 else bank["G"]
                    for j in range(4):
                        kc = g4 * 4 + j
                        kl = kc % 8
                        P.op("pe", lambda E, bk=bk, j=j, kl=kl, st_ap=st_ap: E.transpose(bk.t[:, j * 128:(j + 1) * 128],
                                                                                        st_ap[:, kl * 128:(kl + 1) * 128], ident.t[:]),
                             reads=[st_b, ident.B], writes=[bk.B])
                    dst = x.t[:, g4 * 4:(g4 + 1) * 4, blk * 128:(blk + 1) * 128]
                    srcp = bk.t[:].rearrange("p (j t) -> p j t", j=4)
                    if g4 % 2 == 0:
                        P.op("dve", lambda E, dst=dst, srcp=srcp: E.tensor_copy(dst, srcp), reads=[bk.B], writes=[x.B])
                    else:
                        P.op("act", lambda E, dst=dst, srcp=srcp: E.activation(out=dst, in_=srcp, func=AF.Copy), reads=[bk.B], writes=[x.B])

        def store_x(x, dst):
            for blk in range(4):
                for g4 in range(4):
                    st_ap, st_b = xstg[(blk * 2 + g4 // 2) % 2]
                    bk = bank["H"] if g4 % 2 == 0 else bank["G"]
                    for j in range(4):
                        kc = g4 * 4 + j
                        P.op("pe", lambda E, bk=bk, j=j, kc=kc, blk=blk: E.transpose(
                            bk.t[:, j * 128:(j + 1) * 128], x.t[:, kc, blk * 128:(blk + 1) * 128], ident.t[:]),
                            reads=[x.B, ident.B], writes=[bk.B])
                    dsts = st_ap[:, (g4 % 2) * 512:(g4 % 2 + 1) * 512]
                    if g4 % 2 == 0:
                        P.op("dve", lambda E, dsts=dsts, bk=bk: E.tensor_copy(dsts, bk.t[:]), reads=[bk.B], writes=[st_b])
                    else:
                        P.op("act", lambda E, dsts=dsts, bk=bk: E.activation(out=dsts, in_=bk.t[:], func=AF.Copy), reads=[bk.B], writes=[st_b])
                        P.dma("sp", dst[blk * 128:(blk + 1) * 128, (g4 // 2) * 1024:(g4 // 2 + 1) * 1024], st_ap, reads=[st_b], writes=[Buf()])

        sqr = sb("sqr", (128, 3, TT), BF16, n=3)
        rstd = sb("rstd", (128, TT), F32)
        tmpf = sb("tmpf", (128, 2, TT), F32, n=2)

        def sum_sq_to_rstd(n_feat, dst):
            G = bank["G"]
            P.op("act", lambda E: E.activation(out=dst.t[:], in_=G.t[:], func=AF.Sqrt, bias=epsc.t[:, 0:1], scale=1.0 / n_feat),
                 reads=[G.B, epsc.B], writes=[dst.B])
            P.op("dve", lambda E: E.reciprocal(out=dst.t[:], in_=dst.t[:]), reads=[dst.B], writes=[dst.B])

        def Avec1(l, cnd):
            need_mod(l, 1)
            return A1.t[:, l, cnd, :]

        def Avec2(l, cnd):
            need_mod(l, 4)
            return A2.t[:, l, cnd, :]

        def norm_mod(x, h, Avec, Bvec):
            G = bank["G"]
            for kc in range(KC):
                i = kc % 2
                P.op("act", lambda E, kc=kc, i=i: E.activation(out=sqr.t[:, i, :], in_=x.t[:, kc, :], func=AF.Square),
                     reads=[x.B], writes=[sqr.b[i]])
                mm(G, G.t[:], onesb.t[:], sqr.t[:, i, :], kc == 0, kc == KC - 1, [onesb.B, sqr.b[i]])
            sum_sq_to_rstd(float(D), rstd)
            for kc in range(KC):
                i = kc % 2
                P.op("dve", lambda E, kc=kc, i=i: E.scalar_tensor_tensor(
                    out=tmpf.t[:, i, :], in0=x.t[:, kc, :], scalar=Avec[:, kc:kc + 1], in1=rstd.t[:], op0=ALU.mult, op1=ALU.mult),
                    reads=[x.B, rstd.B, A1.B, A2.B], writes=[tmpf.b[i]])
                P.op("act", lambda E, kc=kc, i=i: E.activation(out=h.t[:, kc, :], in_=tmpf.t[:, i, :], func=AF.Identity,
                                                              bias=Bvec[:, kc:kc + 1], scale=1.0),
                     reads=[tmpf.b[i], mod.B], writes=[h.B])

        hq = sb("hq", (128, TT), F32)
        hq1 = sb("hq1", (128, TT), F32)
        hq2 = [hq, hq1]
        hrs = sb("hrs", (128, TT), F32)
        r1 = sb("r1", (128, TT), F32)
        r2 = sb("r2", (128, TT), F32)
        qb = sb("qb", (128, TT), BF16)
        qb1 = sb("qb1", (128, TT), BF16)
        qb2 = [qb, qb1]
        ropec = sb("ropec", (128, TT), F32)
        ropes = sb("ropes", (128, TT), F32)

        def head_norm(ps, gain_ap, out_f32=None, out_bf=None):
            G = bank["G"]
            P.op("act", lambda E: E.activation(out=sqr.t[:, 0, :], in_=ps.t[:], func=AF.Square), reads=[ps.B], writes=[sqr.b[0]])
            mm(G, G.t[:], onesb.t[:], sqr.t[:, 0, :], True, True, [onesb.B, sqr.b[0]])
            sum_sq_to_rstd(128.0, hrs)
            if out_f32 is not None:
                P.op("dve", lambda E: E.scalar_tensor_tensor(out=out_f32.t[:], in0=ps.t[:], scalar=gain_ap, in1=hrs.t[:],
                                                             op0=ALU.mult, op1=ALU.mult), reads=[ps.B, hrs.B, qg.B, kg.B], writes=[out_f32.B])
            else:
                P.op("dve", lambda E: E.scalar_tensor_tensor(out=out_bf, in0=ps.t[:], scalar=gain_ap, in1=hrs.t[:],
                                                             op0=ALU.mult, op1=ALU.mult), reads=[ps.B, hrs.B, qg.B, kg.B], writes=[])

        def rope_to(src_f32, out_ap, out_buf):
            H = bank["H"]
            mm(H, H.t[:], rt.t[:], src_f32.t[:], True, True, [rt.B, src_f32.B])
            P.op("dve", lambda E: E.tensor_tensor(out=src_f32.t[:], in0=src_f32.t[:], in1=ropec.t[:], op=ALU.mult),
                 reads=[src_f32.B, ropec.B], writes=[src_f32.B])
            P.op("dve", lambda E: E.tensor_tensor(out=tmpf.t[:, 1, :], in0=H.t[:], in1=ropes.t[:], op=ALU.mult),
                 reads=[H.B, ropes.B], writes=[tmpf.b[1]])
            P.op("dve", lambda E: E.tensor_tensor(out=out_ap, in0=src_f32.t[:], in1=tmpf.t[:, 1, :], op=ALU.add),
                 reads=[src_f32.B, tmpf.b[1]], writes=[out_buf])


        def plan_kv(l):
            return [(win_d[l, :, 1024:1280], ("in", 256)), (win_d[l, :, 1280:1536], ("in", 256))]

        def emit_kv(l, h, KTb, Vb, kcol0, vblk0, rope, cache_out=None):
            wk, wkb = w_get(win_d[l, :, 1024:1280], ("in", 256))
            for kvh in range(2):
                bk = bank["A%d" % kvh]
                for kc in range(KC):
                    mm(bk, bk.t[:], wk[:, kc, kvh * 128:(kvh + 1) * 128], h.t[:, kc, :], kc == 0, kc == KC - 1, [wkb, h.B])
                dst = KTb.t[:, kvh, kcol0:kcol0 + TT]
                if rope:
                    head_norm(bk, kg.t[:, l:l + 1], out_f32=hq)
                    rope_to(hq, dst, KTb.B)
                else:
                    head_norm(bk, kg.t[:, l:l + 1], out_f32=hq)
                    P.op("act", lambda E, dst=dst: E.activation(out=dst, in_=hq.t[:], func=AF.Copy), reads=[hq.B], writes=[KTb.B])
                    if cache_out is not None:
                        H = bank["H"]
                        for blk in range(4):
                            P.op("pe", lambda E, blk=blk: E.transpose(H.t[:, blk * 128:(blk + 1) * 128], hq.t[:, blk * 128:(blk + 1) * 128], ident.t[:]),
                                 reads=[hq.B, ident.B], writes=[H.B])
                        P.op("dve", lambda E, kvh=kvh: E.tensor_copy(kvstage.t[:, :, kvh * 128:(kvh + 1) * 128],
                                                                     H.t[:].rearrange("p (b d) -> p b d", b=4)),
                             reads=[H.B], writes=[kvstage.B])
            if cache_out is not None:
                for blk in range(4):
                    P.dma("sp", nk_d[blk // 2, l, (blk % 2) * 128:(blk % 2 + 1) * 128, :], kvstage.t[:, blk, :],
                          reads=[kvstage.B], writes=[Buf()])
            w_release()
            wv, wvb = w_get(win_d[l, :, 1280:1536], ("in", 256))
            for blk in range(4):
                bk = bank["B%d" % (blk // 2)]
                o_ap = bk.t[:, (blk % 2) * 256:(blk % 2 + 1) * 256]
                for kc in range(KC):
                    mm(bk, o_ap, h.t[:, kc, blk * 128:(blk + 1) * 128], wv[:, kc, :], kc == 0, kc == KC - 1, [wvb, h.B])
                if blk % 2 == 1:
                    P.op("act", lambda E, bk=bk, blk=blk: E.activation(out=Vb.t[:, vblk0 + blk - 1:vblk0 + blk + 1, :],
                                                                      in_=bk.t[:].rearrange("p (b d) -> p b d", b=2), func=AF.Copy),
                         reads=[bk.B], writes=[Vb.B])
                    if cache_out is not None:
                        P.op("dve", lambda E, bk=bk, blk=blk: E.tensor_copy(kvstage.t[:, blk - 1:blk + 1, :],
                                                                            bk.t[:].rearrange("p (b d) -> p b d", b=2)),
                             reads=[bk.B], writes=[kvstage.B])
            w_release()
            if cache_out is not None:
                for blk in range(4):
                    P.dma("sp", nv_d[blk // 2, l, (blk % 2) * 128:(blk % 2 + 1) * 128, :], kvstage.t[:, blk, :],
                          reads=[kvstage.B], writes=[Buf()])

        PT = sb("PT", (128, 4, TT), BF16, n=4)
        uf = sb("uf", (128, TT), F32)
        ghat = sb("ghat", (128, 4, 256), BF16)
        gss = sb("gss", (128, 8), F32)
        gsq = sb("gsq", (128, 128), F32)
        ssa = sb("ssa", (128, TT), F32)
        sss = sb("sss", (128, TT), F32)
        SCALE = 1.0 / float(np.sqrt(128.0))

        def plan_mixer(l):
            items = []
            for hp in range(4):
                items.append((win_d[l, :, 2560 + hp * 256:2560 + (hp + 1) * 256], ("in", 256)))
                items.append((win_d[l, :, 1536 + hp * 256:1536 + (hp + 1) * 256], ("in", 256)))
                items.append((win_d[l, :, hp * 256:(hp + 1) * 256], ("in", 256)))
            for pc in range(8):
                items.append((wout_d[l, :, pc * 256:(pc + 1) * 256], ("in", 256)))
            return items

        deferred = []

        def flush():
            for f in deferred:
                f()
            del deferred[:]

        def accum_sumsq(src_f32_ap, src_buf, acc, first, si):
            G = bank["G"]
            if any(getattr(f, "si", None) == si for f in deferred):
                flush()
            P.op("act", lambda E: E.activation(out=sqr.t[:, si, :], in_=src_f32_ap, func=AF.Square), reads=[src_buf], writes=[sqr.b[si]])

            def part2():
                mm(G, G.t[:], onesb.t[:], sqr.t[:, si, :], True, True, [onesb.B, sqr.b[si]])
                if first:
                    P.op("dve", lambda E: E.tensor_copy(acc.t[:], G.t[:]), reads=[G.B], writes=[acc.B])
                else:
                    P.op("dve", lambda E: E.tensor_tensor(out=acc.t[:], in0=acc.t[:], in1=G.t[:], op=ALU.add), reads=[G.B, acc.B], writes=[acc.B])
            part2.si = si
            deferred.append(part2)

        def emit_mixer(l, cnd, x, h, o, KTb, Vb, groups, rope):
            P.dma("sp", bsrep.t[:, 0], bsrep_d[:, l], reads=[bIN], writes=[bsrep.B])
            for hp in range(4):
                c4 = hp
                wg_, wgb = w_get(win_d[l, :, 2560 + hp * 256:2560 + (hp + 1) * 256], ("in", 256))
                for blk in range(4):
                    bk = bank["A%d" % (blk % 2)]
                    o_ap = bk.t[:, 0:256]
                    for kc in range(KC):
                        mm(bk, o_ap, h.t[:, kc, blk * 128:(blk + 1) * 128], wg_[:, kc, :], kc == 0, kc == KC - 1, [wgb, h.B])
                    P.op("dve", lambda E, c4=c4: E.memset(gss.t[:, c4 * 2:c4 * 2 + 2], 0.0), reads=[gss.B], writes=[gss.B])
                    for hh in range(2):
                        hd = c4 * 2 + hh
                        P.op("act", lambda E, bk=bk, hh=hh, hd=hd: E.activation(out=gsq.t[:], in_=bk.t[:, hh * 128:(hh + 1) * 128], func=AF.Square,
                                                                               accum_out=gss.t[:, hd:hd + 1]),
                             reads=[bk.B], writes=[gsq.B, gss.B])
                    P.op("act", lambda E, c4=c4: E.activation(out=gss.t[:, c4 * 2:c4 * 2 + 2], in_=gss.t[:, c4 * 2:c4 * 2 + 2], func=AF.Sqrt,
                                                              bias=epsc.t[:, 0:1], scale=1.0 / 128.0), reads=[gss.B, epsc.B], writes=[gss.B])
                    P.op("dve", lambda E, c4=c4: E.reciprocal(out=gss.t[:, c4 * 2:c4 * 2 + 2], in_=gss.t[:, c4 * 2:c4 * 2 + 2]),
                         reads=[gss.B], writes=[gss.B])
                    for hh in range(2):
                        hd = c4 * 2 + hh
                        P.op("dve", lambda E, bk=bk, hh=hh, hd=hd, blk=blk: E.tensor_scalar(
                            out=ghat.t[:, blk, hh * 128:(hh + 1) * 128], in0=bk.t[:, hh * 128:(hh + 1) * 128],
                            scalar1=gss.t[:, hd:hd + 1], scalar2=None, op0=ALU.mult), reads=[bk.B, gss.B], writes=[ghat.B])
                w_release()
                flush()
                wu_, wub = w_get(win_d[l, :, 1536 + hp * 256:1536 + (hp + 1) * 256], ("in", 256))
                wq_, wqb = w_get(win_d[l, :, hp * 256:(hp + 1) * 256], ("in", 256))
                C, Dk = bank["C"], bank["Dk"]

                def sgu_head(hh):
                    hd = hp * 2 + hh
                    bkA = bank["A0"]
                    for kc in range(KC):
                        mm(bkA, bkA.t[:], wu_[:, kc, hh * 128:(hh + 1) * 128], h.t[:, kc, :], kc == 0, kc == KC - 1, [wub, h.B])
                    P.op("act", lambda E: E.activation(out=uf.t[:], in_=bkA.t[:], func=AF.Copy), reads=[bkA.B], writes=[uf.B])
                    bkB = bank["B%d" % hh]
                    for blk in range(4):
                        mm(bkB, bkB.t[:, blk * 128:(blk + 1) * 128], ghat.t[:, blk, hh * 128:(hh + 1) * 128], wsT.t[:, l, hd, :],
                           True, True, [ghat.B, wsT.B])
                    for blk in range(4):
                        P.op("dve", lambda E, blk=blk: E.scalar_tensor_tensor(
                            out=r1.t[:, blk * 128:(blk + 1) * 128], in0=bkB.t[:, blk * 128:(blk + 1) * 128], scalar=sgn.t[:, l, hd:hd + 1],
                            in1=bsrep.t[:, 0, hd, :], op0=ALU.mult, op1=ALU.add), reads=[bkB.B, sgn.B, bsrep.B], writes=[r1.B])
                    P.op("dve", lambda E: E.tensor_tensor(out=r2.t[:], in0=r1.t[:], in1=uf.t[:], op=ALU.mult), reads=[r1.B, uf.B], writes=[r2.B])
                    P.op("act", lambda E: E.activation(out=o.t[:, 8 + hd, :], in_=r2.t[:], func=AF.Copy, scale=ong.t[:, l, 8 + hd:9 + hd]),
                         reads=[r2.B, ong.B], writes=[o.B])
                    accum_sumsq(r2.t[:], r2.B, sss, hd == 0, 1)

                def q_proj_norm(hh):
                    bkQ = bank["A1"]
                    for kc in range(KC):
                        mm(bkQ, bkQ.t[:], wq_[:, kc, hh * 128:(hh + 1) * 128], h.t[:, kc, :], kc == 0, kc == KC - 1, [wqb, h.B])
                    head_norm(bkQ, qg.t[:, l:l + 1], out_f32=hq2[hh])

                def q_finish(hh):
                    if rope:
                        rope_to(hq2[hh], qb2[hh].t[:], qb2[hh].B)
                    else:
                        P.op("act", lambda E: E.activation(out=qb2[hh].t[:], in_=hq2[hh].t[:], func=AF.Copy), reads=[hq2[hh].B], writes=[qb2[hh].B])

                def attention(hh, hook=None):
                    hd = hp * 2 + hh
                    kvh = hd // 4
                    qbh = qb2[hh]
                    for gi, (q0, q1, kblocks) in enumerate(groups):
                        nkb = len(kblocks)

                        def score(ji, q0=q0, q1=q1, kblocks=kblocks):
                            j = kblocks[ji]
                            bS = bank["B%d" % (ji % 2)]
                            mm(bS, bS.t[:, q0:q1], KTb.t[:, kvh, j * 128:(j + 1) * 128], qbh.t[:, q0:q1], True, True, [KTb.B, qbh.B])
                            P.op("act", lambda E: E.activation(out=PT.t[:, ji % 4, q0:q1], in_=bS.t[:, q0:q1], func=AF.Exp, scale=SCALE),
                                 reads=[bS.B], writes=[PT.b[ji % 4]])
                        score(0)
                        for ji in range(nkb):
                            if ji + 1 < nkb:
                                score(ji + 1)
                            j = kblocks[ji]
                            mm(C, C.t[:, q0:q1], Vb.t[:, j, kvh * 128:(kvh + 1) * 128], PT.t[:, ji % 4, q0:q1], ji == 0, ji == nkb - 1,
                               [Vb.B, PT.b[ji % 4]])
                            mm(Dk, Dk.t[:, q0:q1], onesb.t[:], PT.t[:, ji % 4, q0:q1], ji == 0, ji == nkb - 1, [onesb.B, PT.b[ji % 4]])
                            if hook is not None and gi == 0 and ji == min(3, nkb - 1):
                                hook()
                    P.op("dve", lambda E: E.reciprocal(out=r1.t[:], in_=Dk.t[:]), reads=[Dk.B], writes=[r1.B])
                    P.op("dve", lambda E: E.tensor_tensor(out=r2.t[:], in0=C.t[:], in1=r1.t[:], op=ALU.mult), reads=[C.B, r1.B], writes=[r2.B])
                    P.op("act", lambda E: E.activation(out=o.t[:, hd, :], in_=r2.t[:], func=AF.Copy, scale=ong.t[:, l, hd:hd + 1]),
                         reads=[r2.B, ong.B], writes=[o.B])
                    accum_sumsq(r2.t[:], r2.B, ssa, hd == 0, 2)

                sgu_head(0)
                q_proj_norm(0)
                sgu_head(1)
                flush()
                q_finish(0)
                q_proj_norm(1)
                flush()
                attention(0, hook=lambda: q_finish(1))
                attention(1)
                w_release()
                mod_step(bgc["hp"])
            flush()
            rsa, rss = ssa, sss
            for (acc, dstr) in ((ssa, ssa), (sss, sss)):
                P.op("act", lambda E, acc=acc, dstr=dstr: E.activation(out=dstr.t[:], in_=acc.t[:], func=AF.Sqrt, bias=epsc.t[:, 0:1], scale=1.0 / 1024.0),
                     reads=[acc.B, epsc.B], writes=[dstr.B])
                P.op("dve", lambda E, dstr=dstr: E.reciprocal(out=dstr.t[:], in_=dstr.t[:]), reads=[dstr.B], writes=[dstr.B])
            g1 = mvec(l, cnd, 2)
            for pc in range(8):
                wo_, wob = w_get(wout_d[l, :, pc * 256:(pc + 1) * 256], ("in", 256))
                for c2 in range(2):
                    oc = pc * 2 + c2
                    bkA, bkB = bank["A%d" % c2], bank["B%d" % c2]
                    for kc in range(8):
                        mm(bkA, bkA.t[:], wo_[:, kc, c2 * 128:(c2 + 1) * 128], o.t[:, kc, :], kc == 0, kc == 7, [wob, o.B])
                    for kc in range(8, 16):
                        mm(bkB, bkB.t[:], wo_[:, kc, c2 * 128:(c2 + 1) * 128], o.t[:, kc, :], kc == 8, kc == 15, [wob, o.B])
                    if c2 == 1:
                        w_release()
                        mod_step(bgc["wo"])
                    P.op("dve", lambda E, bkA=bkA: E.tensor_tensor(out=r1.t[:], in0=bkA.t[:], in1=rsa.t[:], op=ALU.mult), reads=[bkA.B, rsa.B], writes=[r1.B])
                    P.op("dve", lambda E, bkB=bkB: E.tensor_tensor(out=r2.t[:], in0=bkB.t[:], in1=rss.t[:], op=ALU.mult), reads=[bkB.B, rss.B], writes=[r2.B])
                    P.op("dve", lambda E: E.tensor_tensor(out=r1.t[:], in0=r1.t[:], in1=r2.t[:], op=ALU.add), reads=[r1.B, r2.B], writes=[r1.B])
                    P.op("dve", lambda E, oc=oc: E.scalar_tensor_tensor(out=x.t[:, oc, :], in0=r1.t[:], scalar=g1[:, oc:oc + 1], in1=x.t[:, oc, :],
                                                                       op0=ALU.mult, op1=ALU.add), reads=[r1.B, mod.B, x.B], writes=[x.B])

        sg = sb("sg", (128, 2, TT), F32, n=2)
        actb = sb("actb", (128, 2, 4, TT), BF16, n=2)
        cwrep = sb("cwrep", (128, TT), F32)
        cw4 = sb("cw4", (128, 4, NE), F32)
        lg = sb("lg", (128, NE), F32)
        lg8 = sb("lg8", (128, 8), F32)
        cw = sb("cw", (128, NE), F32)
        cwb = sb("cwb", (128, 128), F32)
        tcol = sb("tcol", (128, 4), F32)
        wrp = sb("wrp", (128, KC, NE), F32)

        def ffn_panel_list(l):
            out = []
            if l == 0:
                for p in range(DFF // 256):
                    out.append((fg_d[0][:, p * 256:(p + 1) * 256], fu_d[0][:, p * 256:(p + 1) * 256], fd_d[0][p * 256:(p + 1) * 256, :], None))
            else:
                for e in range(NE):
                    for p in range(DFE // 256):
                        out.append((mg_d[0, e][:, p * 256:(p + 1) * 256], mu_d[0, e][:, p * 256:(p + 1) * 256],
                                    md_d[0, e][p * 256:(p + 1) * 256, :], e))
            assert len(out) % 2 == 0
            return out

        def plan_ffn(l):
            pl = ffn_panel_list(l)
            items = []
            for pp in range(len(pl) // 2):
                for half in range(2):
                    g_, u_, d_, e = pl[pp * 2 + half]
                    items.append((g_, ("in", 256)))
                    items.append((u_, ("in", 256)))
                for half in range(2):
                    items.append((pl[pp * 2 + half][2], ("rows", 2)))
            return items

        def emit_ffn_panels(l, x, h2, g2):
            pl = ffn_panel_list(l)
            cur_e = None
            for pp in range(len(pl) // 2):
                pi = pp % 2
                for half in range(2):
                    e = pl[pp * 2 + half][3]
                    if e is not None and e != cur_e:
                        emit_cwrep(e)
                        cur_e = e
                    wg_, wgb = w_get(pl[pp * 2 + half][0], ("in", 256))
                    for c2 in range(2):
                        bkG = bank["A%d" % c2]
                        for kc in range(KC):
                            mm(bkG, bkG.t[:], wg_[:, kc, c2 * 128:(c2 + 1) * 128], h2.t[:, kc, :], kc == 0, kc == KC - 1, [wgb, h2.B])
                    w_release()
                    wu_, wub = w_get(pl[pp * 2 + half][1], ("in", 256))
                    for c2 in range(2):
                        bkU = bank["B%d" % c2]
                        for kc in range(KC):
                            mm(bkU, bkU.t[:], wu_[:, kc, c2 * 128:(c2 + 1) * 128], h2.t[:, kc, :], kc == 0, kc == KC - 1, [wub, h2.B])
                    w_release()
                    for c2 in range(2):
                        bkG, bkU = bank["A%d" % c2], bank["B%d" % c2]
                        P.op("act", lambda E, bkG=bkG, c2=c2: E.activation(out=sg.t[:, c2, :], in_=bkG.t[:], func=AF.Silu), reads=[bkG.B], writes=[sg.b[c2]])
                        if e is not None:
                            P.op("dve", lambda E, c2=c2: E.tensor_tensor(out=sg.t[:, c2, :], in0=sg.t[:, c2, :], in1=cwrep.t[:], op=ALU.mult),
                                 reads=[sg.b[c2], cwrep.B], writes=[sg.b[c2]])
                        P.op("dve", lambda E, bkU=bkU, c2=c2, pi=pi, half=half: E.tensor_tensor(out=actb.t[:, pi, half * 2 + c2, :], in0=bkU.t[:], in1=sg.t[:, c2, :], op=ALU.mult),
                             reads=[bkU.B, sg.b[c2]], writes=[actb.b[pi]])
                wd0, wdb0 = w_get(pl[pp * 2][2], ("rows", 2))
                wd1, wdb1 = w_get(pl[pp * 2 + 1][2], ("rows", 2))
                for oc in range(KC):
                    bk = bank["C"] if oc % 2 == 0 else bank["Dk"]
                    for c4 in range(4):
                        wd_, wdb = (wd0, wdb0) if c4 < 2 else (wd1, wdb1)
                        mm(bk, bk.t[:], wd_[:, c4 % 2, oc * 128:(oc + 1) * 128], actb.t[:, pi, c4, :], c4 == 0, c4 == 3, [wdb, actb.b[pi]])
                    P.op("dve", lambda E, bk=bk, oc=oc: E.scalar_tensor_tensor(out=x.t[:, oc, :], in0=bk.t[:], scalar=g2[:, oc:oc + 1], in1=x.t[:, oc, :],
                                                                              op0=ALU.mult, op1=ALU.add), reads=[bk.B, mod.B, x.B], writes=[x.B])
                w_release()
                mod_step(bgc["ffn"])

        def emit_router(l, cnd, x):
            A2v = Avec2(l, cnd)
            sh2 = mvec(l, cnd, 3)
            for kc in range(KC):
                P.op("dve", lambda E, kc=kc: E.tensor_scalar(out=wrp.t[:, kc, :], in0=wr.t[:, kc, :], scalar1=A2v[:, kc:kc + 1], scalar2=None, op0=ALU.mult),
                     reads=[wr.B, A2.B], writes=[wrp.B])
            H, G = bank["H"], bank["G"]
            for kc in range(KC):
                P.op("dve", lambda E, kc=kc: E.tensor_scalar(out=gsq.t[:], in0=ident.t[:], scalar1=0.0, scalar2=sh2[:, kc:kc + 1], op0=ALU.mult, op1=ALU.add),
                     reads=[ident.B, mod.B, gsq.B], writes=[gsq.B])
                mm(G, G.t[:, 0:NE], gsq.t[:], wr.t[:, kc, :], kc == 0, kc == KC - 1, [gsq.B, wr.B])
            P.op("dve", lambda E: E.tensor_tensor(out=lg8.t[:], in0=G.t[:, 0:NE], in1=brrep.t[:], op=ALU.add), reads=[G.B, brrep.B], writes=[lg8.B])
            for blk in range(4):
                for kc in range(KC):
                    mm(H, H.t[:, 0:NE], x.t[:, kc, blk * 128:(blk + 1) * 128], wrp.t[:, kc, :], kc == 0, kc == KC - 1, [x.B, wrp.B])
                mm(G, G.t[:, 0:1], rstd.t[:, blk * 128:(blk + 1) * 128], ident.t[:, 0:1], True, True, [rstd.B, ident.B])
                P.op("dve", lambda E: E.tensor_copy(tcol.t[:, 0:1], G.t[:, 0:1]), reads=[G.B], writes=[tcol.B])
                P.op("dve", lambda E: E.scalar_tensor_tensor(out=lg.t[:], in0=H.t[:, 0:NE], scalar=tcol.t[:, 0:1], in1=lg8.t[:], op0=ALU.mult, op1=ALU.add),
                     reads=[H.B, tcol.B, lg8.B], writes=[lg.B])
                P.op("dve", lambda E: E.tensor_reduce(out=tcol.t[:, 1:2], in_=lg.t[:], axis=mybir.AxisListType.X, op=ALU.max), reads=[lg.B, tcol.B], writes=[tcol.B])
                P.op("dve", lambda E: E.tensor_scalar(out=cw.t[:], in0=lg.t[:], scalar1=tcol.t[:, 1:2], scalar2=-1e30, op0=ALU.is_ge, op1=ALU.mult),
                     reads=[lg.B, tcol.B], writes=[cw.B])
                P.op("dve", lambda E: E.tensor_tensor(out=cw.t[:], in0=cw.t[:], in1=lg.t[:], op=ALU.add), reads=[cw.B, lg.B], writes=[cw.B])
                P.op("dve", lambda E: E.tensor_reduce(out=tcol.t[:, 2:3], in_=cw.t[:], axis=mybir.AxisListType.X, op=ALU.max), reads=[cw.B, tcol.B], writes=[tcol.B])
                P.op("dve", lambda E: E.tensor_scalar(out=cw.t[:], in0=lg.t[:], scalar1=tcol.t[:, 2:3], scalar2=None, op0=ALU.is_ge), reads=[lg.B, tcol.B], writes=[cw.B])
                P.op("dve", lambda E: E.tensor_scalar(out=tcol.t[:, 3:4], in0=tcol.t[:, 1:2], scalar1=-1.0, scalar2=None, op0=ALU.mult), reads=[tcol.B], writes=[tcol.B])
                P.op("act", lambda E: E.activation(out=lg.t[:], in_=lg.t[:], func=AF.Exp, bias=tcol.t[:, 3:4], scale=1.0), reads=[lg.B, tcol.B], writes=[lg.B])
                P.op("dve", lambda E: E.tensor_tensor(out=cw.t[:], in0=cw.t[:], in1=lg.t[:], op=ALU.mult), reads=[cw.B, lg.B], writes=[cw.B])
                P.op("dve", lambda E: E.tensor_reduce(out=tcol.t[:, 0:1], in_=cw.t[:], axis=mybir.AxisListType.X, op=ALU.add), reads=[cw.B, tcol.B], writes=[tcol.B])
                P.op("dve", lambda E: E.reciprocal(out=tcol.t[:, 0:1], in_=tcol.t[:, 0:1]), reads=[tcol.B], writes=[tcol.B])
                P.op("dve", lambda E, blk=blk: E.tensor_scalar(out=cw4.t[:, blk, :], in0=cw.t[:], scalar1=tcol.t[:, 0:1], scalar2=None, op0=ALU.mult),
                     reads=[cw.B, tcol.B], writes=[cw4.B])

        def emit_cwrep(e):
            bk = bank["H"]
            for blk in range(4):
                P.op("dve", lambda E, blk=blk: E.tensor_scalar(out=cwb.t[:], in0=ident.t[:], scalar1=0.0, scalar2=cw4.t[:, blk, e:e + 1], op0=ALU.mult, op1=ALU.add),
                     reads=[ident.B, cw4.B, cwb.B], writes=[cwb.B])
                mm(bk, bk.t[:, blk * 128:(blk + 1) * 128], cwb.t[:], ident.t[:], True, True, [cwb.B, ident.B])
            P.op("act", lambda E: E.activation(out=cwrep.t[:], in_=bk.t[:], func=AF.Copy), reads=[bk.B], writes=[cwrep.B])

        def emit_ffn(l, cnd, x, h2):
            g2 = mvec(l, cnd, 5)
            norm_mod(x, h2, Avec2(l, cnd), mvec(l, cnd, 3))
            if l == 1:
                emit_router(l, cnd, x)
            emit_ffn_panels(l, x, h2, g2)

        xA = sb("xA", (128, KC, TT), F32)
        xpark = nc.dram_tensor("xpark", [2, 128, KC, TT], F32, kind="Internal").ap()
        bpark = [Buf("xpark0"), Buf("xpark1")]
        kvsend = nc.dram_tensor("kvsend", [128, 4096], BF16, kind="Internal").ap()
        kvrecv = nc.dram_tensor("kvrecv", [256, 4096], BF16, addr_space="Local", kind="Internal").ap()
        bsend, brecv = Buf("kvsend"), Buf("kvrecv")
        kvsend0 = nc.dram_tensor("kvsend0", [128, 4096], BF16, kind="Internal").ap()
        kvrecv0 = nc.dram_tensor("kvrecv0", [256, 4096], BF16, addr_space="Local", kind="Internal").ap()
        bsend0, brecv0 = Buf("kvsend0"), Buf("kvrecv0")
        msend = [nc.dram_tensor("msend%d" % k, [128, (hi - lo) * 4], F32, kind="Internal").ap() for k, (lo, hi) in enumerate(ROUNDS)]
        mrecv = [nc.dram_tensor("mrecv%d" % k, [256, (hi - lo) * 4], F32, addr_space="Local", kind="Internal").ap()
                 for k, (lo, hi) in enumerate(ROUNDS)]
        bmsend = [Buf("msend%d" % k) for k in range(3)]
        bmrecv = [Buf("mrecv%d" % k) for k in range(3)]
        hb = sb("hb", (128, KC, TT), BF16)
        ob = sb("ob", (128, KC, TT), BF16)
        KT0 = sb("KT0", (128, 2, 2304), BF16)
        V0 = sb("V0", (128, 18, 256), BF16)
        KT1 = sb("KT1", (128, 2, 2304), BF16)
        V1 = sb("V1", (128, 18, 256), BF16)
        cstage = T(xstage.t[:, 0:512].rearrange("p (r f) -> p r f", r=2), 1)
        cstage.b = xstage.b
        cstage.B = xstage.B

        def load_rope(t):
            P.dma("sp", ropec.t[:], ropec_d[t], reads=[bIN], writes=[ropec.B])
            P.dma("sp", ropes.t[:], ropes_d[t], reads=[bIN], writes=[ropes.B])

        def load_cache(l, KTb, Vb):
            P.dma("sp", cstage.t, ck_d[l].rearrange("(r p) f -> p r f", p=128), reads=[bIN], writes=[cstage.B])
            H = bank["H"]
            for kvh in range(2):
                for r in range(2):
                    P.op("pe", lambda E, kvh=kvh, r=r: E.transpose(H.t[:, (kvh * 2 + r) * 128:(kvh * 2 + r + 1) * 128],
                                                                   cstage.t[:, r, kvh * 128:(kvh + 1) * 128], ident.t[:]),
                         reads=[cstage.B, ident.B], writes=[H.B])
            P.op("act", lambda E: E.activation(out=KTb.t[:, :, 0:256], in_=H.t[:].rearrange("p (k t) -> p k t", k=2), func=AF.Copy),
                 reads=[H.B], writes=[KTb.B])
            P.dma("pool", Vb.t[:, 0:2, :], cv_d[l].rearrange("(r p) f -> p r f", p=128), reads=[bIN], writes=[Vb.B])

        pgroups = [(0, 256, [0, 1]), (256, 512, [2, 3])]
        sgroups = [(0, 512, list(range(18)))]

        def schedule():
            bgc.update(hp=0, wo=0, ffn=0)
            mod_step(8)
            load_cache(0, KT0, V0)
            load_cache(1, KT1, V1)
            for t in range(2):
                load_x(xs_d[t], xA)
                load_rope(t)
                norm_mod(xA, hb, Avec1(0, 1), mvec(0, 1, 0))
                emit_kv(0, hb, KT0, V0, 256 + t * 512, 2 + t * 4, rope=True)
                mod_step(8)
            P.dma("sp", kvsend0[:, 0:2048].rearrange("p (k n) -> p k n", k=2), KT0.t[:, :, 256:1280], reads=[KT0.B], writes=[bsend0])
            P.dma("sp", kvsend0[:, 2048:4096].rearrange("p (b d) -> p b d", b=8), V0.t[:, 2:10, :], reads=[V0.B], writes=[bsend0])
            P.coll(kvsend0, kvrecv0, PAIRS, reads=[bsend0], writes=[brecv0])
            for r in range(2):
                P.dma("sp", KT0.t[:, :, 256 + r * 1024:1280 + r * 1024],
                      kvrecv0[r * 128:(r + 1) * 128, 0:2048].rearrange("p (k n) -> p k n", k=2), reads=[brecv0], writes=[KT0.B])
                P.dma("sp", V0.t[:, 2 + r * 8:10 + r * 8, :],
                      kvrecv0[r * 128:(r + 1) * 128, 2048:4096].rearrange("p (b d) -> p b d", b=8), reads=[brecv0], writes=[V0.B])
            bgc.update(hp=2, wo=1, ffn=1)
            for t in (0, 1):
                load_x(xs_d[t], xA)
                load_rope(t)
                norm_mod(xA, hb, Avec1(0, 1), mvec(0, 1, 0))
                emit_mixer(0, 1, xA, hb, ob, KT0, V0, sgroups, rope=True)
                emit_ffn(0, 1, xA, hb)
                norm_mod(xA, hb, Avec1(1, 1), mvec(1, 1, 0))
                emit_kv(1, hb, KT1, V1, 256 + t * 512, 2 + t * 4, rope=True)
                P.dma("sp", xpark[t], xA.t[:], reads=[xA.B], writes=[bpark[t]])
            P.dma("sp", kvsend[:, 0:2048].rearrange("p (k n) -> p k n", k=2), KT1.t[:, :, 256:1280], reads=[KT1.B], writes=[bsend])
            P.dma("sp", kvsend[:, 2048:4096].rearrange("p (b d) -> p b d", b=8), V1.t[:, 2:10, :], reads=[V1.B], writes=[bsend])
            P.coll(kvsend, kvrecv, [[0, 1], [2, 3], [4, 5], [6, 7]], reads=[bsend], writes=[brecv])
            load_x(xp_d, xA)
            for l in range(NL):
                norm_mod(xA, hb, Avec1(l, 0), mvec(l, 0, 0))
                emit_kv(l, hb, KT0, V0, 0, 0, rope=False, cache_out=True)
                emit_mixer(l, 0, xA, hb, ob, KT0, V0, pgroups, rope=False)
                emit_ffn(l, 0, xA, hb)
            store_x(xA, yp_d)
            for r in range(2):
                P.dma("sp", KT1.t[:, :, 256 + r * 1024:1280 + r * 1024],
                      kvrecv[r * 128:(r + 1) * 128, 0:2048].rearrange("p (k n) -> p k n", k=2), reads=[brecv], writes=[KT1.B])
                P.dma("sp", V1.t[:, 2 + r * 8:10 + r * 8, :],
                      kvrecv[r * 128:(r + 1) * 128, 2048:4096].rearrange("p (b d) -> p b d", b=8), reads=[brecv], writes=[V1.B])
            for t in (0, 1):
                load_rope(t)
                P.dma("sp", xA.t[:], xpark[t], reads=[bpark[t]], writes=[xA.B])
                norm_mod(xA, hb, Avec1(1, 1), mvec(1, 1, 0))
                emit_mixer(1, 1, xA, hb, ob, KT1, V1, sgroups, rope=True)
                emit_ffn(1, 1, xA, hb)
                store_x(xA, ys_d[t])
            assert modst["i"] == 48 and modst["rounds"] == 3 and not deferred

        P.dry = True
        schedule()
        n_plan = wstate["taken"]
        wstate.update(issued=0, taken=0, released=0)
        modst.update(i=0, rounds=0)
        P.dry = False
        schedule()
        assert wstate["taken"] == n_plan == len(wstate["plan"]), (wstate["taken"], n_plan, len(wstate["plan"]))
        P.finish()
        build_program.stats = (P.n_ops, P.n_wait, nc.sbuf_bytes_remaining)
    return nc


def _rope_tables():
    L_ = 2048
    rows = (np.arange(L_) // 64).astype(np.float32)
    cols = (np.arange(L_) % 64).astype(np.float32)
    inv = (10000.0 ** (-np.arange(0, 64, 2, dtype=np.float32) / 64.0)).astype(np.float32)
    ar = rows[:, None] * inv[None, :]
    ac = cols[:, None] * inv[None, :]
    ang = np.concatenate([ar, ar, ac, ac], axis=1)
    return np.cos(ang).astype(np.float32), np.sin(ang).astype(np.float32)


def _rt_matrix():
    rt = np.zeros((128, 128), np.float32)
    for base in (0, 64):
        for i in range(32):
            m = base + i
            rt[m + 32, m] = -1.0
            rt[m, m + 32] = 1.0
    return rt


def _fm(v):
    v = np.asarray(v, np.float32)
    lead = v.shape[:-1]
    return np.ascontiguousarray(np.moveaxis(v.reshape(lead + (KC, 128)), -1, 0))


_NC_CACHE = {}


def kernel(x_prompt, x_sample, cache_k, cache_v, c, c_ctx, w_ada, b_ada, norm1_g, norm2_g,
           w_in, q_norm_g, k_norm_g, sgu_norm_g, w_spatial, b_spatial, out_norm_g, w_out,
           ffn_w_gate, ffn_w_up, ffn_w_down, w_router, b_router, moe_w_gate, moe_w_up, moe_w_down):
    f32 = lambda a: np.ascontiguousarray(np.asarray(a, dtype=np.float32))
    x_prompt, x_sample, cache_k, cache_v = f32(x_prompt), f32(x_sample), f32(cache_k), f32(cache_v)
    c, c_ctx = f32(c), f32(c_ctx)
    if "nc" not in _NC_CACHE:
        _NC_CACHE["nc"] = build_program()
    nc = _NC_CACHE["nc"]
    in_maps = _prep(x_prompt, x_sample, cache_k, cache_v, c, c_ctx, w_ada, b_ada, norm1_g, norm2_g,
                    w_in, q_norm_g, k_norm_g, sgu_norm_g, w_spatial, b_spatial, out_norm_g, w_out,
                    ffn_w_gate, ffn_w_up, ffn_w_down, w_router, b_router, moe_w_gate, moe_w_up, moe_w_down)
    res = run_bass_kernel_spmd(nc, in_maps, core_ids=list(range(NCORES)))
    return _assemble(res.results)


def _prep(x_prompt, x_sample, cache_k, cache_v, c, c_ctx, w_ada, b_ada, norm1_g, norm2_g,
          w_in, q_norm_g, k_norm_g, sgu_norm_g, w_spatial, b_spatial, out_norm_g, w_out,
          ffn_w_gate, ffn_w_up, ffn_w_down, w_router, b_router, moe_w_gate, moe_w_up, moe_w_down):
    f32 = lambda a: np.ascontiguousarray(np.asarray(a, dtype=np.float32))

    cos, sin = _rope_tables()
    w_ada_f = f32(w_ada)
    shared = {
        "n1g": _fm(norm1_g), "n2g": _fm(norm2_g),
        "bada": np.ascontiguousarray(np.moveaxis(f32(b_ada).reshape(NL, 96, 128), -1, 0)),
        "qg": np.ascontiguousarray(f32(q_norm_g).T), "kg": np.ascontiguousarray(f32(k_norm_g).T),
        "sgn": np.ascontiguousarray(np.moveaxis(f32(sgu_norm_g), -1, 0)),
        "bsrep": np.ascontiguousarray(np.broadcast_to(f32(b_spatial)[None], (128, NL, 8, 128))),
        "wsT": np.ascontiguousarray(np.transpose(f32(w_spatial), (3, 0, 1, 2))),
        "ong": _fm(out_norm_g),
        "wr": np.ascontiguousarray(np.transpose(f32(w_router)[0].reshape(KC, 128, NE), (1, 0, 2))),
        "brrep": np.ascontiguousarray(np.broadcast_to(f32(b_router)[0][None], (128, NE))),
        "ident": np.eye(128, dtype=np.float32), "rt": _rt_matrix(),
        "w_in": f32(w_in), "w_out": f32(w_out),
        "ffn_w_gate": f32(ffn_w_gate), "ffn_w_up": f32(ffn_w_up), "ffn_w_down": f32(ffn_w_down),
        "moe_w_gate": f32(moe_w_gate), "moe_w_up": f32(moe_w_up), "moe_w_down": f32(moe_w_down),
    }
    in_maps = []
    for core in range(NCORES):
        b, half = core // 2, core % 2
        own = x_sample[b, half * 1024:(half + 1) * 1024].reshape(2, TT, D)
        oth = x_sample[b, (1 - half) * 1024:(2 - half) * 1024].reshape(2, TT, D)
        pos = np.concatenate([np.arange(half * 1024, (half + 1) * 1024), np.arange((1 - half) * 1024, (2 - half) * 1024)])
        m = dict(shared)
        m["xs"] = np.ascontiguousarray(np.concatenate([own, oth], axis=0))
        m["xp"] = np.ascontiguousarray(x_prompt[2 * core:2 * core + 2].reshape(TT, D))
        m["ropec"] = np.ascontiguousarray(cos[pos].reshape(4, TT, 128).transpose(0, 2, 1))
        m["ropes"] = np.ascontiguousarray(sin[pos].reshape(4, TT, 128).transpose(0, 2, 1))
        m["ck"] = np.ascontiguousarray(cache_k[b].reshape(NL, 256, 256))
        m["cv"] = np.ascontiguousarray(cache_v[b].reshape(NL, 256, 256))
        cv2 = np.stack([c_ctx, c[b]], axis=-1)
        m["cvec"] = np.ascontiguousarray(cv2.reshape(KC, 128, 2).transpose(1, 0, 2))
        m["w_ada"] = np.ascontiguousarray(w_ada_f.reshape(NL, D, 24, 2, 256)[:, :, :, half, :].reshape(NL, D, 3 * D))
        in_maps.append(m)
    return in_maps


def _assemble(R):
    y_prompt = np.empty((16, 256, D), np.float32)
    y_sample = np.empty((4, 2048, D), np.float32)
    nk = np.empty((16, NL, 256, 2, 128), np.float32)
    nv = np.empty((16, NL, 256, 2, 128), np.float32)
    for core in range(NCORES):
        b, half = core // 2, core % 2
        r = R[core]
        y_prompt[2 * core:2 * core + 2] = np.asarray(r["yp"]).reshape(2, 256, D)
        y_sample[b, half * 1024:(half + 1) * 1024] = np.asarray(r["ys"]).reshape(1024, D)
        nk[2 * core:2 * core + 2] = np.asarray(r["nk"]).reshape(2, NL, 256, 2, 128)
        nv[2 * core:2 * core + 2] = np.asarray(r["nv"]).reshape(2, NL, 256, 2, 128)
    return (y_prompt, y_sample, nk, nv)
```

```python
import numpy as np
from contextlib import ExitStack
import concourse.bass as bass
import concourse.mybir as mybir
from concourse.bass_utils import run_bass_kernel_spmd

F32 = mybir.dt.float32
BF16 = mybir.dt.bfloat16
ALU = mybir.AluOpType
AF = mybir.ActivationFunctionType

ENGS = ("pe", "act", "dve", "pool", "sp")

D = 2048
KC = 16
TT = 512
NL = 2
INW = 3584
DFF = 5632
NE = 8
DFE = 2816
EPS = 1e-6
NCORES = 8


class Buf:
    __slots__ = ("name", "w", "r", "excl")

    def __init__(self, name="", excl=False):
        self.name = name
        self.excl = excl
        self.w = None
        self.r = []


class Prog:
    NDMA = 6

    def __init__(self, nc, stack):
        self.nc = nc
        self.stack = stack
        self.ops = {e: [] for e in ENGS}
        self.sem = {}
        self.cnt = {}
        self.seen = {e: {} for e in ENGS}
        for e in ENGS:
            self._mk("c_" + e)
        self.dma_rr = {}
        for q in ("sp", "pool"):
            for i in range(self.NDMA):
                self._mk("d_%s_%d" % (q, i))
            self.dma_rr[q] = 0
        self._mk("d_cc")
        self.n_wait = 0
        self.n_ops = 0
        self.dry = False

    def _mk(self, key):
        self.sem[key] = self.stack.enter_context(self.nc.semaphore(key))
        self.cnt[key] = 0

    def _deps(self, reads, writes):
        d = {}

        def add(ev):
            if ev is None:
                return
            k, v = ev
            if d.get(k, 0) < v:
                d[k] = v
        for b in reads:
            add(b.w)
        for b in writes:
            add(b.w)
            for ev in b.r:
                add(ev)
        return d

    def _emit_waits(self, eng, deps):
        seen = self.seen[eng]
        for k, v in deps.items():
            if eng == "pe" and k == "c_pe":
                continue
            if seen.get(k, 0) >= v:
                continue
            seen[k] = v
            sem = self.sem[k]
            self.ops[eng].append(lambda E, sem=sem, v=v: E.wait_ge(sem, v))
            self.n_wait += 1

    def _record(self, ev, reads, writes):
        for b in reads:
            b.r.append(ev)
            if len(b.r) > 48:
                m = {}
                for k, v in b.r:
                    if m.get(k, 0) < v:
                        m[k] = v
                b.r = list(m.items())
        for b in writes:
            b.w = ev
            b.r = []

    def op(self, eng, fn, reads=(), writes=()):
        if self.dry:
            return
        if any(b.excl for b in reads):
            writes = list(writes) + [b for b in reads if b.excl]
            reads = [b for b in reads if not b.excl]
        deps = self._deps(reads, writes)
        self._emit_waits(eng, deps)
        key = "c_" + eng
        self.cnt[key] += 1
        v = self.cnt[key]
        sem = self.sem[key]
        self.ops[eng].append(lambda E, fn=fn, sem=sem: fn(E).then_inc(sem, 1))
        self._record((key, v), reads, writes)
        self.n_ops += 1

    def dma(self, q, out, in_, reads=(), writes=()):
        if self.dry:
            return
        deps = self._deps(reads, writes)
        i = self.dma_rr[q]
        self.dma_rr[q] = (i + 1) % self.NDMA
        key = "d_%s_%d" % (q, i)
        if self.cnt[key] > 0 and deps.get(key, 0) < self.cnt[key]:
            deps[key] = self.cnt[key]
        self._emit_waits(q, deps)
        self.cnt[key] += 16
        v = self.cnt[key]
        sem = self.sem[key]
        self.ops[q].append(lambda E, out=out, in_=in_, sem=sem: E.dma_start(out=out, in_=in_).then_inc(sem, 16))
        self._record((key, v), reads, writes)
        self.n_ops += 1

    def coll(self, in_ap, out_ap, groups, reads=(), writes=()):
        if self.dry:
            return
        deps = self._deps(reads, writes)
        self._emit_waits("pool", deps)
        key = "d_cc"
        self.cnt[key] += 1
        v = self.cnt[key]
        sem = self.sem[key]
        self.ops["pool"].append(lambda E: E.collective_compute(
            "AllGather", ALU.bypass, replica_groups=groups, ins=[in_ap], outs=[out_ap]).then_inc(sem, 1))
        self._record((key, v), reads, writes)
        self.n_ops += 1

    def finish(self):
        deps = {k: v for k, v in self.cnt.items() if k.startswith("d_") and v > 0}
        self._emit_waits("sp", deps)
        ops = self.ops
        with self.nc.Block() as block:
            @block.tensor
            def _(E):
                for f in ops["pe"]:
                    f(E)

            @block.scalar
            def _(E):
                for f in ops["act"]:
                    f(E)

            @block.vector
            def _(E):
                for f in ops["dve"]:
                    f(E)

            @block.gpsimd
            def _(E):
                for f in ops["pool"]:
                    f(E)

            @block.sync
            def _(E):
                for f in ops["sp"]:
                    f(E)


class T:
    def __init__(self, t, n=1, excl=False, name=""):
        self.t = t
        self.b = [Buf(name + str(i), excl) for i in range(n)]
        self.B = self.b[0]


def build_program(debug=0):
    nc = bass.Bass("TRN2", target_bir_lowering=False, num_devices=NCORES)
    dt_in = lambda name, shape: nc.dram_tensor(name, list(shape), F32, kind="ExternalInput").ap()
    dt_out = lambda name, shape: nc.dram_tensor(name, list(shape), F32, kind="ExternalOutput").ap()
    xs_d = dt_in("xs", (4, TT, D))
    xp_d = dt_in("xp", (TT, D))
    ropec_d = dt_in("ropec", (4, 128, TT))
    ropes_d = dt_in("ropes", (4, 128, TT))
    ck_d = dt_in("ck", (NL, 256, 256))
    cv_d = dt_in("cv", (NL, 256, 256))
    cvec_d = dt_in("cvec", (128, KC, 2))
    n1g_d = dt_in("n1g", (128, NL, KC))
    n2g_d = dt_in("n2g", (128, NL, KC))
    bada_d = dt_in("bada", (128, NL, 96))
    qg_d = dt_in("qg", (128, NL))
    kg_d = dt_in("kg", (128, NL))
    sgn_d = dt_in("sgn", (128, NL, 8))
    bsrep_d = dt_in("bsrep", (128, NL, 8, 128))
    wsT_d = dt_in("wsT", (128, NL, 8, 128))
    ong_d = dt_in("ong", (128, NL, KC))
    wr_d = dt_in("wr", (128, KC, NE))
    brrep_d = dt_in("brrep", (128, NE))
    ident_d = dt_in("ident", (128, 128))
    rt_d = dt_in("rt", (128, 128))
    wada_d = dt_in("w_ada", (NL, D, 3 * D))
    win_d = dt_in("w_in", (NL, D, INW))
    wout_d = dt_in("w_out", (NL, D, D))
    fg_d = dt_in("ffn_w_gate", (1, D, DFF))
    fu_d = dt_in("ffn_w_up", (1, D, DFF))
    fd_d = dt_in("ffn_w_down", (1, DFF, D))
    mg_d = dt_in("moe_w_gate", (1, NE, D, DFE))
    mu_d = dt_in("moe_w_up", (1, NE, D, DFE))
    md_d = dt_in("moe_w_down", (1, NE, DFE, D))
    yp_d = dt_out("yp", (TT, D))
    ys_d = dt_out("ys", (2, TT, D))
    nk_d = dt_out("nk", (2, NL, 256, 256))
    nv_d = dt_out("nv", (2, NL, 256, 256))
    bIN = Buf("dram_in")

    with ExitStack() as st:
        P = Prog(nc, st)

        def sb(name, shape, dt, n=1, stack=st):
            return T(stack.enter_context(nc.sbuf_tensor("s_" + name, list(shape), dt)), n, name=name)

        bank = {}
        for nm in ("A0", "A1", "B0", "B1", "C", "Dk", "G", "H"):
            bank[nm] = T(st.enter_context(nc.psum_tensor("ps" + nm, [128, 512], F32)), 1, excl=True, name="ps" + nm)

        ident = sb("ident", (128, 128), F32)
        identb = sb("identb", (128, 128), BF16)
        rt = sb("rt", (128, 128), F32)
        onesb = sb("onesb", (128, 128), BF16)
        epsc = sb("epsc", (128, 1), F32)
        cvec = sb("cvec", (128, KC, 2), F32)
        scb = sb("scb", (128, KC, 2), BF16)
        modraw = sb("modraw", (128, 48, 2), F32)
        modg = sb("modg", (128, 2, 96), F32)
        n1g = sb("n1g", (128, NL, KC), F32)
        n2g = sb("n2g", (128, NL, KC), F32)
        bada = sb("bada", (128, NL, 96), F32)
        qg = sb("qg", (128, NL), F32)
        kg = sb("kg", (128, NL), F32)
        sgn = sb("sgn", (128, NL, 8), F32)
        bsrep = sb("bsrep", (128, 1, 8, 128), F32)
        wsT = sb("wsT", (128, NL, 8, 128), BF16)
        ong = sb("ong", (128, NL, KC), F32)
        wr = sb("wr", (128, KC, NE), F32)
        brrep = sb("brrep", (128, NE), F32)
        mod = sb("mod", (128, NL, 2, 96), F32)
        A1 = sb("A1", (128, NL, 2, KC), F32)
        A2 = sb("A2", (128, NL, 2, KC), F32)

        for (t_, d_) in ((ident, ident_d), (rt, rt_d), (cvec, cvec_d), (n1g, n1g_d), (n2g, n2g_d), (bada, bada_d),
                         (qg, qg_d), (kg, kg_d), (sgn, sgn_d), (ong, ong_d),
                         (wr, wr_d), (brrep, brrep_d)):
            P.dma("sp", t_.t[:], d_, reads=[bIN], writes=[t_.B])
        P.op("dve", lambda E: E.memset(onesb.t[:], 1.0), writes=[onesb.B])
        P.op("dve", lambda E: E.memset(epsc.t[:], EPS), writes=[epsc.B])
        P.op("dve", lambda E: E.tensor_copy(identb.t[:], ident.t[:]), reads=[ident.B], writes=[identb.B])
        P.dma("pool", wsT.t[:], wsT_d, reads=[bIN], writes=[wsT.B])
        P.op("act", lambda E: E.activation(out=scb.t[:], in_=cvec.t[:], func=AF.Silu), reads=[cvec.B], writes=[scb.B])

        NSLOT = 4
        wslots = [sb("wslot%d" % i, (128, 4096), BF16) for i in range(NSLOT)]
        wstate = {"plan": [], "issued": 0, "taken": 0, "released": 0}

        def w_view(i, shape):
            s_ = wslots[i % NSLOT]
            if shape[0] == "in":
                return s_.t[:, 0:KC * shape[1]].rearrange("p (k n) -> p k n", k=KC), s_.B
            return s_.t[:, 0:shape[1] * D].rearrange("p (k n) -> p k n", k=shape[1]), s_.B

        def w_issue_upto(n):
            while wstate["issued"] < min(n, len(wstate["plan"])):
                i = wstate["issued"]
                src, shape = wstate["plan"][i]
                dst, db = w_view(i, shape)
                P.dma("pool", dst, src.rearrange("(k p) n -> p k n", p=128), reads=[bIN], writes=[db])
                wstate["issued"] += 1

        def w_get(src, shape):
            i = wstate["taken"]
            wstate["taken"] += 1
            if P.dry:
                wstate["plan"].append((src, shape))
                return w_view(i, shape)
            assert i < len(wstate["plan"]) and wstate["plan"][i][1] == shape, "weight plan mismatch"
            w_issue_upto(max(wstate["released"] + NSLOT, i + 1))
            assert wstate["issued"] <= wstate["released"] + NSLOT and i - wstate["released"] < NSLOT
            return w_view(i, shape)

        def w_release():
            if P.dry:
                return
            wstate["released"] = wstate["taken"]
            w_issue_upto(wstate["released"] + NSLOT)

        def mm(out_bank, out_ap, lhsT, rhs, start, stop, reads):
            P.op("pe", lambda E: E.matmul(out_ap, lhsT=lhsT, rhs=rhs, start=start, stop=stop), reads=reads, writes=[out_bank.B])

        rr = {"act_dve": 0}

        modst = {"i": 0, "rounds": 0}
        ROUNDS = [(0, 8), (8, 24), (24, 48)]
        PAIRS = [[0, 1], [2, 3], [4, 5], [6, 7]]

        def mod_exchange(ri):
            lo, hi = ROUNDS[ri]
            n = (hi - lo) * 4
            l = lo // 24
            q0, q1 = lo % 24, (hi - 1) % 24 + 1
            P.dma("sp", msend[ri], modraw.t[:, 0:(hi - lo) * 2, :].rearrange("p m k -> p (m k)"), reads=[modraw.B], writes=[bmsend[ri]])
            P.coll(msend[ri], mrecv[ri], PAIRS, reads=[bmsend[ri]], writes=[bmrecv[ri]])
            P.dma("sp", modg.t[:, :, 0:n], mrecv[ri].rearrange("(r p) f -> p r f", p=128), reads=[bmrecv[ri]], writes=[modg.B])
            for r in range(2):
                for cnd in range(2):
                    o_ = mod.t[:, l, cnd, :].rearrange("p (q r c) -> p q r c", q=24, r=2)[:, q0:q1, r, :]
                    b_ = bada.t[:, l, :].rearrange("p (q r c) -> p q r c", q=24, r=2)[:, q0:q1, r, :]
                    i_ = modg.t[:, r, 0:n].rearrange("p (q c k) -> p q c k", c=2, k=2)[:, :, :, cnd]
                    P.op("dve", lambda E, o_=o_, i_=i_, b_=b_: E.tensor_tensor(out=o_, in0=i_, in1=b_, op=ALU.add),
                         reads=[modg.B, bada.B], writes=[mod.B])
            for (A, g_, off, rr_) in ((A1, n1g, 16, (0, 2)), (A2, n2g, 64, (1, 2))):
                if ri in rr_:
                    for cnd in range(2):
                        P.op("dve", lambda E, A=A, g_=g_, off=off, l=l, cnd=cnd: E.scalar_tensor_tensor(
                            out=A.t[:, l, cnd, :], in0=mod.t[:, l, cnd, off:off + 16], scalar=1.0, in1=g_.t[:, l, :],
                            op0=ALU.add, op1=ALU.mult), reads=[mod.B, g_.B], writes=[A.B])
            modst["rounds"] = ri + 1

        def mod_step(n):
            for _ in range(n):
                i = modst["i"]
                if i >= 48:
                    return
                modst["i"] = i + 1
                l, q = divmod(i, 24)
                ri = [k for k, (lo, hi) in enumerate(ROUNDS) if lo <= i < hi][0]
                mi = i - ROUNDS[ri][0]
                w, wb = w_get(wada_d[l, :, q * 256:(q + 1) * 256], ("in", 256))
                bk = bank["G"] if i % 2 == 0 else bank["H"]
                for c2 in range(2):
                    for kc in range(KC):
                        mm(bk, bk.t[:, c2 * 2:c2 * 2 + 2], w[:, kc, c2 * 128:(c2 + 1) * 128], scb.t[:, kc, :],
                           kc == 0, kc == KC - 1, [wb, scb.B])
                w_release()
                P.op("dve", lambda E, mi=mi, bk=bk: E.tensor_copy(modraw.t[:, mi * 2:mi * 2 + 2, :],
                                                                 bk.t[:, 0:4].rearrange("p (c k) -> p c k", c=2)),
                     reads=[bk.B], writes=[modraw.B])
                if i + 1 == ROUNDS[ri][1]:
                    mod_exchange(ri)

        def need_mod(l, which):
            req = 3 if l == 1 else (1 if which < 2 else 2)
            assert modst["rounds"] >= req, ("modulation not exchanged yet", l, which, modst)

        bgc = {"hp": 0, "wo": 0, "ffn": 0}

        def mvec(l, cnd, which):
            need_mod(l, which)
            return mod.t[:, l, cnd, which * 16:(which + 1) * 16]

        xstage = sb("xstage", (128, 1024), F32, n=1)
        kvstage = sb("kvstage", (128, 4, 256), F32)
        xstg = [(xstage.t[:], xstage.B), (kvstage.t[:].rearrange("p b d -> p (b d)"), kvstage.B)]

        def load_x(src, x):
            for blk in range(4):
                for g4 in range(4):
                    st_ap, st_b = xstg[(blk * 2 + g4 // 2) % 2]
                    if g4 % 2 == 0:
                        P.dma("sp", st_ap, src[blk * 128:(blk + 1) * 128, (g4 // 2) * 1024:(g4 // 2 + 1) * 1024], reads=[bIN], writes=[st_b])
                    bk = bank["H"] if g4 % 2 == 0 else bank["G"]
                    for j in range(4):
                        kc = g4 * 4 + j
                        kl = kc % 8
                        P.op("pe", lambda E, bk=bk, j=j, kl=kl, st_ap=st_ap: E.transpose(bk.t[:, j * 128:(j + 1) * 128],
                                                                                        st_ap[:, kl * 128:(kl + 1) * 128], ident.t[:]),
                             reads=[st_b, ident.B], writes=[bk.B])
                    dst = x.t[:, g4 * 4:(g4 + 1) * 4, blk * 128:(blk + 1) * 128]
                    srcp = bk.t[:].rearrange("p (j t) -> p j t", j=4)
                    if g4 % 2 == 0:
                        P.op("dve", lambda E, dst=dst, srcp=srcp: E.tensor_copy(dst, srcp), reads=[bk.B], writes=[x.B])
                    else:
                        P.op("act", lambda E, dst=dst, srcp=srcp: E.activation(out=dst, in_=srcp, func=AF.Copy), reads=[bk.B], writes=[x.B])

        def store_x(x, dst):
            for blk in range(4):
                for g4 in range(4):
                    st_ap, st_b = xstg[(blk * 2 + g4 // 2) % 2]
                    bk = bank["H"] if g4 % 2 == 0 else bank["G"]
                    for j in range(4):
                        kc = g4 * 4 + j
                        P.op("pe", lambda E, bk=bk, j=j, kc=kc, blk=blk: E.transpose(
                            bk.t[:, j * 128:(j + 1) * 128], x.t[:, kc, blk * 128:(blk + 1) * 128], ident.t[:]),
                            reads=[x.B, ident.B], writes=[bk.B])
                    dsts = st_ap[:, (g4 % 2) * 512:(g4 % 2 + 1) * 512]
                    if g4 % 2 == 0:
                        P.op("dve", lambda E, dsts=dsts, bk=bk: E.tensor_copy(dsts, bk.t[:]), reads=[bk.B], writes=[st_b])
                    else:
                        P.op("act", lambda E, dsts=dsts, bk=bk: E.activation(out=dsts, in_=bk.t[:], func=AF.Copy), reads=[bk.B], writes=[st_b])
                        P.dma("sp", dst[blk * 128:(blk + 1) * 128, (g4 // 2) * 1024:(g4 // 2 + 1) * 1024], st_ap, reads=[st_b], writes=[Buf()])

        sqr = sb("sqr", (128, 3, TT), BF16, n=3)
        rstd = sb("rstd", (128, TT), F32)
        tmpf = sb("tmpf", (128, 2, TT), F32, n=2)

        def sum_sq_to_rstd(n_feat, dst):
            G = bank["G"]
            P.op("act", lambda E: E.activation(out=dst.t[:], in_=G.t[:], func=AF.Sqrt, bias=epsc.t[:, 0:1], scale=1.0 / n_feat),
                 reads=[G.B, epsc.B], writes=[dst.B])
            P.op("dve", lambda E: E.reciprocal(out=dst.t[:], in_=dst.t[:]), reads=[dst.B], writes=[dst.B])

        def Avec1(l, cnd):
            need_mod(l, 1)
            return A1.t[:, l, cnd, :]

        def Avec2(l, cnd):
            need_mod(l, 4)
            return A2.t[:, l, cnd, :]

        def norm_mod(x, h, Avec, Bvec):
            G = bank["G"]
            for kc in range(KC):
                i = kc % 2
                P.op("act", lambda E, kc=kc, i=i: E.activation(out=sqr.t[:, i, :], in_=x.t[:, kc, :], func=AF.Square),
                     reads=[x.B], writes=[sqr.b[i]])
                mm(G, G.t[:], onesb.t[:], sqr.t[:, i, :], kc == 0, kc == KC - 1, [onesb.B, sqr.b[i]])
            sum_sq_to_rstd(float(D), rstd)
            for kc in range(KC):
                i = kc % 2
                P.op("dve", lambda E, kc=kc, i=i: E.scalar_tensor_tensor(
                    out=tmpf.t[:, i, :], in0=x.t[:, kc, :], scalar=Avec[:, kc:kc + 1], in1=rstd.t[:], op0=ALU.mult, op1=ALU.mult),
                    reads=[x.B, rstd.B, A1.B, A2.B], writes=[tmpf.b[i]])
                P.op("act", lambda E, kc=kc, i=i: E.activation(out=h.t[:, kc, :], in_=tmpf.t[:, i, :], func=AF.Identity,
                                                              bias=Bvec[:, kc:kc + 1], scale=1.0),
                     reads=[tmpf.b[i], mod.B], writes=[h.B])

        hq = sb("hq", (128, TT), F32)
        hq1 = sb("hq1", (128, TT), F32)
        hq2 = [hq, hq1]
        hrs = sb("hrs", (128, TT), F32)
        r1 = sb("r1", (128, TT), F32)
        r2 = sb("r2", (128, TT), F32)
        qb = sb("qb", (128, TT), BF16)
        qb1 = sb("qb1", (128, TT), BF16)
        qb2 = [qb, qb1]
        ropec = sb("ropec", (128, TT), F32)
        ropes = sb("ropes", (128, TT), F32)

        def head_norm(ps, gain_ap, out_f32=None, out_bf=None):
            G = bank["G"]
            P.op("act", lambda E: E.activation(out=sqr.t[:, 0, :], in_=ps.t[:], func=AF.Square), reads=[ps.B], writes=[sqr.b[0]])
            mm(G, G.t[:], onesb.t[:], sqr.t[:, 0, :], True, True, [onesb.B, sqr.b[0]])
            sum_sq_to_rstd(128.0, hrs)
            if out_f32 is not None:
                P.op("dve", lambda E: E.scalar_tensor_tensor(out=out_f32.t[:], in0=ps.t[:], scalar=gain_ap, in1=hrs.t[:],
                                                             op0=ALU.mult, op1=ALU.mult), reads=[ps.B, hrs.B, qg.B, kg.B], writes=[out_f32.B])
            else:
                P.op("dve", lambda E: E.scalar_tensor_tensor(out=out_bf, in0=ps.t[:], scalar=gain_ap, in1=hrs.t[:],
                                                             op0=ALU.mult, op1=ALU.mult), reads=[ps.B, hrs.B, qg.B, kg.B], writes=[])

        def rope_to(src_f32, out_ap, out_buf):
            H = bank["H"]
            mm(H, H.t[:], rt.t[:], src_f32.t[:], True, True, [rt.B, src_f32.B])
            P.op("dve", lambda E: E.tensor_tensor(out=src_f32.t[:], in0=src_f32.t[:], in1=ropec.t[:], op=ALU.mult),
                 reads=[src_f32.B, ropec.B], writes=[src_f32.B])
            P.op("dve", lambda E: E.tensor_tensor(out=tmpf.t[:, 1, :], in0=H.t[:], in1=ropes.t[:], op=ALU.mult),
                 reads=[H.B, ropes.B], writes=[tmpf.b[1]])
            P.op("dve", lambda E: E.tensor_tensor(out=out_ap, in0=src_f32.t[:], in1=tmpf.t[:, 1, :], op=ALU.add),
                 reads=[src_f32.B, tmpf.b[1]], writes=[out_buf])


        def plan_kv(l):
            return [(win_d[l, :, 1024:1280], ("in", 256)), (win_d[l, :, 1280:1536], ("in", 256))]

        def emit_kv(l, h, KTb, Vb, kcol0, vblk0, rope, cache_out=None):
            wk, wkb = w_get(win_d[l, :, 1024:1280], ("in", 256))
            for kvh in range(2):
                bk = bank["A%d" % kvh]
                for kc in range(KC):
                    mm(bk, bk.t[:], wk[:, kc, kvh * 128:(kvh + 1) * 128], h.t[:, kc, :], kc == 0, kc == KC - 1, [wkb, h.B])
                dst = KTb.t[:, kvh, kcol0:kcol0 + TT]
                if rope:
                    head_norm(bk, kg.t[:, l:l + 1], out_f32=hq)
                    rope_to(hq, dst, KTb.B)
                else:
                    head_norm(bk, kg.t[:, l:l + 1], out_f32=hq)
                    P.op("act", lambda E, dst=dst: E.activation(out=dst, in_=hq.t[:], func=AF.Copy), reads=[hq.B], writes=[KTb.B])
                    if cache_out is not None:
                        H = bank["H"]
                        for blk in range(4):
                            P.op("pe", lambda E, blk=blk: E.transpose(H.t[:, blk * 128:(blk + 1) * 128], hq.t[:, blk * 128:(blk + 1) * 128], ident.t[:]),
                                 reads=[hq.B, ident.B], writes=[H.B])
                        P.op("dve", lambda E, kvh=kvh: E.tensor_copy(kvstage.t[:, :, kvh * 128:(kvh + 1) * 128],
                                                                     H.t[:].rearrange("p (b d) -> p b d", b=4)),
                             reads=[H.B], writes=[kvstage.B])
            if cache_out is not None:
                for blk in range(4):
                    P.dma("sp", nk_d[blk // 2, l, (blk % 2) * 128:(blk % 2 + 1) * 128, :], kvstage.t[:, blk, :],
                          reads=[kvstage.B], writes=[Buf()])
            w_release()
            wv, wvb = w_get(win_d[l, :, 1280:1536], ("in", 256))
            for blk in range(4):
                bk = bank["B%d" % (blk // 2)]
                o_ap = bk.t[:, (blk % 2) * 256:(blk % 2 + 1) * 256]
                for kc in range(KC):
                    mm(bk, o_ap, h.t[:, kc, blk * 128:(blk + 1) * 128], wv[:, kc, :], kc == 0, kc == KC - 1, [wvb, h.B])
                if blk % 2 == 1:
                    P.op("act", lambda E, bk=bk, blk=blk: E.activation(out=Vb.t[:, vblk0 + blk - 1:vblk0 + blk + 1, :],
                                                                      in_=bk.t[:].rearrange("p (b d) -> p b d", b=2), func=AF.Copy),
                         reads=[bk.B], writes=[Vb.B])
                    if cache_out is not None:
                        P.op("dve", lambda E, bk=bk, blk=blk: E.tensor_copy(kvstage.t[:, blk - 1:blk + 1, :],
                                                                            bk.t[:].rearrange("p (b d) -> p b d", b=2)),
                             reads=[bk.B], writes=[kvstage.B])
            w_release()
            if cache_out is not None:
                for blk in range(4):
                    P.dma("sp", nv_d[blk // 2, l, (blk % 2) * 128:(blk % 2 + 1) * 128, :], kvstage.t[:, blk, :],
                          reads=[kvstage.B], writes=[Buf()])

        PT = sb("PT", (128, 4, TT), BF16, n=4)
        uf = sb("uf", (128, TT), F32)
        ghat = sb("ghat", (128, 4, 256), BF16)
        gss = sb("gss", (128, 8), F32)
        gsq = sb("gsq", (128, 128), F32)
        ssa = sb("ssa", (128, TT), F32)
        sss = sb("sss", (128, TT), F32)
        SCALE = 1.0 / float(np.sqrt(128.0))

        def plan_mixer(l):
            items = []
            for hp in range(4):
                items.append((win_d[l, :, 2560 + hp * 256:2560 + (hp + 1) * 256], ("in", 256)))
                items.append((win_d[l, :, 1536 + hp * 256:1536 + (hp + 1) * 256], ("in", 256)))
                items.append((win_d[l, :, hp * 256:(hp + 1) * 256], ("in", 256)))
            for pc in range(8):
                items.append((wout_d[l, :, pc * 256:(pc + 1) * 256], ("in", 256)))
            return items

        deferred = []

        def flush():
            for f in deferred:
                f()
            del deferred[:]

        def accum_sumsq(src_f32_ap, src_buf, acc, first, si):
            G = bank["G"]
            if any(getattr(f, "si", None) == si for f in deferred):
                flush()
            P.op("act", lambda E: E.activation(out=sqr.t[:, si, :], in_=src_f32_ap, func=AF.Square), reads=[src_buf], writes=[sqr.b[si]])

            def part2():
                mm(G, G.t[:], onesb.t[:], sqr.t[:, si, :], True, True, [onesb.B, sqr.b[si]])
                if first:
                    P.op("dve", lambda E: E.tensor_copy(acc.t[:], G.t[:]), reads=[G.B], writes=[acc.B])
                else:
                    P.op("dve", lambda E: E.tensor_tensor(out=acc.t[:], in0=acc.t[:], in1=G.t[:], op=ALU.add), reads=[G.B, acc.B], writes=[acc.B])
            part2.si = si
            deferred.append(part2)

        def emit_mixer(l, cnd, x, h, o, KTb, Vb, groups, rope):
            P.dma("sp", bsrep.t[:, 0], bsrep_d[:, l], reads=[bIN], writes=[bsrep.B])
            for hp in range(4):
                c4 = hp
                wg_, wgb = w_get(win_d[l, :, 2560 + hp * 256:2560 + (hp + 1) * 256], ("in", 256))
                for blk in range(4):
                    bk = bank["A%d" % (blk % 2)]
                    o_ap = bk.t[:, 0:256]
                    for kc in range(KC):
                        mm(bk, o_ap, h.t[:, kc, blk * 128:(blk + 1) * 128], wg_[:, kc, :], kc == 0, kc == KC - 1, [wgb, h.B])
                    P.op("dve", lambda E, c4=c4: E.memset(gss.t[:, c4 * 2:c4 * 2 + 2], 0.0), reads=[gss.B], writes=[gss.B])
                    for hh in range(2):
                        hd = c4 * 2 + hh
                        P.op("act", lambda E, bk=bk, hh=hh, hd=hd: E.activation(out=gsq.t[:], in_=bk.t[:, hh * 128:(hh + 1) * 128], func=AF.Square,
                                                                               accum_out=gss.t[:, hd:hd + 1]),
                             reads=[bk.B], writes=[gsq.B, gss.B])
                    P.op("act", lambda E, c4=c4: E.activation(out=gss.t[:, c4 * 2:c4 * 2 + 2], in_=gss.t[:, c4 * 2:c4 * 2 + 2], func=AF.Sqrt,
                                                              bias=epsc.t[:, 0:1], scale=1.0 / 128.0), reads=[gss.B, epsc.B], writes=[gss.B])
                    P.op("dve", lambda E, c4=c4: E.reciprocal(out=gss.t[:, c4 * 2:c4 * 2 + 2], in_=gss.t[:, c4 * 2:c4 * 2 + 2]),
                         reads=[gss.B], writes=[gss.B])
                    for hh in range(2):
                        hd = c4 * 2 + hh
                        P.op("dve", lambda E, bk=bk, hh=hh, hd=hd, blk=blk: E.tensor_scalar(
                            out=ghat.t[:, blk, hh * 128:(hh + 1) * 128], in0=bk.t[:, hh * 128:(hh + 1) * 128],
                            scalar1=gss.t[:, hd:hd + 1], scalar2=None, op0=ALU.mult), reads=[bk.B, gss.B], writes=[ghat.B])
                w_release()
                flush()
                wu_, wub = w_get(win_d[l, :, 1536 + hp * 256:1536 + (hp + 1) * 256], ("in", 256))
                wq_, wqb = w_get(win_d[l, :, hp * 256:(hp + 1) * 256], ("in", 256))
                C, Dk = bank["C"], bank["Dk"]

                def sgu_head(hh):
                    hd = hp * 2 + hh
                    bkA = bank["A0"]
                    for kc in range(KC):
                        mm(bkA, bkA.t[:], wu_[:, kc, hh * 128:(hh + 1) * 128], h.t[:, kc, :], kc == 0, kc == KC - 1, [wub, h.B])
                    P.op("act", lambda E: E.activation(out=uf.t[:], in_=bkA.t[:], func=AF.Copy), reads=[bkA.B], writes=[uf.B])
                    bkB = bank["B%d" % hh]
                    for blk in range(4):
                        mm(bkB, bkB.t[:, blk * 128:(blk + 1) * 128], ghat.t[:, blk, hh * 128:(hh + 1) * 128], wsT.t[:, l, hd, :],
                           True, True, [ghat.B, wsT.B])
                    for blk in range(4):
                        P.op("dve", lambda E, blk=blk: E.scalar_tensor_tensor(
                            out=r1.t[:, blk * 128:(blk + 1) * 128], in0=bkB.t[:, blk * 128:(blk + 1) * 128], scalar=sgn.t[:, l, hd:hd + 1],
                            in1=bsrep.t[:, 0, hd, :], op0=ALU.mult, op1=ALU.add), reads=[bkB.B, sgn.B, bsrep.B], writes=[r1.B])
                    P.op("dve", lambda E: E.tensor_tensor(out=r2.t[:], in0=r1.t[:], in1=uf.t[:], op=ALU.mult), reads=[r1.B, uf.B], writes=[r2.B])
                    P.op("act", lambda E: E.activation(out=o.t[:, 8 + hd, :], in_=r2.t[:], func=AF.Copy, scale=ong.t[:, l, 8 + hd:9 + hd]),
                         reads=[r2.B, ong.B], writes=[o.B])
                    accum_sumsq(r2.t[:], r2.B, sss, hd == 0, 1)

                def q_proj_norm(hh):
                    bkQ = bank["A1"]
                    for kc in range(KC):
                        mm(bkQ, bkQ.t[:], wq_[:, kc, hh * 128:(hh + 1) * 128], h.t[:, kc, :], kc == 0, kc == KC - 1, [wqb, h.B])
                    head_norm(bkQ, qg.t[:, l:l + 1], out_f32=hq2[hh])

                def q_finish(hh):
                    if rope:
                        rope_to(hq2[hh], qb2[hh].t[:], qb2[hh].B)
                    else:
                        P.op("act", lambda E: E.activation(out=qb2[hh].t[:], in_=hq2[hh].t[:], func=AF.Copy), reads=[hq2[hh].B], writes=[qb2[hh].B])

                def attention(hh, hook=None):
                    hd = hp * 2 + hh
                    kvh = hd // 4
                    qbh = qb2[hh]
                    for gi, (q0, q1, kblocks) in enumerate(groups):
                        nkb = len(kblocks)

                        def score(ji, q0=q0, q1=q1, kblocks=kblocks):
                            j = kblocks[ji]
                            bS = bank["B%d" % (ji % 2)]
                            mm(bS, bS.t[:, q0:q1], KTb.t[:, kvh, j * 128:(j + 1) * 128], qbh.t[:, q0:q1], True, True, [KTb.B, qbh.B])
                            P.op("act", lambda E: E.activation(out=PT.t[:, ji % 4, q0:q1], in_=bS.t[:, q0:q1], func=AF.Exp, scale=SCALE),
                                 reads=[bS.B], writes=[PT.b[ji % 4]])
                        score(0)
                        for ji in range(nkb):
                            if ji + 1 < nkb:
                                score(ji + 1)
                            j = kblocks[ji]
                            mm(C, C.t[:, q0:q1], Vb.t[:, j, kvh * 128:(kvh + 1) * 128], PT.t[:, ji % 4, q0:q1], ji == 0, ji == nkb - 1,
                               [Vb.B, PT.b[ji % 4]])
                            mm(Dk, Dk.t[:, q0:q1], onesb.t[:], PT.t[:, ji % 4, q0:q1], ji == 0, ji == nkb - 1, [onesb.B, PT.b[ji % 4]])
                            if hook is not None and gi == 0 and ji == min(3, nkb - 1):
                                hook()
                    P.op("dve", lambda E: E.reciprocal(out=r1.t[:], in_=Dk.t[:]), reads=[Dk.B], writes=[r1.B])
                    P.op("dve", lambda E: E.tensor_tensor(out=r2.t[:], in0=C.t[:], in1=r1.t[:], op=ALU.mult), reads=[C.B, r1.B], writes=[r2.B])
                    P.op("act", lambda E: E.activation(out=o.t[:, hd, :], in_=r2.t[:], func=AF.Copy, scale=ong.t[:, l, hd:hd + 1]),
                         reads=[r2.B, ong.B], writes=[o.B])
                    accum_sumsq(r2.t[:], r2.B, ssa, hd == 0, 2)

                sgu_head(0)
                q_proj_norm(0)
                sgu_head(1)
                flush()
                q_finish(0)
                q_proj_norm(1)
                flush()
                attention(0, hook=lambda: q_finish(1))
                attention(1)
                w_release()
                mod_step(bgc["hp"])
            flush()
            rsa, rss = ssa, sss
            for (acc, dstr) in ((ssa, ssa), (sss, sss)):
                P.op("act", lambda E, acc=acc, dstr=dstr: E.activation(out=dstr.t[:], in_=acc.t[:], func=AF.Sqrt, bias=epsc.t[:, 0:1], scale=1.0 / 1024.0),
                     reads=[acc.B, epsc.B], writes=[dstr.B])
                P.op("dve", lambda E, dstr=dstr: E.reciprocal(out=dstr.t[:], in_=dstr.t[:]), reads=[dstr.B], writes=[dstr.B])
            g1 = mvec(l, cnd, 2)
            for pc in range(8):
                wo_, wob = w_get(wout_d[l, :, pc * 256:(pc + 1) * 256], ("in", 256))
                for c2 in range(2):
                    oc = pc * 2 + c2
                    bkA, bkB = bank["A%d" % c2], bank["B%d" % c2]
                    for kc in range(8):
                        mm(bkA, bkA.t[:], wo_[:, kc, c2 * 128:(c2 + 1) * 128], o.t[:, kc, :], kc == 0, kc == 7, [wob, o.B])
                    for kc in range(8, 16):
                        mm(bkB, bkB.t[:], wo_[:, kc, c2 * 128:(c2 + 1) * 128], o.t[:, kc, :], kc == 8, kc == 15, [wob, o.B])
                    if c2 == 1:
                        w_release()
                        mod_step(bgc["wo"])
                    P.op("dve", lambda E, bkA=bkA: E.tensor_tensor(out=r1.t[:], in0=bkA.t[:], in1=rsa.t[:], op=ALU.mult), reads=[bkA.B, rsa.B], writes=[r1.B])
                    P.op("dve", lambda E, bkB=bkB: E.tensor_tensor(out=r2.t[:], in0=bkB.t[:], in1=rss.t[:], op=ALU.mult), reads=[bkB.B, rss.B], writes=[r2.B])
                    P.op("dve", lambda E: E.tensor_tensor(out=r1.t[:], in0=r1.t[:], in1=r2.t[:], op=ALU.add), reads=[r1.B, r2.B], writes=[r1.B])
                    P.op("dve", lambda E, oc=oc: E.scalar_tensor_tensor(out=x.t[:, oc, :], in0=r1.t[:], scalar=g1[:, oc:oc + 1], in1=x.t[:, oc, :],
                                                                       op0=ALU.mult, op1=ALU.add), reads=[r1.B, mod.B, x.B], writes=[x.B])

        sg = sb("sg", (128, 2, TT), F32, n=2)
        actb = sb("actb", (128, 2, 4, TT), BF16, n=2)
        cwrep = sb("cwrep", (128, TT), F32)
        cw4 = sb("cw4", (128, 4, NE), F32)
        lg = sb("lg", (128, NE), F32)
        lg8 = sb("lg8", (128, 8), F32)
        cw = sb("cw", (128, NE), F32)
        cwb = sb("cwb", (128, 128), F32)
        tcol = sb("tcol", (128, 4), F32)
        wrp = sb("wrp", (128, KC, NE), F32)

        def ffn_panel_list(l):
            out = []
            if l == 0:
                for p in range(DFF // 256):
                    out.append((fg_d[0][:, p * 256:(p + 1) * 256], fu_d[0][:, p * 256:(p + 1) * 256], fd_d[0][p * 256:(p + 1) * 256, :], None))
            else:
                for e in range(NE):
                    for p in range(DFE // 256):
                        out.append((mg_d[0, e][:, p * 256:(p + 1) * 256], mu_d[0, e][:, p * 256:(p + 1) * 256],
                                    md_d[0, e][p * 256:(p + 1) * 256, :], e))
            assert len(out) % 2 == 0
            return out

        def plan_ffn(l):
            pl = ffn_panel_list(l)
            items = []
            for pp in range(len(pl) // 2):
                for half in range(2):
                    g_, u_, d_, e = pl[pp * 2 + half]
                    items.append((g_, ("in", 256)))
                    items.append((u_, ("in", 256)))
                for half in range(2):
                    items.append((pl[pp * 2 + half][2], ("rows", 2)))
            return items

        def emit_ffn_panels(l, x, h2, g2):
            pl = ffn_panel_list(l)
            cur_e = None
            for pp in range(len(pl) // 2):
                pi = pp % 2
                for half in range(2):
                    e = pl[pp * 2 + half][3]
                    if e is not None and e != cur_e:
                        emit_cwrep(e)
                        cur_e = e
                    wg_, wgb = w_get(pl[pp * 2 + half][0], ("in", 256))
                    for c2 in range(2):
                        bkG = bank["A%d" % c2]
                        for kc in range(KC):
                            mm(bkG, bkG.t[:], wg_[:, kc, c2 * 128:(c2 + 1) * 128], h2.t[:, kc, :], kc == 0, kc == KC - 1, [wgb, h2.B])
                    w_release()
                    wu_, wub = w_get(pl[pp * 2 + half][1], ("in", 256))
                    for c2 in range(2):
                        bkU = bank["B%d" % c2]
                        for kc in range(KC):
                            mm(bkU, bkU.t[:], wu_[:, kc, c2 * 128:(c2 + 1) * 128], h2.t[:, kc, :], kc == 0, kc == KC - 1, [wub, h2.B])
                    w_release()
                    for c2 in range(2):
                        bkG, bkU = bank["A%d" % c2], bank["B%d" % c2]
                        P.op("act", lambda E, bkG=bkG, c2=c2: E.activation(out=sg.t[:, c2, :], in_=bkG.t[:], func=AF.Silu), reads=[bkG.B], writes=[sg.b[c2]])
                        if e is not None:
                            P.op("dve", lambda E, c2=c2: E.tensor_tensor(out=sg.t[:, c2, :], in0=sg.t[:, c2, :], in1=cwrep.t[:], op=ALU.mult),
                                 reads=[sg.b[c2], cwrep.B], writes=[sg.b[c2]])
                        P.op("dve", lambda E, bkU=bkU, c2=c2, pi=pi, half=half: E.tensor_tensor(out=actb.t[:, pi, half * 2 + c2, :], in0=bkU.t[:], in1=sg.t[:, c2, :], op=ALU.mult),
                             reads=[bkU.B, sg.b[c2]], writes=[actb.b[pi]])
                wd0, wdb0 = w_get(pl[pp * 2][2], ("rows", 2))
                wd1, wdb1 = w_get(pl[pp * 2 + 1][2], ("rows", 2))
                for oc in range(KC):
                    bk = bank["C"] if oc % 2 == 0 else bank["Dk"]
                    for c4 in range(4):
                        wd_, wdb = (wd0, wdb0) if c4 < 2 else (wd1, wdb1)
                        mm(bk, bk.t[:], wd_[:, c4 % 2, oc * 128:(oc + 1) * 128], actb.t[:, pi, c4, :], c4 == 0, c4 == 3, [wdb, actb.b[pi]])
                    P.op("dve", lambda E, bk=bk, oc=oc: E.scalar_tensor_tensor(out=x.t[:, oc, :], in0=bk.t[:], scalar=g2[:, oc:oc + 1], in1=x.t[:, oc, :],
                                                                              op0=ALU.mult, op1=ALU.add), reads=[bk.B, mod.B, x.B], writes=[x.B])
                w_release()
                mod_step(bgc["ffn"])

        def emit_router(l, cnd, x):
            A2v = Avec2(l, cnd)
            sh2 = mvec(l, cnd, 3)
            for kc in range(KC):
                P.op("dve", lambda E, kc=kc: E.tensor_scalar(out=wrp.t[:, kc, :], in0=wr.t[:, kc, :], scalar1=A2v[:, kc:kc + 1], scalar2=None, op0=ALU.mult),
                     reads=[wr.B, A2.B], writes=[wrp.B])
            H, G = bank["H"], bank["G"]
            for kc in range(KC):
                P.op("dve", lambda E, kc=kc: E.tensor_scalar(out=gsq.t[:], in0=ident.t[:], scalar1=0.0, scalar2=sh2[:, kc:kc + 1], op0=ALU.mult, op1=ALU.add),
                     reads=[ident.B, mod.B, gsq.B], writes=[gsq.B])
                mm(G, G.t[:, 0:NE], gsq.t[:], wr.t[:, kc, :], kc == 0, kc == KC - 1, [gsq.B, wr.B])
            P.op("dve", lambda E: E.tensor_tensor(out=lg8.t[:], in0=G.t[:, 0:NE], in1=brrep.t[:], op=ALU.add), reads=[G.B, brrep.B], writes=[lg8.B])
            for blk in range(4):
                for kc in range(KC):
                    mm(H, H.t[:, 0:NE], x.t[:, kc, blk * 128:(blk + 1) * 128], wrp.t[:, kc, :], kc == 0, kc == KC - 1, [x.B, wrp.B])
                mm(G, G.t[:, 0:1], rstd.t[:, blk * 128:(blk + 1) * 128], ident.t[:, 0:1], True, True, [rstd.B, ident.B])
                P.op("dve", lambda E: E.tensor_copy(tcol.t[:, 0:1], G.t[:, 0:1]), reads=[G.B], writes=[tcol.B])
                P.op("dve", lambda E: E.scalar_tensor_tensor(out=lg.t[:], in0=H.t[:, 0:NE], scalar=tcol.t[:, 0:1], in1=lg8.t[:], op0=ALU.mult, op1=ALU.add),
                     reads=[H.B, tcol.B, lg8.B], writes=[lg.B])
                P.op("dve", lambda E: E.tensor_reduce(out=tcol.t[:, 1:2], in_=lg.t[:], axis=mybir.AxisListType.X, op=ALU.max), reads=[lg.B, tcol.B], writes=[tcol.B])
                P.op("dve", lambda E: E.tensor_scalar(out=cw.t[:], in0=lg.t[:], scalar1=tcol.t[:, 1:2], scalar2=-1e30, op0=ALU.is_ge, op1=ALU.mult),
                     reads=[lg.B, tcol.B], writes=[cw.B])
                P.op("dve", lambda E: E.tensor_tensor(out=cw.t[:], in0=cw.t[:], in1=lg.t[:], op=ALU.add), reads=[cw.B, lg.B], writes=[cw.B])
                P.op("dve", lambda E: E.tensor_reduce(out=tcol.t[:, 2:3], in_=cw.t[:], axis=mybir.AxisListType.X, op=ALU.max), reads=[cw.B, tcol.B], writes=[tcol.B])
                P.op("dve", lambda E: E.tensor_scalar(out=cw.t[:], in0=lg.t[:], scalar1=tcol.t[:, 2:3], scalar2=None, op0=ALU.is_ge), reads=[lg.B, tcol.B], writes=[cw.B])
                P.op("dve", lambda E: E.tensor_scalar(out=tcol.t[:, 3:4], in0=tcol.t[:, 1:2], scalar1=-1.0, scalar2=None, op0=ALU.mult), reads=[tcol.B], writes=[tcol.B])
                P.op("act", lambda E: E.activation(out=lg.t[:], in_=lg.t[:], func=AF.Exp, bias=tcol.t[:, 3:4], scale=1.0), reads=[lg.B, tcol.B], writes=[lg.B])
                P.op("dve", lambda E: E.tensor_tensor(out=cw.t[:], in0=cw.t[:], in1=lg.t[:], op=ALU.mult), reads=[cw.B, lg.B], writes=[cw.B])
                P.op("dve", lambda E: E.tensor_reduce(out=tcol.t[:, 0:1], in_=cw.t[:], axis=mybir.AxisListType.X, op=ALU.add), reads=[cw.B, tcol.B], writes=[tcol.B])
                P.op("dve", lambda E: E.reciprocal(out=tcol.t[:, 0:1], in_=tcol.t[:, 0:1]), reads=[tcol.B], writes=[tcol.B])
                P.op("dve", lambda E, blk=blk: E.tensor_scalar(out=cw4.t[:, blk, :], in0=cw.t[:], scalar1=tcol.t[:, 0:1], scalar2=None, op0=ALU.mult),
                     reads=[cw.B, tcol.B], writes=[cw4.B])

        def emit_cwrep(e):
            bk = bank["H"]
            for blk in range(4):
                P.op("dve", lambda E, blk=blk: E.tensor_scalar(out=cwb.t[:], in0=ident.t[:], scalar1=0.0, scalar2=cw4.t[:, blk, e:e + 1], op0=ALU.mult, op1=ALU.add),
                     reads=[ident.B, cw4.B, cwb.B], writes=[cwb.B])
                mm(bk, bk.t[:, blk * 128:(blk + 1) * 128], cwb.t[:], ident.t[:], True, True, [cwb.B, ident.B])
            P.op("act", lambda E: E.activation(out=cwrep.t[:], in_=bk.t[:], func=AF.Copy), reads=[bk.B], writes=[cwrep.B])

        def emit_ffn(l, cnd, x, h2):
            g2 = mvec(l, cnd, 5)
            norm_mod(x, h2, Avec2(l, cnd), mvec(l, cnd, 3))
            if l == 1:
                emit_router(l, cnd, x)
            emit_ffn_panels(l, x, h2, g2)

        xA = sb("xA", (128, KC, TT), F32)
        xpark = nc.dram_tensor("xpark", [2, 128, KC, TT], F32, kind="Internal").ap()
        bpark = [Buf("xpark0"), Buf("xpark1")]
        kvsend = nc.dram_tensor("kvsend", [128, 4096], BF16, kind="Internal").ap()
        kvrecv = nc.dram_tensor("kvrecv", [256, 4096], BF16, addr_space="Local", kind="Internal").ap()
        bsend, brecv = Buf("kvsend"), Buf("kvrecv")
        kvsend0 = nc.dram_tensor("kvsend0", [128, 4096], BF16, kind="Internal").ap()
        kvrecv0 = nc.dram_tensor("kvrecv0", [256, 4096], BF16, addr_space="Local", kind="Internal").ap()
        bsend0, brecv0 = Buf("kvsend0"), Buf("kvrecv0")
        msend = [nc.dram_tensor("msend%d" % k, [128, (hi - lo) * 4], F32, kind="Internal").ap() for k, (lo, hi) in enumerate(ROUNDS)]
        mrecv = [nc.dram_tensor("mrecv%d" % k, [256, (hi - lo) * 4], F32, addr_space="Local", kind="Internal").ap()
                 for k, (lo, hi) in enumerate(ROUNDS)]
        bmsend = [Buf("msend%d" % k) for k in range(3)]
        bmrecv = [Buf("mrecv%d" % k) for k in range(3)]
        hb = sb("hb", (128, KC, TT), BF16)
        ob = sb("ob", (128, KC, TT), BF16)
        KT0 = sb("KT0", (128, 2, 2304), BF16)
        V0 = sb("V0", (128, 18, 256), BF16)
        KT1 = sb("KT1", (128, 2, 2304), BF16)
        V1 = sb("V1", (128, 18, 256), BF16)
        cstage = T(xstage.t[:, 0:512].rearrange("p (r f) -> p r f", r=2), 1)
        cstage.b = xstage.b
        cstage.B = xstage.B

        def load_rope(t):
            P.dma("sp", ropec.t[:], ropec_d[t], reads=[bIN], writes=[ropec.B])
            P.dma("sp", ropes.t[:], ropes_d[t], reads=[bIN], writes=[ropes.B])

        def load_cache(l, KTb, Vb):
            P.dma("sp", cstage.t, ck_d[l].rearrange("(r p) f -> p r f", p=128), reads=[bIN], writes=[cstage.B])
            H = bank["H"]
            for kvh in range(2):
                for r in range(2):
                    P.op("pe", lambda E, kvh=kvh, r=r: E.transpose(H.t[:, (kvh * 2 + r) * 128:(kvh * 2 + r + 1) * 128],
                                                                   cstage.t[:, r, kvh * 128:(kvh + 1) * 128], ident.t[:]),
                         reads=[cstage.B, ident.B], writes=[H.B])
            P.op("act", lambda E: E.activation(out=KTb.t[:, :, 0:256], in_=H.t[:].rearrange("p (k t) -> p k t", k=2), func=AF.Copy),
                 reads=[H.B], writes=[KTb.B])
            P.dma("pool", Vb.t[:, 0:2, :], cv_d[l].rearrange("(r p) f -> p r f", p=128), reads=[bIN], writes=[Vb.B])

        pgroups = [(0, 256, [0, 1]), (256, 512, [2, 3])]
        sgroups = [(0, 512, list(range(18)))]

        def schedule():
            bgc.update(hp=0, wo=0, ffn=0)
            load_cache(0, KT0, V0)
            load_x(xs_d[0], xA)
            load_rope(0)
            mod_step(8)
            load_cache(1, KT1, V1)
            for t in range(2):
                if t > 0:
                    load_x(xs_d[t], xA)
                    load_rope(t)
                norm_mod(xA, hb, Avec1(0, 1), mvec(0, 1, 0))
                emit_kv(0, hb, KT0, V0, 256 + t * 512, 2 + t * 4, rope=True)
                mod_step(8)
            P.dma("sp", kvsend0[:, 0:2048].rearrange("p (k n) -> p k n", k=2), KT0.t[:, :, 256:1280], reads=[KT0.B], writes=[bsend0])
            P.dma("sp", kvsend0[:, 2048:4096].rearrange("p (b d) -> p b d", b=8), V0.t[:, 2:10, :], reads=[V0.B], writes=[bsend0])
            P.coll(kvsend0, kvrecv0, PAIRS, reads=[bsend0], writes=[brecv0])
            for r in range(2):
                P.dma("sp", KT0.t[:, :, 256 + r * 1024:1280 + r * 1024],
                      kvrecv0[r * 128:(r + 1) * 128, 0:2048].rearrange("p (k n) -> p k n", k=2), reads=[brecv0], writes=[KT0.B])
                P.dma("sp", V0.t[:, 2 + r * 8:10 + r * 8, :],
                      kvrecv0[r * 128:(r + 1) * 128, 2048:4096].rearrange("p (b d) -> p b d", b=8), reads=[brecv0], writes=[V0.B])
            bgc.update(hp=4, wo=1, ffn=1)
            for t in (0, 1):
                load_x(xs_d[t], xA)
                load_rope(t)
                norm_mod(xA, hb, Avec1(0, 1), mvec(0, 1, 0))
                emit_mixer(0, 1, xA, hb, ob, KT0, V0, sgroups, rope=True)
                emit_ffn(0, 1, xA, hb)
                norm_mod(xA, hb, Avec1(1, 1), mvec(1, 1, 0))
                emit_kv(1, hb, KT1, V1, 256 + t * 512, 2 + t * 4, rope=True)
                P.dma("sp", xpark[t], xA.t[:], reads=[xA.B], writes=[bpark[t]])
            P.dma("sp", kvsend[:, 0:2048].rearrange("p (k n) -> p k n", k=2), KT1.t[:, :, 256:1280], reads=[KT1.B], writes=[bsend])
            P.dma("sp", kvsend[:, 2048:4096].rearrange("p (b d) -> p b d", b=8), V1.t[:, 2:10, :], reads=[V1.B], writes=[bsend])
            P.coll(kvsend, kvrecv, [[0, 1], [2, 3], [4, 5], [6, 7]], reads=[bsend], writes=[brecv])
            load_x(xp_d, xA)
            for l in range(NL):
                norm_mod(xA, hb, Avec1(l, 0), mvec(l, 0, 0))
                emit_kv(l, hb, KT0, V0, 0, 0, rope=False, cache_out=True)
                emit_mixer(l, 0, xA, hb, ob, KT0, V0, pgroups, rope=False)
                emit_ffn(l, 0, xA, hb)
            store_x(xA, yp_d)
            for r in range(2):
                P.dma("sp", KT1.t[:, :, 256 + r * 1024:1280 + r * 1024],
                      kvrecv[r * 128:(r + 1) * 128, 0:2048].rearrange("p (k n) -> p k n", k=2), reads=[brecv], writes=[KT1.B])
                P.dma("sp", V1.t[:, 2 + r * 8:10 + r * 8, :],
                      kvrecv[r * 128:(r + 1) * 128, 2048:4096].rearrange("p (b d) -> p b d", b=8), reads=[brecv], writes=[V1.B])
            for t in (0, 1):
                load_rope(t)
                P.dma("sp", xA.t[:], xpark[t], reads=[bpark[t]], writes=[xA.B])
                norm_mod(xA, hb, Avec1(1, 1), mvec(1, 1, 0))
                emit_mixer(1, 1, xA, hb, ob, KT1, V1, sgroups, rope=True)
                emit_ffn(1, 1, xA, hb)
                store_x(xA, ys_d[t])
            assert modst["i"] == 48 and modst["rounds"] == 3 and not deferred

        P.dry = True
        schedule()
        n_plan = wstate["taken"]
        wstate.update(issued=0, taken=0, released=0)
        modst.update(i=0, rounds=0)
        P.dry = False
        schedule()
        assert wstate["taken"] == n_plan == len(wstate["plan"]), (wstate["taken"], n_plan, len(wstate["plan"]))
        P.finish()
        build_program.stats = (P.n_ops, P.n_wait, nc.sbuf_bytes_remaining)
    return nc


def _rope_tables():
    L_ = 2048
    rows = (np.arange(L_) // 64).astype(np.float32)
    cols = (np.arange(L_) % 64).astype(np.float32)
    inv = (10000.0 ** (-np.arange(0, 64, 2, dtype=np.float32) / 64.0)).astype(np.float32)
    ar = rows[:, None] * inv[None, :]
    ac = cols[:, None] * inv[None, :]
    ang = np.concatenate([ar, ar, ac, ac], axis=1)
    return np.cos(ang).astype(np.float32), np.sin(ang).astype(np.float32)


def _rt_matrix():
    rt = np.zeros((128, 128), np.float32)
    for base in (0, 64):
        for i in range(32):
            m = base + i
            rt[m + 32, m] = -1.0
            rt[m, m + 32] = 1.0
    return rt


def _fm(v):
    v = np.asarray(v, np.float32)
    lead = v.shape[:-1]
    return np.ascontiguousarray(np.moveaxis(v.reshape(lead + (KC, 128)), -1, 0))


_NC_CACHE = {}


def kernel(x_prompt, x_sample, cache_k, cache_v, c, c_ctx, w_ada, b_ada, norm1_g, norm2_g,
           w_in, q_norm_g, k_norm_g, sgu_norm_g, w_spatial, b_spatial, out_norm_g, w_out,
           ffn_w_gate, ffn_w_up, ffn_w_down, w_router, b_router, moe_w_gate, moe_w_up, moe_w_down):
    f32 = lambda a: np.ascontiguousarray(np.asarray(a, dtype=np.float32))
    x_prompt, x_sample, cache_k, cache_v = f32(x_prompt), f32(x_sample), f32(cache_k), f32(cache_v)
    c, c_ctx = f32(c), f32(c_ctx)
    if "nc" not in _NC_CACHE:
        _NC_CACHE["nc"] = build_program()
    nc = _NC_CACHE["nc"]
    in_maps = _prep(x_prompt, x_sample, cache_k, cache_v, c, c_ctx, w_ada, b_ada, norm1_g, norm2_g,
                    w_in, q_norm_g, k_norm_g, sgu_norm_g, w_spatial, b_spatial, out_norm_g, w_out,
                    ffn_w_gate, ffn_w_up, ffn_w_down, w_router, b_router, moe_w_gate, moe_w_up, moe_w_down)
    res = run_bass_kernel_spmd(nc, in_maps, core_ids=list(range(NCORES)))
    return _assemble(res.results)


def _prep(x_prompt, x_sample, cache_k, cache_v, c, c_ctx, w_ada, b_ada, norm1_g, norm2_g,
          w_in, q_norm_g, k_norm_g, sgu_norm_g, w_spatial, b_spatial, out_norm_g, w_out,
          ffn_w_gate, ffn_w_up, ffn_w_down, w_router, b_router, moe_w_gate, moe_w_up, moe_w_down):
    f32 = lambda a: np.ascontiguousarray(np.asarray(a, dtype=np.float32))

    cos, sin = _rope_tables()
    w_ada_f = f32(w_ada)
    shared = {
        "n1g": _fm(norm1_g), "n2g": _fm(norm2_g),
        "bada": np.ascontiguousarray(np.moveaxis(f32(b_ada).reshape(NL, 96, 128), -1, 0)),
        "qg": np.ascontiguousarray(f32(q_norm_g).T), "kg": np.ascontiguousarray(f32(k_norm_g).T),
        "sgn": np.ascontiguousarray(np.moveaxis(f32(sgu_norm_g), -1, 0)),
        "bsrep": np.ascontiguousarray(np.broadcast_to(f32(b_spatial)[None], (128, NL, 8, 128))),
        "wsT": np.ascontiguousarray(np.transpose(f32(w_spatial), (3, 0, 1, 2))),
        "ong": _fm(out_norm_g),
        "wr": np.ascontiguousarray(np.transpose(f32(w_router)[0].reshape(KC, 128, NE), (1, 0, 2))),
        "brrep": np.ascontiguousarray(np.broadcast_to(f32(b_router)[0][None], (128, NE))),
        "ident": np.eye(128, dtype=np.float32), "rt": _rt_matrix(),
        "w_in": f32(w_in), "w_out": f32(w_out),
        "ffn_w_gate": f32(ffn_w_gate), "ffn_w_up": f32(ffn_w_up), "ffn_w_down": f32(ffn_w_down),
        "moe_w_gate": f32(moe_w_gate), "moe_w_up": f32(moe_w_up), "moe_w_down": f32(moe_w_down),
    }
    in_maps = []
    for core in range(NCORES):
        b, half = core // 2, core % 2
        own = x_sample[b, half * 1024:(half + 1) * 1024].reshape(2, TT, D)
        oth = x_sample[b, (1 - half) * 1024:(2 - half) * 1024].reshape(2, TT, D)
        pos = np.concatenate([np.arange(half * 1024, (half + 1) * 1024), np.arange((1 - half) * 1024, (2 - half) * 1024)])
        m = dict(shared)
        m["xs"] = np.ascontiguousarray(np.concatenate([own, oth], axis=0))
        m["xp"] = np.ascontiguousarray(x_prompt[2 * core:2 * core + 2].reshape(TT, D))
        m["ropec"] = np.ascontiguousarray(cos[pos].reshape(4, TT, 128).transpose(0, 2, 1))
        m["ropes"] = np.ascontiguousarray(sin[pos].reshape(4, TT, 128).transpose(0, 2, 1))
        m["ck"] = np.ascontiguousarray(cache_k[b].reshape(NL, 256, 256))
        m["cv"] = np.ascontiguousarray(cache_v[b].reshape(NL, 256, 256))
        cv2 = np.stack([c_ctx, c[b]], axis=-1)
        m["cvec"] = np.ascontiguousarray(cv2.reshape(KC, 128, 2).transpose(1, 0, 2))
        m["w_ada"] = np.ascontiguousarray(w_ada_f.reshape(NL, D, 24, 2, 256)[:, :, :, half, :].reshape(NL, D, 3 * D))
        in_maps.append(m)
    return in_maps


def _assemble(R):
    y_prompt = np.empty((16, 256, D), np.float32)
    y_sample = np.empty((4, 2048, D), np.float32)
    nk = np.empty((16, NL, 256, 2, 128), np.float32)
    nv = np.empty((16, NL, 256, 2, 128), np.float32)
    for core in range(NCORES):
        b, half = core // 2, core % 2
        r = R[core]
        y_prompt[2 * core:2 * core + 2] = np.asarray(r["yp"]).reshape(2, 256, D)
        y_sample[b, half * 1024:(half + 1) * 1024] = np.asarray(r["ys"]).reshape(1024, D)
        nk[2 * core:2 * core + 2] = np.asarray(r["nk"]).reshape(2, NL, 256, 2, 128)
        nv[2 * core:2 * core + 2] = np.asarray(r["nv"]).reshape(2, NL, 256, 2, 128)
    return (y_prompt, y_sample, nk, nv)
```
